# Optimizing a Trainium2 kernel written in Bass

```python
import jax, jax.numpy as jnp
from jax import lax
import numpy as np

D_MODEL = 1024
BATCH = 8
SEQ = 4096
DEPTH = 4

GRID_W = 64
CTX_LEN = 256
N_MIXERS = 2
N_MOD = 6
CHUNK_A = 128
A_WIDTH = D_MODEL
A_GROUPS = 8
A_GROUP_DIM = A_WIDTH // A_GROUPS
RET_HEADS = 4
RET_DK = D_MODEL // RET_HEADS
RET_DV = D_MODEL // RET_HEADS
RET_CHUNK = 128
ROPE_BASE = 10000.0
N_EXPERTS = 16
EC_CAPACITY = 2
EXPERT_FF = D_MODEL
EPS = 1e-6

kernel_name = 'hybrid_gmlp_retention_ec_dit'


def rms_norm(x, g):
    x32 = x.astype(jnp.float32)
    y = x32 * lax.rsqrt(jnp.mean(x32 * x32, axis=-1, keepdims=True) + EPS)
    return (y * g.astype(jnp.float32)).astype(x.dtype)


def layer_norm(x, g, b):
    x32 = x.astype(jnp.float32)
    mu = jnp.mean(x32, axis=-1, keepdims=True)
    var = jnp.mean(jnp.square(x32 - mu), axis=-1, keepdims=True)
    y = (x32 - mu) * lax.rsqrt(var + EPS)
    return (y * g.astype(jnp.float32) + b.astype(jnp.float32)).astype(x.dtype)


def head_norm(o):
    mu = jnp.mean(o, axis=-1, keepdims=True)
    var = jnp.mean(jnp.square(o - mu), axis=-1, keepdims=True)
    return (o - mu) * lax.rsqrt(var + EPS)


def modulate(h, shift, scale):
    return h * (1.0 + scale) + shift


def chunk_gmlp(h, w_in, ln_g, ln_b, w_s, b_s, w_out):
    bsz, n, _ = h.shape
    z = jax.nn.gelu(h @ w_in)
    u, v = jnp.split(z, 2, axis=-1)
    v = layer_norm(v, ln_g, ln_b)
    v = v.reshape(bsz, n // CHUNK_A, CHUNK_A, A_GROUPS, A_GROUP_DIM)
    s = jnp.einsum('gnm,bcmgd->bcngd', w_s, v) + jnp.swapaxes(b_s, 0, 1)[None, None, :, :, None].astype(v.dtype)
    return (u * s.reshape(bsz, n, A_WIDTH)) @ w_out


def split_heads(t, d):
    bsz, n, _ = t.shape
    return t.reshape(bsz, n, RET_HEADS, d).transpose(0, 2, 1, 3)


def axial_rope(t):
    n = t.shape[2]
    rows = n // GRID_W
    pos_r = jnp.repeat(jnp.arange(rows), GRID_W).astype(jnp.float32)
    pos_c = jnp.tile(jnp.arange(GRID_W), rows).astype(jnp.float32)
    n_freq = RET_DK // 4
    inv = jnp.power(ROPE_BASE, -jnp.arange(n_freq, dtype=jnp.float32) / n_freq)
    ang = jnp.concatenate([pos_r[:, None] * inv[None], pos_c[:, None] * inv[None]], axis=-1)
    cos, sin = jnp.cos(ang), jnp.sin(ang)
    t1, t2 = jnp.split(t, 2, axis=-1)
    return jnp.concatenate([t1 * cos - t2 * sin, t1 * sin + t2 * cos], axis=-1)


def decay_tables(log_g, strict):
    idx = jnp.arange(RET_CHUNK, dtype=jnp.float32)
    diff = idx[:, None] - idx[None, :]
    mask = (diff > 0) if strict else (diff >= 0)
    intra = jnp.where(mask, jnp.exp(jnp.where(mask, diff, 0.0)[None] * log_g[:, None, None]), 0.0)
    q_dec = jnp.exp((idx + 1.0)[None] * log_g[:, None])
    k_dec = jnp.exp((RET_CHUNK - 1.0 - idx)[None] * log_g[:, None])
    chunk_dec = jnp.exp(RET_CHUNK * log_g)
    return intra, q_dec, k_dec, chunk_dec


def retention_scan(q, k, v, log_g, s0, strict):
    bsz, h, n, _ = q.shape
    nc = n // RET_CHUNK
    intra, q_dec, k_dec, chunk_dec = decay_tables(log_g, strict)

    def to_chunks(t):
        return jnp.moveaxis(t.reshape(bsz, h, nc, RET_CHUNK, t.shape[-1]), 2, 0)

    def step(s, qkv):
        qc, kc, vc = qkv
        att = jnp.einsum('bhnk,bhmk->bhnm', qc, kc) * intra[None]
        o = jnp.einsum('bhnm,bhmv->bhnv', att, vc) + jnp.einsum('bhnk,bhkv->bhnv', qc * q_dec[None, :, :, None], s)
        s = chunk_dec[None, :, None, None] * s + jnp.einsum('bhmk,bhmv->bhkv', kc * k_dec[None, :, :, None], vc)
        return s, o

    s_fin, o = lax.scan(step, s0, (to_chunks(q), to_chunks(k), to_chunks(v)))
    return jnp.moveaxis(o, 0, 2).reshape(bsz, h, n, -1), s_fin


def retention_final_state(k, v, log_g):
    n = k.shape[2]
    w = jnp.exp((n - 1.0 - jnp.arange(n, dtype=jnp.float32))[None] * log_g[:, None])
    return jnp.einsum('bhnk,hn,bhnv->bhkv', k, w, v)


def retention_merge(o_f, o_b, g_f, g_b, w_out, dtype):
    def heads_out(o):
        bsz, h, n, dv = o.shape
        return head_norm(o).transpose(0, 2, 1, 3).reshape(bsz, n, h * dv)
    y = jax.nn.silu(g_f) * heads_out(o_f) + jax.nn.silu(g_b) * heads_out(o_b)
    return y.astype(dtype) @ w_out


def retention_mixer(h_ctx, h_lat, w_in, decay_f, decay_b, w_out, need_ctx_out):
    qk_w = RET_HEADS * RET_DK
    v_w = RET_HEADS * RET_DV
    k_scale = RET_DK ** -0.5
    lg_f = jax.nn.log_sigmoid(decay_f.astype(jnp.float32))
    lg_b = jax.nn.log_sigmoid(decay_b.astype(jnp.float32))

    def flip(t):
        return t[:, :, ::-1]

    def project(h):
        z = (h @ w_in).astype(jnp.float32)
        q, k, v, g_f, g_b = jnp.split(z, [qk_w, 2 * qk_w, 2 * qk_w + v_w, 2 * qk_w + 2 * v_w], axis=-1)
        return split_heads(q, RET_DK), split_heads(k, RET_DK) * k_scale, split_heads(v, RET_DV), g_f, g_b

    bsz = h_ctx.shape[0]
    if need_ctx_out:
        qc, kc, vc, gfc, gbc = project(h_ctx)
        s0 = jnp.zeros((bsz, RET_HEADS, RET_DK, RET_DV), jnp.float32)
        oc_f, s_f = retention_scan(qc, kc, vc, lg_f, s0, False)
        oc_b, s_b = retention_scan(flip(qc), flip(kc), flip(vc), lg_b, s0, True)
        y_ctx = retention_merge(oc_f, flip(oc_b), gfc, gbc, w_out, h_ctx.dtype)
    else:
        z = (h_ctx @ w_in[:, qk_w:2 * qk_w + v_w]).astype(jnp.float32)
        kc = split_heads(z[..., :qk_w], RET_DK) * k_scale
        vc = split_heads(z[..., qk_w:], RET_DV)
        s_f = retention_final_state(kc, vc, lg_f)
        s_b = retention_final_state(flip(kc), flip(vc), lg_b)
        y_ctx = None

    q, k, v, g_f, g_b = project(h_lat)
    q = axial_rope(q)
    k = axial_rope(k)
    o_f, _ = retention_scan(q, k, v, lg_f, s_f, False)
    o_b, _ = retention_scan(flip(q), flip(k), flip(v), lg_b, s_b, True)
    y_lat = retention_merge(o_f, flip(o_b), g_f, g_b, w_out, h_lat.dtype)
    return y_ctx, y_lat


def expert_choice_ffn(h, w_router, w_gate, w_up, w_down):
    bsz, n, d = h.shape
    cap = EC_CAPACITY * n // N_EXPERTS
    aff = jax.nn.softmax((h @ w_router).astype(jnp.float32), axis=-1)
    gate, idx = lax.top_k(jnp.swapaxes(aff, 1, 2), cap)
    xs = jax.vmap(lambda hb, ib: hb[ib])(h, idx)
    hid = jax.nn.silu(jnp.einsum('becd,edf->becf', xs, w_gate)) * jnp.einsum('becd,edf->becf', xs, w_up)
    ye = jnp.einsum('becf,efd->becd', hid, w_down) * gate[..., None].astype(h.dtype)
    return jax.vmap(lambda yb, ib: jnp.zeros((n, d), yb.dtype).at[ib.reshape(-1)].add(yb.reshape(-1, d)))(ye, idx)


def setup_inputs(seed: int = 0) -> dict:
    key = jax.random.key(seed)
    ks = jax.random.split(key, 24)
    n_a = (DEPTH + 1) // 2
    n_b = DEPTH // 2
    f32 = jnp.float32

    def nrm(k, shape, scale):
        return jax.random.normal(k, shape, f32) * scale

    d = D_MODEL
    decay_base = jnp.log(jnp.power(2.0, 5.0 + jnp.arange(RET_HEADS, dtype=f32)) - 1.0)
    return {
        'x': nrm(ks[0], (BATCH, SEQ, d), 1.0),
        'c': nrm(ks[1], (BATCH, d), 1.0),
        'ctx': nrm(ks[2], (BATCH, CTX_LEN, d), 1.0),
        'c_ctx': nrm(ks[3], (d,), 1.0),
        'ada_w': nrm(ks[4], (DEPTH, d, N_MOD * d), 0.5 * d ** -0.5),
        'ada_b': nrm(ks[5], (DEPTH, N_MOD * d), 0.02),
        'norm_mix_g': 1.0 + nrm(ks[6], (DEPTH, d), 0.02),
        'norm_ffn_g': 1.0 + nrm(ks[7], (DEPTH, d), 0.02),
        'a_w_in': nrm(ks[8], (n_a, d, 2 * A_WIDTH), d ** -0.5),
        'a_ln_g': 1.0 + nrm(ks[9], (n_a, A_WIDTH), 0.02),
        'a_ln_b': nrm(ks[10], (n_a, A_WIDTH), 0.02),
        'a_w_s': nrm(ks[11], (n_a, A_GROUPS, CHUNK_A, CHUNK_A), CHUNK_A ** -0.5),
        'a_b_s': 1.0 + nrm(ks[12], (n_a, A_GROUPS, CHUNK_A), 0.02),
        'a_w_out': nrm(ks[13], (n_a, A_WIDTH, d), A_WIDTH ** -0.5),
        'r_w_in': nrm(ks[14], (n_b, d, 2 * RET_HEADS * RET_DK + 3 * RET_HEADS * RET_DV), d ** -0.5),
        'r_decay_f': decay_base[None] + nrm(ks[15], (n_b, RET_HEADS), 0.1),
        'r_decay_b': decay_base[None] + nrm(ks[16], (n_b, RET_HEADS), 0.1),
        'r_w_out': nrm(ks[17], (n_b, RET_HEADS * RET_DV, d), (RET_HEADS * RET_DV) ** -0.5),
        'moe_w_router': nrm(ks[18], (DEPTH, d, N_EXPERTS), d ** -0.5),
        'moe_w_gate': nrm(ks[19], (DEPTH, N_EXPERTS, d, EXPERT_FF), d ** -0.5),
        'moe_w_up': nrm(ks[20], (DEPTH, N_EXPERTS, d, EXPERT_FF), d ** -0.5),
        'moe_w_down': nrm(ks[21], (DEPTH, N_EXPERTS, EXPERT_FF, d), EXPERT_FF ** -0.5),
        'final_norm_g': 1.0 + nrm(ks[22], (d,), 0.02),
    }


def reference(x, c, ctx, c_ctx, ada_w, ada_b, norm_mix_g, norm_ffn_g, a_w_in, a_ln_g, a_ln_b, a_w_s, a_b_s,
              a_w_out, r_w_in, r_decay_f, r_decay_b, r_w_out, moe_w_router, moe_w_gate, moe_w_up, moe_w_down,
              final_norm_g):
    d = D_MODEL
    for i in range(DEPTH):
        last = i == DEPTH - 1
        kind = i % N_MIXERS
        j = i // N_MIXERS
        mod_lat = jax.nn.silu(c) @ ada_w[i] + ada_b[i]
        sh1, sc1, g1, sh2, sc2, g2 = [m[:, None, :] for m in jnp.split(mod_lat, N_MOD, axis=-1)]
        h_lat = modulate(rms_norm(x, norm_mix_g[i]), sh1, sc1)
        ctx_read = (not last) or kind == 1
        if ctx_read:
            n_mod_ctx = N_MOD if not last else 2
            mod_ctx = jax.nn.silu(c_ctx) @ ada_w[i, :, :n_mod_ctx * d] + ada_b[i, :n_mod_ctx * d]
            parts = jnp.split(mod_ctx, n_mod_ctx)
            h_ctx = modulate(rms_norm(ctx, norm_mix_g[i]), parts[0], parts[1])
        if kind == 0:
            y_lat = chunk_gmlp(h_lat, a_w_in[j], a_ln_g[j], a_ln_b[j], a_w_s[j], a_b_s[j], a_w_out[j])
            y_ctx = None if last else chunk_gmlp(h_ctx, a_w_in[j], a_ln_g[j], a_ln_b[j], a_w_s[j], a_b_s[j], a_w_out[j])
        else:
            y_ctx, y_lat = retention_mixer(h_ctx, h_lat, r_w_in[j], r_decay_f[j], r_decay_b[j], r_w_out[j],
                                           not last)
        x = x + g1 * y_lat
        x = x + g2 * expert_choice_ffn(modulate(rms_norm(x, norm_ffn_g[i]), sh2, sc2),
                                       moe_w_router[i], moe_w_gate[i], moe_w_up[i], moe_w_down[i])
        if not last:
            ctx = ctx + parts[2] * y_ctx
            ctx = ctx + parts[5] * expert_choice_ffn(modulate(rms_norm(ctx, norm_ffn_g[i]), parts[3], parts[4]),
                                                     moe_w_router[i], moe_w_gate[i], moe_w_up[i], moe_w_down[i])
    return rms_norm(x, final_norm_g)
```

```python
import numpy as np
from contextlib import ExitStack
import concourse.bass as bass
import concourse.mybir as mybir
from concourse.bass_utils import run_bass_kernel_spmd

F32 = mybir.dt.float32
BF16 = mybir.dt.bfloat16
F16 = mybir.dt.float16
I32 = mybir.dt.int32
AF = mybir.ActivationFunctionType
ALU = mybir.AluOpType
AX = mybir.AxisListType

D = 1024
NL = 4096
NCX = 256
NT = 34
NLT = 32
DEPTH = 4
NE = 16
CAP = 512
CAPC = 32
EPS = 1e-6


class Buf:
    __slots__ = ("name", "w", "r")

    def __init__(self, name=""):
        self.name = name
        self.w = None
        self.r = {}


class T:
    __slots__ = ("ap", "bufs")

    def __init__(self, ap, bufs):
        self.ap = ap
        self.bufs = bufs if isinstance(bufs, (list, tuple)) else [bufs]

    def __getitem__(self, k):
        return T(self.ap[k], self.bufs)

    def re(self, s, **kw):
        return T(self.ap.rearrange(s, **kw), self.bufs)

    def bitcast(self, dt):
        return T(self.ap.bitcast(dt), self.bufs)

    def bc(self, axis, shape):
        return T(self.ap.unsqueeze(axis).to_broadcast(list(shape)), self.bufs)

    @property
    def shape(self):
        return self.ap.shape


class Prog:
    ENGS = ("pe", "dve", "act", "pool", "sp")
    NDMA = {"sp": 24, "pool": 12, "act": 8}

    def __init__(self, nc, same_engine_sync=True):
        self.nc = nc
        self.items = {e: [] for e in self.ENGS}
        self.cnt = {e: 0 for e in self.ENGS}
        self.dma_idx = {q: 0 for q in self.NDMA}
        self.seen = {e: {} for e in self.ENGS}
        self.same_engine_sync = same_engine_sync
        self.sems = {}
        self.last_dma = {}

    def _need(self, eng, dep):
        if dep is None:
            return
        key, val = dep
        if key == eng and (eng == "pe" or not self.same_engine_sync):
            return
        if self.seen[eng].get(key, 0) >= val:
            return
        self.seen[eng][key] = val
        self.items[eng].append(("wait", key, val))

    def _deps(self, eng, reads, writes):
        for b in reads:
            self._need(eng, b.w)
        for b in writes:
            self._need(eng, b.w)
            for d in b.r.items():
                self._need(eng, d)

    def _mark(self, me, reads, writes):
        for b in reads:
            if b.r.get(me[0], 0) < me[1]:
                b.r[me[0]] = me[1]
        for b in writes:
            b.w = me
            b.r = {}

    def op(self, eng, fn, reads=(), writes=()):
        self._deps(eng, reads, writes)
        self.cnt[eng] += 1
        me = (eng, self.cnt[eng])
        self.items[eng].append(("op", fn, eng))
        self._mark(me, reads, writes)
        return me

    def dma(self, q, fn, reads=(), writes=()):
        R = self.NDMA[q]
        i = self.dma_idx[q]
        self.dma_idx[q] += 1
        key = ("dma", q, i % R)
        if i >= R:
            self._need(q, (key, 16 * (i // R)))
        self._deps(q, reads, writes)
        me = (key, 16 * (i // R + 1))
        self.items[q].append(("dma", fn, key))
        self._mark(me, reads, writes)
        self.last_dma[key] = me
        return me

    def barrier(self):
        for e in self.ENGS:
            for e2 in ("pe", "dve", "act", "pool"):
                if self.cnt[e2] > 0:
                    self._need(e, (e2, self.cnt[e2]))
            for key, me in self.last_dma.items():
                self._need(e, me)

    def emit(self):
        nc = self.nc
        with ExitStack() as es:
            for e in ("pe", "dve", "act", "pool"):
                self.sems[e] = es.enter_context(nc.semaphore("s_" + e))
            for q, R in self.NDMA.items():
                for j in range(R):
                    self.sems[("dma", q, j)] = es.enter_context(nc.semaphore("d_%s_%d" % (q, j)))
            block = es.enter_context(nc.Block())
            sems = self.sems

            def run(engobj, items):
                for it in items:
                    if it[0] == "wait":
                        engobj.wait_ge(sems[it[1]], it[2])
                    elif it[0] == "op":
                        it[1](engobj).then_inc(sems[it[2]], 1)
                    else:
                        it[1](engobj).then_inc(sems[it[2]], 16)

            @block.tensor
            def _(e):
                run(e, self.items["pe"])

            @block.vector
            def _(e):
                run(e, self.items["dve"])

            @block.scalar
            def _(e):
                run(e, self.items["act"])

            @block.gpsimd
            def _(e):
                run(e, self.items["pool"])

            @block.sync
            def _(e):
                run(e, self.items["sp"])


def _b(*ts):
    out = []
    for t in ts:
        if isinstance(t, T):
            out.extend(t.bufs)
    return out


def _a(x):
    return x.ap if isinstance(x, T) else x


class K:
    def __init__(self, nlayers=DEPTH, debug=False):
        self.nlayers = nlayers
        self.debug = debug
        self.nc = bass.Bass("TRN2", target_bir_lowering=False)
        self.p = Prog(self.nc)
        self.es = ExitStack()
        self.dq = 0

    def act(self, out, in_, func, bias=None, scale=None, accum=None):
        kw = {}
        if bias is not None:
            kw["bias"] = _a(bias)
        if scale is not None:
            kw["scale"] = _a(scale)
        if accum is not None:
            kw["accum_out"] = _a(accum)
        o, i = out.ap, in_.ap
        self.p.op("act", lambda e: e.activation(out=o, in_=i, func=func, **kw),
                  _b(in_, bias, scale), _b(out, accum))

    def ts(self, eng, out, in0, s1, s2=None, op0=ALU.mult, op1=None, accum=None):
        kw = {}
        if op1 is not None:
            kw["op1"] = op1
        if accum is not None:
            kw["accum_out"] = _a(accum)
        o, i, a1, a2 = out.ap, in0.ap, _a(s1), _a(s2)
        ename = eng
        self.p.op(ename, lambda e: e.tensor_scalar(out=o, in0=i, scalar1=a1, scalar2=a2, op0=op0, **kw),
                  _b(in0, s1, s2), _b(out, accum))

    def tt(self, eng, out, in0, in1, op):
        o, a, b = out.ap, in0.ap, in1.ap
        self.p.op(eng, lambda e: e.tensor_tensor(out=o, in0=a, in1=b, op=op), _b(in0, in1), _b(out))

    def stt(self, out, in0, scalar, in1, op0, op1):
        o, a, s, b = out.ap, in0.ap, _a(scalar), in1.ap
        self.p.op("dve", lambda e: e.scalar_tensor_tensor(out=o, in0=a, scalar=s, in1=b, op0=op0, op1=op1),
                  _b(in0, scalar, in1), _b(out))

    def copy(self, eng, out, in_):
        o, i = out.ap, in_.ap
        if eng == "act":
            self.p.op("act", lambda e: e.activation(out=o, in_=i, func=AF.Copy), _b(in_), _b(out))
        else:
            self.p.op(eng, lambda e: e.tensor_copy(out=o, in_=i), _b(in_), _b(out))

    def memset(self, eng, out, val):
        o = out.ap
        self.p.op(eng, lambda e: e.memset(o, val), (), _b(out))

    def recip(self, out, in_):
        o, i = out.ap, in_.ap
        self.p.op("dve", lambda e: e.reciprocal(out=o, in_=i), _b(in_), _b(out))

    def mm(self, out, pairs, extra_reads=()):
        o = out.ap
        ps = [(l.ap, r.ap) for l, r in pairs]
        n = len(ps)

        def fn(e):
            ins = None
            for i, (l, r) in enumerate(ps):
                ins = e.matmul(o, l, r, start=(i == 0), stop=(i == n - 1))
            return ins
        rd = []
        for l, r in pairs:
            rd += _b(l, r)
        self.p.op("pe", fn, rd + list(extra_reads), _b(out))

    def tr(self, out, in_, ident):
        o, i = out.ap, in_.ap
        P = i.shape[0]
        d = ident.ap[0:P, 0:P]
        self.p.op("pe", lambda e: e.transpose(out=o, in_=i, identity=d), _b(in_, ident), _b(out))

    def trs(self, items, ident):
        lst = [(o.ap, i.ap) for o, i in items]
        d = ident.ap

        def fn(e):
            ins = None
            for o, i in lst:
                P = i.shape[0]
                ins = e.transpose(out=o, in_=i, identity=d[0:P, 0:P])
            return ins
        rd, wr = _b(ident), []
        for o, i in items:
            rd += _b(i)
            wr += _b(o)
        self.p.op("pe", fn, rd, wr)

    def dma(self, out, in_, q=None, **kw):
        if q is None:
            q = "sp"
        o, i = out.ap, in_.ap
        self.p.dma(q, lambda e: e.dma_start(out=o, in_=i, **kw), _b(in_), _b(out))

    def arena_reset(self, off=None):
        self.aoff = self.persist_end if off is None else off

    def alloc(self, shape, dt, name=""):
        esz = {F32: 4, BF16: 2, F16: 2, I32: 4}[dt]
        n = int(np.prod(shape[1:])) * esz
        n4 = (n + 3) // 4
        off = self.aoff
        self.aoff += n4 + (-n4) % 8
        assert self.aoff <= self.arena_n, ("SBUF arena overflow", name, self.aoff * 4)
        ap = self.arena[0:shape[0], off:off + n4]
        if dt != F32:
            ap = ap.bitcast(dt)
        ap = ap[:, 0:int(np.prod(shape[1:]))]
        if len(shape) > 2:
            names = " ".join("d%d" % i for i in range(len(shape) - 1))
            ap = ap.rearrange("p (%s) -> p %s" % (names, names), **{"d%d" % i: shape[i + 1] for i in range(len(shape) - 1)})
        return T(ap, Buf(name))

    def build(self):
        nc, es = self.nc, self.es
        dbg = self.debug

        def din(name, shape, dt=F32):
            return nc.dram_tensor(name, list(shape), dt, kind="ExternalInput").ap()

        def dscr(name, shape, dt=F32):
            kind = "ExternalOutput" if (dbg and name in dbg) else "Internal"
            return nc.dram_tensor(name, list(shape), dt, kind=kind).ap()

        I = {}
        I["x"] = din("x", [NL, D])
        I["ctx"] = din("ctx", [NCX, D])
        I["cvec"] = din("cvec", [2, D])
        I["ada_w"] = din("ada_w", [DEPTH, D, 6 * D])
        I["ada_b"] = din("ada_b", [DEPTH, 6 * D])
        I["norm_mix_g"] = din("norm_mix_g", [DEPTH, D])
        I["norm_ffn_g"] = din("norm_ffn_g", [DEPTH, D])
        I["a_w_in"] = din("a_w_in", [2, D, 2 * D])
        I["a_ln_g"] = din("a_ln_g", [2, D])
        I["a_ln_b"] = din("a_ln_b", [2, D])
        I["a_w_s"] = din("a_w_s", [2, 8, 128, 128])
        I["a_b_s"] = din("a_b_s", [2, 8, 128])
        I["a_w_out"] = din("a_w_out", [2, D, D])
        I["r_w_in"] = din("r_w_in", [2, D, 5 * D])
        I["r_decay_f"] = din("r_decay_f", [2, 4])
        I["r_decay_b"] = din("r_decay_b", [2, 4])
        I["r_w_out"] = din("r_w_out", [2, D, D])
        I["moe_w_router"] = din("moe_w_router", [DEPTH, D, NE])
        I["moe_w_gate"] = din("moe_w_gate", [DEPTH, NE, D, D])
        I["moe_w_up"] = din("moe_w_up", [DEPTH, NE, D, D])
        I["moe_w_down"] = din("moe_w_down", [DEPTH, NE, D, D])
        I["final_norm_g"] = din("final_norm_g", [D])
        I["ropecs"] = din("ropecs", [2, 128, NL])
        self.I = I
        self.out = nc.dram_tensor("out", [NL, D], F32, kind="ExternalOutput").ap()
        self.outB = [Buf("out%d" % t) for t in range(NLT)]

        self.Xd = dscr("X", [NT * 128, D])
        self.XB = [Buf("X%d" % t) for t in range(NT)]
        self.H2d = dscr("H2", [NT * 128, D], BF16)
        self.H2B = [Buf("H2_%d" % t) for t in range(NT)]
        self.BCRd = dscr("BCR", [DEPTH, 2, 6 * D])
        self.BCRB = [Buf("BCR%d" % i) for i in range(DEPTH)]
        self.QTd = dscr("QT", [NT, 128, 8, 128], BF16)
        self.KTd = dscr("KT", [NT, 128, 8, 128], BF16)
        self.KKd = dscr("KK", [NT, 128, D], BF16)
        self.VVd = dscr("VV", [NT, 128, D], BF16)
        self.GFd = dscr("GF", [NT, 128, D], BF16)
        self.GBd = dscr("GB", [NT, 128, D], BF16)
        self.HNd = dscr("HN", [NT, 128, D])
        self.RB = {n: [Buf("%s%d" % (n, t)) for t in range(NT)] for n in ("QT", "KT", "KK", "VV", "GF", "GB", "HN")}
        if dbg and "AFF" in dbg:
            self.AFFd = dscr("AFF", [128, NT * NE])
            self.IDXd = dscr("IDX", [128, NE * 5], I32)
            self.GATd = dscr("GAT", [128, NE * 5])

        self.arena_n = 49 * 1024
        self.arena = es.enter_context(nc.sbuf_tensor("arena", [128, self.arena_n], F32))
        self.aoff = 0
        self.persist_end = 0
        self.PS = []
        for b in range(8):
            t = es.enter_context(nc.psum_tensor("ps%d" % b, [128, 512], F32))
            self.PS.append(T(t[:, :], Buf("ps%d" % b)))

        self.identF = self.alloc([128, 128], F32, "identF")
        self.identB = self.alloc([128, 128], BF16, "identB")
        self.iota512 = self.alloc([128, 512], F16, "iota512")
        self.affTok = self.alloc([128, NT, NE], F32, "affTok")
        self.csT = self.alloc([128, 8, 2], F32, "csT")
        self.idxAll = self.alloc([128, NE, 5], I32, "idxAll")
        self.gateAll = self.alloc([128, NE, 5], F32, "gateAll")
        self.epsT = self.alloc([128, 1], F32, "eps")
        self.persist_end = self.aoff

        self.setup_consts()
        self.phase_mods()
        for i in range(self.nlayers):
            last = i == DEPTH - 1
            if i % 2 == 0:
                self.phase_gmlp(i)
            else:
                self.phase_ret(i)
            self.phase_routing(i)
            self.phase_experts(i)
        if self.nlayers == DEPTH:
            self.phase_final()
        self.p.barrier()
        self.p.emit()
        return self.nc

    def setup_consts(self):
        p = self.p
        iF, iB = self.identF, self.identB
        self.memset("pool", iF, 1.0)
        o = iF.ap
        p.op("pool", lambda e, o=o: e.affine_select(out=o, in_=o, pattern=[[-1, 128]], base=0, channel_multiplier=1,
                                               compare_op=ALU.is_equal, fill=0.0), _b(iF), _b(iF))
        self.copy("dve", iB, iF)
        io = self.iota512.ap
        p.op("pool", lambda e, io=io: e.iota(io, pattern=[[1, 512]], base=0, channel_multiplier=0,
                                      allow_small_or_imprecise_dtypes=True), (), _b(self.iota512))
        self.memset("dve", self.epsT, EPS)
        self.arena_reset()
        cv2 = self.alloc([2, D], F32, "cv2")
        self.dma(cv2, T(self.I["cvec"], Buf("cvec")))
        self.act(cv2, cv2, AF.Silu)
        pp = self.PS[0]
        self.trs([(pp[:, k * 2:(k + 1) * 2], cv2[:, k * 128:(k + 1) * 128]) for k in range(8)], self.identF)
        self.copy("dve", self.csT, pp[:, 0:16].re("p (k r) -> p k r", k=8))

    def load_bc(self, dst, src_ap1d, buf=None, q="sp"):
        P = dst.shape[0]
        self.dma(dst, T(src_ap1d.partition_broadcast(P), buf if buf is not None else Buf("const")), q=q)

    def cast_pieces(self, dst, src_ap, stage, nk=8):
        ncols = dst.shape[2]
        sv = src_ap.rearrange("(kc p) n -> p kc n", p=128)
        out = []
        for c0 in range(0, ncols, 1024):
            cw = min(1024, ncols - c0)
            for k0 in range(0, nk, 2):
                def piece(c0=c0, cw=cw, k0=k0):
                    st = stage[self.dq % len(stage)]
                    self.dq += 1
                    s = st[:, :, 0:cw]
                    self.dma(s, T(sv[:, k0:k0 + 2, c0:c0 + cw], Buf("w")))
                    d = dst[:, k0:k0 + 2, c0:c0 + cw]
                    m = self.dq % 8
                    eng = "dve" if m < 3 else ("act" if m < 6 else "pool")
                    self.copy(eng, d, s)
                out.append(piece)
        return out

    def load_cast(self, dst, src_ap, stage, nk=8):
        for f in self.cast_pieces(dst, src_ap, stage, nk):
            f()

    def phase_mods(self):
        p = self.p
        p.barrier()
        self.arena_reset()
        I = self.I
        stg = [self.alloc([128, 8, 512], F32, "adastg%d" % s) for s in range(2)]
        modv = self.alloc([2, 6 * D], F32, "modv")
        adab = self.alloc([2, 6 * D], F32, "adab")
        ng = self.alloc([2, 2, D], F32, "ng")
        for i in range(self.nlayers):
            self.load_bc(adab, I["ada_b"][i])
            self.load_bc(ng[:, 0, :], I["norm_mix_g"][i])
            self.load_bc(ng[:, 1, :], I["norm_ffn_g"][i])
            wv = I["ada_w"][i].rearrange("(kc p) n -> p kc n", p=128)
            for cg in range(12):
                st = stg[cg % 2]
                self.dma(st, T(wv[:, :, cg * 512:(cg + 1) * 512], Buf("adaw")))
                ps = self.PS[cg % 2][0:2, :]
                self.mm(ps, [(self.csT[:, kc, :], st[:, kc, :]) for kc in range(8)])
                self.tt("dve", modv[:, cg * 512:(cg + 1) * 512], ps, adab[:, cg * 512:(cg + 1) * 512], ALU.add)
            for s, g in ((1, 0), (4, 1)):
                v = modv[:, s * D:(s + 1) * D]
                self.stt(v, v, 1.0, ng[:, g, :], ALU.add, ALU.mult)
            self.dma(T(self.BCRd[i], self.BCRB[i]), modv)

    def rms_mod(self, xt, junk, A, B, out32, ss, rstd, out_eng="pool", outb=None):
        self.act(junk, xt, AF.Square, accum=ss)
        self.act(rstd, ss, AF.Sqrt, bias=self.epsT, scale=1.0 / D)
        self.recip(rstd, rstd)
        self.stt(out32, xt, rstd, A, ALU.mult, ALU.mult)
        if outb is None:
            self.tt(out_eng, out32, out32, B, ALU.add)
        else:
            self.tt(out_eng, outb, out32, B, ALU.add)

    def moe_prep(self, i, t, xn, W, ctxrow):
        A2, B2 = W["A2"], W["B2"]
        h2, junk, h2b, h2T = W["h2"], W["junk"], W["h2b"], W["h2T"]
        ss, rstd = W["ss2"], W["rstd2"]
        self.rms_mod(xn, junk, A2, B2, h2, ss, rstd)
        self.copy("act", h2b, h2)
        self.dma(T(self.H2d[t * 128:(t + 1) * 128, :], self.H2B[t]), h2b)
        pa, pb = self.PS[5], self.PS[6]
        for half, pp in ((0, pa), (1, pb)):
            self.trs([(pp[:, k * 128:(k + 1) * 128], h2[:, (half * 4 + k) * 128:(half * 4 + k + 1) * 128]) for k in range(4)],
                     self.identF)
            self.copy("act" if half == 0 else "dve", h2T[:, half * 4:(half + 1) * 4, :],
                      pp.re("p (k n) -> p k n", k=4))
        lg = self.PS[7][:, 0:NE]
        self.mm(lg, [(h2T[:, kc, :], W["WR"][:, kc, :]) for kc in range(8)])
        mx, sm, ex = W["mx"], W["sm"], W["ex"]
        o, a = mx.ap, lg.ap
        self.p.op("dve", lambda e, o=o, a=a: e.tensor_reduce(out=o, in_=a, axis=AX.X, op=ALU.max), _b(lg), _b(mx))
        self.ts("dve", mx, mx, -1.0, op0=ALU.mult)
        self.act(ex, lg, AF.Exp, bias=mx, scale=1.0, accum=sm)
        self.recip(sm, sm)
        self.ts("dve", self.affTok[:, t, :], ex, sm, op0=ALU.mult)

    def alloc_prep(self, i):
        W = {}
        W["h2"] = self.alloc([128, D], F32, "h2")
        W["junk"] = self.alloc([128, D], BF16, "junk")
        W["h2b"] = self.alloc([128, D], BF16, "h2b")
        W["h2T"] = self.alloc([128, 8, 128], F32, "h2T")
        W["WR"] = self.alloc([128, 8, NE], F32, "WR")
        for n in ("ss", "rstd", "ss2", "rstd2", "mx", "sm"):
            W[n] = self.alloc([128, 1], F32, n)
        W["ex"] = self.alloc([128, NE], F32, "ex")
        self.dma(W["WR"], T(self.I["moe_w_router"][i].rearrange("(kc p) e -> p kc e", p=128), Buf("wr")))
        return W

    def load_bcr(self, W, i, r, names):
        idx = {"B1": 0, "A1": 1, "G1": 2, "B2": 3, "A2": 4, "G2": 5}
        for n in names:
            k = idx[n]
            self.load_bc(W[n], self.BCRd[i, r, k * D:(k + 1) * D], self.BCRB[i])

    def xsrc(self, i, t):
        if i == 0:
            if t < NLT:
                return T(self.I["x"][t * 128:(t + 1) * 128, :], Buf("xin"))
            return T(self.I["ctx"][(t - NLT) * 128:(t - NLT + 1) * 128, :], Buf("cin"))
        return T(self.Xd[t * 128:(t + 1) * 128, :], self.XB[t])

    def phase_gmlp(self, i):
        p = self.p
        j = i // 2
        I = self.I
        p.barrier()
        self.arena_reset()
        stage = [self.alloc([128, 2, 1024], F32, "stg%d" % s) for s in range(2)]
        WIN = self.alloc([128, 8, 2 * D], BF16, "WIN")
        WOUT = self.alloc([128, 8, D], BF16, "WOUT")
        WST = self.alloc([128, 8, 128], BF16, "WST")
        bsB = self.alloc([128, 8, 128], F32, "bsB")
        W = self.alloc_prep(i)
        for n in ("A1", "B1", "G1", "A2", "B2", "LNG", "LNB"):
            W[n] = self.alloc([128, D], F32, n)
        xt = self.alloc([128, D], F32, "xt")
        h32 = self.alloc([128, D], F32, "h32")
        hb = self.alloc([128, D], BF16, "hb")
        hT = self.alloc([128, 8, 128], BF16, "hT")
        uT = self.alloc([128, 8, 128], F32, "uT")
        v = self.alloc([128, D], F32, "v")
        vn = self.alloc([128, D], F32, "vn")
        vlb = self.alloc([128, D], BF16, "vlb")
        t1 = self.alloc([128, 8, 128], F32, "t1")
        prodT = self.alloc([128, 8, 128], BF16, "prodT")
        xn = self.alloc([128, D], F32, "xn")
        bst = self.alloc([128, 2, 6], F32, "bst")
        mv = self.alloc([128, 2], F32, "mv")
        rs = self.alloc([128, 1], F32, "rs")
        wsl = self.alloc([128, 8, 128], F32, "wsl")

        self.load_cast(WIN, I["a_w_in"][j], stage)
        self.load_cast(WOUT, I["a_w_out"][j], stage)
        self.dma(wsl, T(I["a_w_s"][j].rearrange("g n m -> n g m"), Buf("ws")))
        for g in range(8):
            pp = self.PS[g % 2][:, 0:128]
            self.tr(pp, wsl[:, g, :], self.identF)
            self.copy("dve", WST[:, g, :], pp)
        self.load_bc(bsB.re("p g n -> p (g n)"), I["a_b_s"][j].rearrange("g n -> (g n)"))
        self.load_bc(W["LNG"], I["a_ln_g"][j])
        self.load_bc(W["LNB"], I["a_ln_b"][j])
        PS = self.PS
        ntiles = NT
        for t in range(ntiles):
            if t == 0:
                self.load_bcr(W, i, 0, ("A1", "B1", "G1", "A2", "B2"))
            if t == NLT:
                self.load_bcr(W, i, 1, ("A1", "B1", "G1", "A2", "B2"))
            self.dma(xt, self.xsrc(i, t))
            self.rms_mod(xt, W["junk"], W["A1"], W["B1"], h32, W["ss"], W["rstd"], outb=hb)
            pb = PS[0].bitcast(BF16)
            self.trs([(pb[:, k * 128:(k + 1) * 128], hb[:, k * 128:(k + 1) * 128]) for k in range(8)], self.identB)
            self.copy("act", hT, pb.re("p (k n) -> p k n", k=8))
            for half in range(2):
                pu = PS[1 + half]
                for q4 in range(4):
                    oc = half * 4 + q4
                    self.mm(pu[:, q4 * 128:(q4 + 1) * 128],
                            [(WIN[:, kc, oc * 128:(oc + 1) * 128], hT[:, kc, :]) for kc in range(8)])
                self.act(uT[:, half * 4:(half + 1) * 4, :], pu.re("p (k n) -> p k n", k=4), AF.Gelu_apprx_tanh)
            for half in range(2):
                pv = PS[3 + half]
                self.mm(pv, [(hT[:, kc, :], WIN[:, kc, D + half * 512:D + (half + 1) * 512]) for kc in range(8)])
                self.act(v[:, half * 512:(half + 1) * 512], pv, AF.Gelu_apprx_tanh)
            for half in range(2):
                o, a = bst[:, half, :].ap, v[:, half * 512:(half + 1) * 512].ap
                p.op("dve", lambda e, o=o, a=a: e.bn_stats(out=o, in_=a), _b(v), _b(bst))
            o, a = mv.ap, bst.re("p a b -> p (a b)").ap
            p.op("dve", lambda e, o=o, a=a: e.bn_aggr(out=o, in_=a), _b(bst), _b(mv))
            self.act(rs, mv[:, 1:2], AF.Sqrt, bias=self.epsT, scale=1.0)
            self.recip(rs, rs)
            self.ts("dve", vn, v, mv[:, 0:1], rs, op0=ALU.subtract, op1=ALU.mult)
            self.tt("pool", vn, vn, W["LNG"], ALU.mult)
            self.tt("pool", vlb, vn, W["LNB"], ALU.add)
            for half in range(2):
                psm = PS[5 + half]
                for q4 in range(4):
                    g = half * 4 + q4
                    self.mm(psm[:, q4 * 128:(q4 + 1) * 128], [(vlb[:, g * 128:(g + 1) * 128], WST[:, g, :])])
                self.tt("dve", t1[:, half * 4:(half + 1) * 4, :], psm.re("p (k n) -> p k n", k=4),
                        bsB[:, half * 4:(half + 1) * 4, :], ALU.add)
            self.tt("pool", prodT, t1, uT, ALU.mult)
            for half in range(2):
                py = PS[3 + half]
                self.mm(py, [(prodT[:, g, :], WOUT[:, g, half * 512:(half + 1) * 512]) for g in range(8)])
                self.tt("dve", xn[:, half * 512:(half + 1) * 512], py, W["G1"][:, half * 512:(half + 1) * 512], ALU.mult)
            self.tt("pool", xn, xn, xt, ALU.add)
            self.dma(T(self.Xd[t * 128:(t + 1) * 128, :], self.XB[t]), xn)
            self.moe_prep(i, t, xn, W, t >= NLT)

    def phase_ret(self, i):
        p = self.p
        j = i // 2
        I = self.I
        PS = self.PS
        last = i == DEPTH - 1
        RB = self.RB
        p.barrier()
        self.arena_reset()
        stage = [self.alloc([128, 2, 1024], F32, "stg%d" % s) for s in range(2)]
        WQ, WK, WV, WGF, WGB = [self.alloc([128, 8, D], BF16, "Wr%d" % m) for m in range(5)]
        for m, Wm in enumerate((WQ, WK, WV, WGF, WGB)):
            self.load_cast(Wm, I["r_w_in"][j][:, m * D:(m + 1) * D], stage)
        A1 = self.alloc([128, D], F32, "A1")
        B1 = self.alloc([128, D], F32, "B1")
        Wb = {"A1": A1, "B1": B1}
        xt = self.alloc([128, D], F32, "xt")
        junk = self.alloc([128, D], BF16, "junk")
        h32 = self.alloc([128, D], F32, "h32")
        hb = self.alloc([128, D], BF16, "hb")
        hT = self.alloc([128, 8, 128], BF16, "hT")
        ss = self.alloc([128, 1], F32, "ss")
        rstd = self.alloc([128, 1], F32, "rstd")
        cs = self.alloc([128, 2, 128], F32, "cs")
        cs16 = self.alloc([128, 2, 128], F32, "cs16")
        qr = self.alloc([128, 8, 128], BF16, "qr")
        kr = self.alloc([128, 8, 128], BF16, "kr")
        ta = [self.alloc([128, 2, 128], F32, "ta%d" % s) for s in range(4)]
        kk = self.alloc([128, D], BF16, "kk")
        vv = self.alloc([128, D], BF16, "vv")
        gf = self.alloc([128, D], BF16, "gf")
        gb = self.alloc([128, D], BF16, "gb")
        csd = I["ropecs"].rearrange("c p n -> p c n")
        for t in range(NT):
            is_ctx = t >= NLT
            if t == 0:
                self.load_bcr(Wb, i, 0, ("A1", "B1"))
            if t == NLT:
                self.load_bcr(Wb, i, 1, ("A1", "B1"))
            self.dma(xt, self.xsrc(i, t))
            self.rms_mod(xt, junk, A1, B1, h32, ss, rstd, outb=hb)
            pb = PS[0].bitcast(BF16)
            self.trs([(pb[:, k * 128:(k + 1) * 128], hb[:, k * 128:(k + 1) * 128]) for k in range(8)], self.identB)
            self.copy("act", hT, pb.re("p (k n) -> p k n", k=8))
            need_q = not (last and is_ctx)
            if not is_ctx:
                self.dma(cs, T(csd[:, :, t * 128:(t + 1) * 128], Buf("ropecs")))
                self.ts("pool", cs16, cs, 0.0625, op0=ALU.mult)
            for (Wm, dst, base, tab) in ((WQ, qr, 1, cs), (WK, kr, 3, cs16)):
                if Wm is WQ and not need_q:
                    continue
                for half in range(2):
                    bank = PS[base + half]
                    for q4 in range(4):
                        oc = half * 4 + q4
                        self.mm(bank[:, q4 * 128:(q4 + 1) * 128],
                                [(Wm[:, kc, oc * 128:(oc + 1) * 128], hT[:, kc, :]) for kc in range(8)])
                    dview = dst[:, half * 4:(half + 1) * 4, :]
                    if is_ctx:
                        if Wm is WQ:
                            self.copy("act", dview, bank.re("p (k n) -> p k n", k=4))
                        else:
                            self.ts("dve", dview, bank.re("p (k n) -> p k n", k=4), 0.0625, op0=ALU.mult)
                    else:
                        bv = bank.re("p (h c n) -> p h c n", h=2, c=2)
                        t1, t2 = bv[:, :, 0, :], bv[:, :, 1, :]
                        cosb = tab[:, 0, :].bc(1, [128, 2, 128])
                        sinb = tab[:, 1, :].bc(1, [128, 2, 128])
                        dv4 = dview.re("p (h c) n -> p h c n", c=2)
                        self.tt("dve", ta[0], t1, cosb, ALU.mult)
                        self.tt("dve", ta[1], t2, sinb, ALU.mult)
                        self.tt("pool", dv4[:, :, 0, :], ta[0], ta[1], ALU.subtract)
                        self.tt("dve", ta[2], t1, sinb, ALU.mult)
                        self.tt("dve", ta[3], t2, cosb, ALU.mult)
                        self.tt("pool", dv4[:, :, 1, :], ta[2], ta[3], ALU.add)
            for (Wm, dst, fn) in ((WV, vv, AF.Copy), (WGF, gf, AF.Silu), (WGB, gb, AF.Silu)):
                if last and is_ctx and Wm is not WV:
                    continue
                for half in range(2):
                    bank = PS[5 + half]
                    self.mm(bank, [(hT[:, kc, :], Wm[:, kc, half * 512:(half + 1) * 512]) for kc in range(8)])
                    self.act(dst[:, half * 512:(half + 1) * 512], bank, fn)
            pb7 = PS[7].bitcast(BF16)
            self.trs([(pb7[:, oc * 128:(oc + 1) * 128], kr[:, oc, :]) for oc in range(8)], self.identB)
            self.copy("dve", kk, pb7)
            if need_q:
                self.dma(T(self.QTd[t], RB["QT"][t]), qr)
            self.dma(T(self.KTd[t], RB["KT"][t]), kr)
            self.dma(T(self.KKd[t], RB["KK"][t]), kk)
            self.dma(T(self.VVd[t], RB["VV"][t]), vv)
            if not (last and is_ctx):
                self.dma(T(self.GFd[t], RB["GF"][t]), gf)
                self.dma(T(self.GBd[t], RB["GB"][t]), gb)

        p.barrier()
        self.arena_reset()
        dcy = self.alloc([128, 8], F32, "dcy")
        lg = self.alloc([128, 8], F32, "lg")
        nlg = self.alloc([128, 8], F32, "nlg")
        one = self.alloc([128, 1], F32, "one")
        maskT = self.alloc([128, 8, 128], F32, "maskT")
        qdec = self.alloc([128, 8, 128], F32, "qdec")
        kdec = self.alloc([128, 8], F32, "kdec")
        cd = self.alloc([128, 8], F32, "cd")
        diff = self.alloc([128, 128], F32, "diff")
        rowf = self.alloc([128, 128], F32, "rowf")
        rowb = self.alloc([128, 128], F32, "rowb")
        colf = self.alloc([128, 1], F32, "colf")
        colb = self.alloc([128, 1], F32, "colb")
        self.load_bc(dcy[:, 0:4], I["r_decay_f"][j])
        self.load_bc(dcy[:, 4:8], I["r_decay_b"][j])
        self.memset("dve", one, 1.0)
        self.act(nlg, dcy, AF.Exp, scale=-1.0)
        self.act(nlg, nlg, AF.Ln, bias=one, scale=1.0)
        self.ts("dve", lg, nlg, -1.0, op0=ALU.mult)

        def iota(tile, pattern, base, cm):
            o = tile.ap
            p.op("pool", lambda e, o=o: e.iota(o, pattern=pattern, base=base, channel_multiplier=cm,
                                               allow_small_or_imprecise_dtypes=True), (), _b(tile))
        iota(diff, [[1, 128]], 0, -1)
        iota(rowf, [[1, 128]], 1, 0)
        iota(rowb, [[-1, 128]], 128, 0)
        iota(colf, [[0, 1]], 127, -1)
        iota(colb, [[0, 1]], 0, 1)
        for h in range(4):
            mf, mb = maskT[:, h, :], maskT[:, 4 + h, :]
            self.act(mf, diff, AF.Exp, scale=lg[:, h:h + 1])
            o = mf.ap
            p.op("pool", lambda e, o=o: e.affine_select(out=o, in_=o, pattern=[[1, 128]], base=0, channel_multiplier=-1,
                                                        compare_op=ALU.is_ge, fill=0.0), _b(mf), _b(mf))
            self.act(mb, diff, AF.Exp, scale=nlg[:, 4 + h:5 + h])
            o = mb.ap
            p.op("pool", lambda e, o=o: e.affine_select(out=o, in_=o, pattern=[[-1, 128]], base=0, channel_multiplier=1,
                                                        compare_op=ALU.is_gt, fill=0.0), _b(mb), _b(mb))
            self.act(qdec[:, h, :], rowf, AF.Exp, scale=lg[:, h:h + 1])
            self.act(qdec[:, 4 + h, :], rowb, AF.Exp, scale=lg[:, 4 + h:5 + h])
            self.act(kdec[:, h:h + 1], colf, AF.Exp, scale=lg[:, h:h + 1])
            self.act(kdec[:, 4 + h:5 + h], colb, AF.Exp, scale=lg[:, 4 + h:5 + h])
        self.act(cd, lg, AF.Exp, scale=128.0)
        keep = self.aoff

        def scan_pass(d):
            p.barrier()
            self.arena_reset(keep)
            ring = [{n: self.alloc(([128, 8, 128] if n in ("QT", "KT") else [128, D]), BF16, "%s%d" % (n, s))
                     for n in ("QT", "KT", "KK", "VV")} for s in range(2)]
            Qd = self.alloc([128, 8, 128], BF16, "Qd")
            Kd = self.alloc([128, D], BF16, "Kd")
            attm = [self.alloc([128, 128], BF16, "attm%d" % h) for h in range(4)]
            S32 = [self.alloc([128, 2, 256], F32, "S32_%d" % h) for h in range(4)]
            Sbf = [self.alloc([128, 2, 256], BF16, "Sbf_%d" % h) for h in range(4)]
            HN = self.alloc([128, D], F32, "HN")
            bst = self.alloc([128, 6], F32, "bst")
            mv = self.alloc([128, 2], F32, "mv")
            rs = self.alloc([128, 1], F32, "rs")
            for h in range(4):
                self.memset("pool", S32[h], 0.0)
                self.memset("pool", Sbf[h], 0.0)
            if d == 1:
                stage = [self.alloc([128, 2, 1024], F32, "stg%d" % s) for s in range(2)]
                WO = self.alloc([128, 8, D], BF16, "WO")
                self.load_cast(WO, I["r_w_out"][j], stage)
                W = self.alloc_prep(i)
                for n in ("G1", "A2", "B2"):
                    W[n] = self.alloc([128, D], F32, n)
                HNf = self.alloc([128, D], F32, "HNf")
                GFc = self.alloc([128, D], BF16, "GFc")
                GBc = self.alloc([128, D], BF16, "GBc")
                yb = self.alloc([128, D], BF16, "yb")
                yT = self.alloc([128, 8, 128], BF16, "yT")
                xt = self.alloc([128, D], F32, "xt")
                xn = self.alloc([128, D], F32, "xn")
            order = [NLT, NLT + 1] + list(range(NLT)) if d == 0 else [NLT + 1, NLT] + list(range(NLT - 1, -1, -1))
            for step, c in enumerate(order):
                is_ctx = c >= NLT
                want_out = not (last and is_ctx)
                R_ = ring[step % 2]
                if want_out:
                    self.dma(R_["QT"], T(self.QTd[c], RB["QT"][c]))
                    self.dma(R_["KT"], T(self.KTd[c], RB["KT"][c]))
                self.dma(R_["KK"], T(self.KKd[c], RB["KK"][c]))
                self.dma(R_["VV"], T(self.VVd[c], RB["VV"][c]))
                QTc, KTc, KKc, VVc = R_["QT"], R_["KT"], R_["KK"], R_["VV"]
                if want_out:
                    self.tt("pool", Qd.re("p (h c) n -> p h c n", c=2), QTc.re("p (h c) n -> p h c n", c=2),
                            qdec[:, d * 4:(d + 1) * 4, :].bc(2, [128, 4, 2, 128]), ALU.mult)
                self.tt("pool", Kd.re("p (h k) -> p h k", h=4), KKc.re("p (h k) -> p h k", h=4),
                        kdec[:, d * 4:(d + 1) * 4].bc(2, [128, 4, 256]), ALU.mult)
                for h in range(4):
                    vh = VVc[:, h * 256:(h + 1) * 256]
                    if want_out:
                        att = PS[0][:, h * 128:(h + 1) * 128]
                        self.mm(att, [(KTc[:, 2 * h + jj, :], QTc[:, 2 * h + jj, :]) for jj in range(2)])
                        self.tt("dve", attm[h], att, maskT[:, d * 4 + h, :], ALU.mult)
                        pso = PS[1 + h // 2][:, (h % 2) * 256:(h % 2 + 1) * 256]
                        self.mm(pso, [(attm[h], vh), (Qd[:, 2 * h, :], Sbf[h][:, 0, :]), (Qd[:, 2 * h + 1, :], Sbf[h][:, 1, :])])
                    pss = PS[3 + h]
                    for jj in range(2):
                        self.mm(pss[:, jj * 256:(jj + 1) * 256], [(Kd[:, h * 256 + jj * 128:h * 256 + (jj + 1) * 128], vh)])
                    s32 = S32[h].re("p a b -> p (a b)")
                    self.stt(s32, s32, cd[:, d * 4 + h:d * 4 + h + 1], pss, ALU.mult, ALU.add)
                    self.copy("act", Sbf[h].re("p a b -> p (a b)"), s32)
                    if want_out:
                        o_, a_ = bst.ap, pso.ap
                        p.op("dve", lambda e, o_=o_, a_=a_: e.bn_stats(out=o_, in_=a_), _b(pso), _b(bst))
                        o_, a_ = mv.ap, bst.ap
                        p.op("dve", lambda e, o_=o_, a_=a_: e.bn_aggr(out=o_, in_=a_), _b(bst), _b(mv))
                        self.act(rs, mv[:, 1:2], AF.Sqrt, bias=self.epsT, scale=1.0)
                        self.recip(rs, rs)
                        self.ts("dve", HN[:, h * 256:(h + 1) * 256], pso, mv[:, 0:1], rs, op0=ALU.subtract, op1=ALU.mult)
                if not want_out:
                    continue
                if d == 0:
                    self.dma(T(self.HNd[c], RB["HN"][c]), HN)
                    continue
                if step == 0 and not last:
                    self.load_bcr(W, i, 1, ("G1", "A2", "B2"))
                if c == NLT - 1:
                    self.load_bcr(W, i, 0, ("G1", "A2", "B2"))
                self.dma(HNf, T(self.HNd[c], RB["HN"][c]))
                self.dma(GFc, T(self.GFd[c], RB["GF"][c]))
                self.dma(GBc, T(self.GBd[c], RB["GB"][c]))
                self.dma(xt, self.xsrc(i, c))
                self.tt("pool", HNf, HNf, GFc, ALU.mult)
                self.tt("pool", HN, HN, GBc, ALU.mult)
                self.tt("pool", yb, HNf, HN, ALU.add)
                pb7 = PS[7].bitcast(BF16)
                self.trs([(pb7[:, k * 128:(k + 1) * 128], yb[:, k * 128:(k + 1) * 128]) for k in range(8)], self.identB)
                self.copy("act", yT, pb7.re("p (k n) -> p k n", k=8))
                for half in range(2):
                    py = PS[1 + half]
                    self.mm(py, [(yT[:, kc, :], WO[:, kc, half * 512:(half + 1) * 512]) for kc in range(8)])
                    self.tt("dve", xn[:, half * 512:(half + 1) * 512], py, W["G1"][:, half * 512:(half + 1) * 512], ALU.mult)
                self.tt("pool", xn, xn, xt, ALU.add)
                self.dma(T(self.Xd[c * 128:(c + 1) * 128, :], self.XB[c]), xn)
                self.moe_prep(i, c, xn, W, is_ctx)

        scan_pass(0)
        scan_pass(1)

    def phase_routing(self, i):
        p = self.p
        p.barrier()
        self.arena_reset()
        last = i == DEPTH - 1
        PS = self.PS
        NP = 64
        affT = self.alloc([NP, NL], F32, "affT")
        msk = self.alloc([NP, NL], F32, "msk")
        cum = self.alloc([NP, NL], F32, "cum")
        junk = self.alloc([NP, NL], BF16, "rjunk")
        posT = self.alloc([128, NT, NP], F32, "posT")
        rhs5 = self.alloc([128, NT, NE, 5], BF16, "rhs5")
        affC = self.alloc([128, 2, 48], F32, "affC")
        sm = {n: self.alloc([NP, 1], F32, n) for n in ("lo", "hi", "mid", "cnt", "ge", "d", "cap", "zero")}
        r1 = self.alloc([128, NT, NE], F32, "r1")
        pc = self.alloc([128, NT, NE], F32, "pc")
        ohs = [self.alloc([128, 512], BF16, "oh%d" % s) for s in range(4)]
        ohc = [self.alloc([128, 32], BF16, "ohc%d" % s) for s in range(2)]
        res = self.alloc([128, 5, 5], F32, "res")
        idf = self.alloc([128, 5], F32, "idf")
        self.memset("dve", res, 0.0)

        if self.debug and "AFF" in self.debug:
            self.dma(T(self.AFFd, Buf("affd")), self.affTok.re("p t e -> p (t e)"))
        self.memset("pool", affT, -1.0)
        for g in range(8):
            pp = PS[g % 2]
            self.trs([(pp[0:NE, k * 128:(k + 1) * 128], self.affTok[:, g * 4 + k, :]) for k in range(4)], self.identF)
            self.copy("dve" if g % 2 == 0 else "act", affT[0:NE, g * 512:(g + 1) * 512], pp[0:NE, :])
        if not last:
            self.memset("dve", affC, 0.0)
            self.copy("dve", affC[:, :, 32:48], self.affTok[:, NLT:NT, :])
            pp = PS[2]
            self.trs([(pp[0:48, k * 128:(k + 1) * 128], affC[:, k, :]) for k in range(2)], self.identF)
            self.copy("dve", affT[32:48, 0:256], pp[32:48, 0:256])
        self.memset("dve", sm["cap"][0:32, :], float(CAP))
        self.memset("dve", sm["cap"][32:64, :], float(CAPC))
        self.memset("dve", sm["lo"], 0.0)
        self.memset("dve", sm["hi"], 1.0)
        lo, hi, mid, cnt, ge, d, cap = (sm[n] for n in ("lo", "hi", "mid", "cnt", "ge", "d", "cap"))
        for it in range(30):
            self.tt("dve", mid, lo, hi, ALU.add)
            self.ts("dve", mid, mid, 0.5, op0=ALU.mult)
            self.ts("dve", junk, affT, mid, 0.0, op0=ALU.is_ge, op1=ALU.add, accum=cnt)
            self.tt("dve", ge, cnt, cap, ALU.is_ge)
            self.tt("dve", d, mid, lo, ALU.subtract)
            self.stt(lo, d, ge, lo, ALU.mult, ALU.add)
            self.tt("dve", d, hi, mid, ALU.subtract)
            self.stt(hi, d, ge, mid, ALU.mult, ALU.add)
        self.ts("dve", msk, affT, lo, op0=ALU.is_ge)
        o, a = cum.ap, msk.ap
        self.memset("dve", sm["zero"], 0.0)
        z = sm["zero"].ap
        p.op("dve", lambda e, o=o, a=a, z=z: e.tensor_tensor_scan(out=o, data0=a, data1=a, initial=z, op0=ALU.add, op1=ALU.max),
             _b(msk, sm["zero"]), _b(cum))
        self.tt("dve", cum, cum, msk, ALU.mult)
        self.ts("dve", cum, cum, -1.0, op0=ALU.add)
        for g in range(4):
            pp = PS[3 + g % 2]
            self.trs([(pp[:, k * NP:(k + 1) * NP], cum[:, (g * 8 + k) * 128:(g * 8 + k + 1) * 128]) for k in range(8)],
                     self.identF)
            self.copy("act" if g % 2 else "dve", posT[:, g * 8:(g + 1) * 8, :], pp.re("p (k n) -> p k n", k=8))
        if not last:
            pp = PS[5]
            self.trs([(pp[:, k * NP:(k + 1) * NP], cum[:, k * 128:(k + 1) * 128]) for k in range(2)], self.identF)
            self.copy("dve", posT[:, NLT:NT, :], pp[:, 0:2 * NP].re("p (k n) -> p k n", k=2))
        o = pc.ap
        p.op("pool", lambda e, o=o: e.iota(o, pattern=[[0, NT * NE]], base=0, channel_multiplier=1,
                                      allow_small_or_imprecise_dtypes=True), (), _b(pc))
        self.copy("pool", rhs5[:, :, :, 0], pc)
        p.op("pool", lambda e, o=o: e.iota(o, pattern=[[1, NT], [0, NE]], base=0, channel_multiplier=0,
                                      allow_small_or_imprecise_dtypes=True), _b(rhs5), _b(pc))
        self.copy("pool", rhs5[:, :, :, 1], pc)
        self.copy("dve", rhs5[:, :, :, 2], self.affTok)
        self.tt("dve", r1, self.affTok, rhs5[:, :, :, 2], ALU.subtract)
        self.copy("dve", rhs5[:, :, :, 3], r1)
        self.tt("dve", r1, r1, rhs5[:, :, :, 3], ALU.subtract)
        self.copy("dve", rhs5[:, :, :, 4], r1)
        k = 0
        for e in range(NE):
            banks = [PS[(e % 2) * 4 + c] for c in range(4)]
            for t in range(NLT):
                oh = ohs[k % 4]
                k += 1
                self.ts("dve", oh, self.iota512, posT[:, t, e:e + 1], op0=ALU.is_equal)
                for c in range(4):
                    o_, l_, r_ = banks[c][:, 0:5].ap, oh[:, c * 128:(c + 1) * 128].ap, rhs5[:, t, e, :].ap
                    st, sp_ = (t == 0), (t == NLT - 1)
                    p.op("pe", lambda en, o_=o_, l_=l_, r_=r_, st=st, sp_=sp_: en.matmul(o_, l_, r_, start=st, stop=sp_),
                         _b(oh, rhs5), _b(banks[c]))
            for c in range(4):
                self.copy("act", res[:, c, :], banks[c][:, 0:5])
            if not last:
                for tt_ in range(2):
                    oc_ = ohc[tt_]
                    self.ts("dve", oc_, self.iota512[:, 0:32], posT[:, NLT + tt_, 32 + e:33 + e], op0=ALU.is_equal)
                bk = banks[0]
                self.mm(bk[0:32, 8:13], [(ohc[tt_], rhs5[:, NLT + tt_, e, :]) for tt_ in range(2)])
                self.copy("act", res[0:32, 4, :], bk[0:32, 8:13])
            nch = 4 if last else 5
            self.stt(idf[:, 0:nch], res[:, 0:nch, 1], 128.0, res[:, 0:nch, 0], ALU.mult, ALU.add)
            self.copy("dve", self.idxAll[:, e, 0:nch], idf[:, 0:nch])
            self.tt("dve", idf[:, 0:nch], res[:, 0:nch, 2], res[:, 0:nch, 3], ALU.add)
            self.tt("dve", self.gateAll[:, e, 0:nch], idf[:, 0:nch], res[:, 0:nch, 4], ALU.add)
        if self.debug and "AFF" in self.debug:
            self.dma(T(self.IDXd, Buf("idxd")), self.idxAll.re("p e c -> p (e c)"))
            self.dma(T(self.GATd, Buf("gatd")), self.gateAll.re("p e c -> p (e c)"))

    def phase_experts(self, i):
        p = self.p
        p.barrier()
        self.arena_reset()
        last = i == DEPTH - 1
        I = self.I
        PS = self.PS
        stage = [self.alloc([128, 2, 1024], F32, "stg%d" % s) for s in range(2)]
        WS = [[self.alloc([128, 8, D], BF16, "W%d_%d" % (s, m)) for m in range(3)] for s in range(2)]
        NS = 512 if last else 544
        nch = 4 if last else 5
        xs = [self.alloc([128, 5, D], BF16, "xs%d" % s) for s in range(2)]
        xsT = self.alloc([128, 8, 544], BF16, "xsT")
        hidT = self.alloc([128, 8, 544], BF16, "hidT")
        sg = self.alloc([128, 544], F32, "sg")
        ye = [self.alloc([128, D], F32, "ye%d" % s) for s in range(2)]
        G2 = self.alloc([128, D], F32, "G2")
        G2c = self.alloc([128, D], F32, "G2c")
        self.load_bcr({"G2": G2}, i, 0, ("G2",))
        if not last:
            self.load_bcr({"G2": G2c}, i, 1, ("G2",))
        XSC = Buf("xscatter")
        halves = [(0, NS // 2), (NS // 2, NS)]
        nye = 0
        allX = self.XB

        def gather(e):
            x = xs[e % 2]
            for c in range(nch):
                rows = 128 if c < 4 else 32
                o_ = x[0:rows, c, :].ap
                ix = self.idxAll[0:rows, e, c:c + 1].ap
                src = self.H2d
                p.dma("pool", lambda en, o_=o_, ix=ix, src=src: en.indirect_dma_start(
                    out=o_, out_offset=None, in_=src, in_offset=bass.IndirectOffsetOnAxis(ap=ix, axis=0)),
                    _b(self.idxAll) + self.H2B, _b(x))

        def wpieces(e):
            s = e % 2
            return (self.cast_pieces(WS[s][0], I["moe_w_gate"][i, e], stage)
                    + self.cast_pieces(WS[s][1], I["moe_w_up"][i, e], stage)
                    + self.cast_pieces(WS[s][2], I["moe_w_down"][i, e], stage))

        gather(0)
        for f in wpieces(0):
            f()
        for e in range(NE):
            pend = []
            if e + 1 < NE:
                gather(e + 1)
                pend = wpieces(e + 1)
            x = xs[e % 2]
            WG, WU, WD = WS[e % 2]
            for c in range(nch):
                rows = 128 if c < 4 else 32
                pb = PS[c % 2].bitcast(BF16)
                self.trs([(pb[:, k * 128:k * 128 + rows], x[0:rows, c, k * 128:(k + 1) * 128]) for k in range(8)],
                         self.identB)
                self.copy("act" if c % 2 else "dve", xsT[:, :, c * 128:c * 128 + rows],
                          pb.re("p (k n) -> p k n", k=8)[:, :, 0:rows])
            for fc in range(8):
                if pend:
                    pend.pop(0)()
                for hi_, (a0, a1) in enumerate(halves):
                    pg, pu = PS[2 + hi_], PS[4 + hi_]
                    n = a1 - a0
                    self.mm(pg[:, 0:n], [(WG[:, kc, fc * 128:(fc + 1) * 128], xsT[:, kc, a0:a1]) for kc in range(8)])
                    self.mm(pu[:, 0:n], [(WU[:, kc, fc * 128:(fc + 1) * 128], xsT[:, kc, a0:a1]) for kc in range(8)])
                    self.act(sg[:, a0:a1], pg[:, 0:n], AF.Silu)
                    self.tt("dve", hidT[:, fc, a0:a1], pu[:, 0:n], sg[:, a0:a1], ALU.mult)
            for c in range(nch):
                if pend:
                    pend.pop(0)()
                rows = 128 if c < 4 else 32
                y = ye[nye % 2]
                nye += 1
                g2 = G2 if c < 4 else G2c
                for half in range(2):
                    py = PS[6 + half]
                    self.mm(py[0:rows, :], [(hidT[:, fc, c * 128:c * 128 + rows], WD[:, fc, half * 512:(half + 1) * 512])
                                            for fc in range(8)])
                    self.stt(y[0:rows, half * 512:(half + 1) * 512], py[0:rows, :], self.gateAll[0:rows, e, c:c + 1],
                             g2[0:rows, half * 512:(half + 1) * 512], ALU.mult, ALU.mult)
                o_ = self.Xd
                ix = self.idxAll[0:rows, e, c:c + 1].ap
                i_ = y[0:rows, :].ap
                p.dma("pool", lambda en, ix=ix, i_=i_, o_=o_: en.indirect_dma_start(
                    out=o_, out_offset=bass.IndirectOffsetOnAxis(ap=ix, axis=0), in_=i_, in_offset=None,
                    compute_op=ALU.add), _b(self.idxAll, y), [XSC] + allX)
            for f in pend:
                f()

    def phase_final(self):
        p = self.p
        p.barrier()
        self.arena_reset()
        FG = self.alloc([128, D], F32, "FG")
        self.load_bc(FG, self.I["final_norm_g"])
        xts = [self.alloc([128, D], F32, "fx%d" % s) for s in range(2)]
        outs = [self.alloc([128, D], F32, "fo%d" % s) for s in range(2)]
        junk = self.alloc([128, D], BF16, "fjunk")
        ss = self.alloc([128, 1], F32, "fss")
        rstd = self.alloc([128, 1], F32, "frstd")
        for t in range(NLT):
            xt, o = xts[t % 2], outs[t % 2]
            self.dma(xt, T(self.Xd[t * 128:(t + 1) * 128, :], self.XB[t]))
            self.act(junk, xt, AF.Square, accum=ss)
            self.act(rstd, ss, AF.Sqrt, bias=self.epsT, scale=1.0 / D)
            self.recip(rstd, rstd)
            self.stt(o, xt, rstd, FG, ALU.mult, ALU.mult)
            self.dma(T(self.out[t * 128:(t + 1) * 128, :], self.outB[t]), o)


def rope_tables():
    n = np.arange(NL)
    pos_r = (n // 64).astype(np.float32)
    pos_c = (n % 64).astype(np.float32)
    inv = np.power(np.float32(10000.0), -np.arange(64, dtype=np.float32) / np.float32(64)).astype(np.float32)
    ang = np.concatenate([pos_r[:, None] * inv[None], pos_c[:, None] * inv[None]], axis=-1)
    cs = np.stack([np.cos(ang).T, np.sin(ang).T]).astype(np.float32)
    return np.ascontiguousarray(cs)


_CACHE = {}


def make_in_maps(inputs, cores):
    f = lambda a: np.ascontiguousarray(np.asarray(a, dtype=np.float32))
    shared = {k: f(inputs[k]) for k in ("ada_w", "ada_b", "norm_mix_g", "norm_ffn_g", "a_w_in", "a_ln_g", "a_ln_b",
                                        "a_w_s", "a_b_s", "a_w_out", "r_w_in", "r_decay_f", "r_decay_b", "r_w_out",
                                        "moe_w_router", "moe_w_gate", "moe_w_up", "moe_w_down", "final_norm_g")}
    shared["ropecs"] = rope_tables()
    x, c, ctx, c_ctx = f(inputs["x"]), f(inputs["c"]), f(inputs["ctx"]), f(inputs["c_ctx"])
    maps = []
    for b in cores:
        m = dict(shared)
        m["x"] = x[b]
        m["ctx"] = ctx[b]
        m["cvec"] = np.ascontiguousarray(np.stack([c[b], c_ctx]))
        maps.append(m)
    return maps


def kernel(**inputs):
    if "nc" not in _CACHE:
        _CACHE["nc"] = K().build()
    nc = _CACHE["nc"]
    maps = make_in_maps(inputs, list(range(8)))
    res = run_bass_kernel_spmd(nc, maps, core_ids=list(range(8)))
    out = np.stack([np.asarray(r["out"], dtype=np.float32) for r in res.results], axis=0)
    return out
```

```python
import numpy as np
import threading
from contextlib import ExitStack
import concourse.bass as bass
import concourse.mybir as mybir
from concourse.bass_utils import run_bass_kernel_spmd

F32 = mybir.dt.float32
BF16 = mybir.dt.bfloat16
F16 = mybir.dt.float16
I32 = mybir.dt.int32
AF = mybir.ActivationFunctionType
ALU = mybir.AluOpType
AX = mybir.AxisListType

D = 1024
NL = 4096
NCX = 256
NT = 34
NLT = 32
DEPTH = 4
NE = 16
CAP = 512
CAPC = 32
EPS = 1e-6


class Buf:
    __slots__ = ("name", "w", "r")

    def __init__(self, name=""):
        self.name = name
        self.w = None
        self.r = {}


class T:
    __slots__ = ("ap", "bufs")

    def __init__(self, ap, bufs):
        self.ap = ap
        self.bufs = bufs if isinstance(bufs, (list, tuple)) else [bufs]

    def __getitem__(self, k):
        return T(self.ap[k], self.bufs)

    def re(self, s, **kw):
        return T(self.ap.rearrange(s, **kw), self.bufs)

    def bitcast(self, dt):
        return T(self.ap.bitcast(dt), self.bufs)

    def bc(self, axis, shape):
        return T(self.ap.unsqueeze(axis).to_broadcast(list(shape)), self.bufs)

    @property
    def shape(self):
        return self.ap.shape


class Prog:
    ENGS = ("pe", "dve", "act", "pool", "sp")
    NDMA = {"sp": 24, "pool": 12, "act": 8}

    def __init__(self, nc, same_engine_sync=True):
        self.nc = nc
        self.items = {e: [] for e in self.ENGS}
        self.cnt = {e: 0 for e in self.ENGS}
        self.dma_idx = {q: 0 for q in self.NDMA}
        self.seen = {e: {} for e in self.ENGS}
        self.same_engine_sync = same_engine_sync
        self.sems = {}
        self.last_dma = {}
        self.hook = None

    def _need(self, eng, dep):
        if dep is None:
            return
        key, val = dep
        if key == eng and (eng == "pe" or not self.same_engine_sync):
            return
        if self.seen[eng].get(key, 0) >= val:
            return
        self.seen[eng][key] = val
        self.items[eng].append(("wait", key, val))

    def _deps(self, eng, reads, writes):
        for b in reads:
            self._need(eng, b.w)
        for b in writes:
            self._need(eng, b.w)
            for d in b.r.items():
                self._need(eng, d)

    def _mark(self, me, reads, writes):
        for b in reads:
            if b.r.get(me[0], 0) < me[1]:
                b.r[me[0]] = me[1]
        for b in writes:
            b.w = me
            b.r = {}

    def op(self, eng, fn, reads=(), writes=()):
        self._deps(eng, reads, writes)
        self.cnt[eng] += 1
        me = (eng, self.cnt[eng])
        self.items[eng].append(("op", fn, eng))
        self._mark(me, reads, writes)
        if self.hook is not None:
            self.hook()
        return me

    def dma(self, q, fn, reads=(), writes=()):
        R = self.NDMA[q]
        i = self.dma_idx[q]
        self.dma_idx[q] += 1
        key = ("dma", q, i % R)
        if i >= R:
            self._need(q, (key, 16 * (i // R)))
        self._deps(q, reads, writes)
        me = (key, 16 * (i // R + 1))
        self.items[q].append(("dma", fn, key))
        self._mark(me, reads, writes)
        self.last_dma[key] = me
        if self.hook is not None:
            self.hook()
        return me

    def barrier(self):
        for e in self.ENGS:
            for e2 in ("pe", "dve", "act", "pool"):
                if self.cnt[e2] > 0:
                    self._need(e, (e2, self.cnt[e2]))
            for key, me in self.last_dma.items():
                self._need(e, me)

    def emit(self):
        nc = self.nc
        with ExitStack() as es:
            for e in ("pe", "dve", "act", "pool"):
                self.sems[e] = es.enter_context(nc.semaphore("s_" + e))
            for q, R in self.NDMA.items():
                for j in range(R):
                    self.sems[("dma", q, j)] = es.enter_context(nc.semaphore("d_%s_%d" % (q, j)))
            block = es.enter_context(nc.Block())
            sems = self.sems

            def run(engobj, items):
                for it in items:
                    if it[0] == "wait":
                        engobj.wait_ge(sems[it[1]], it[2])
                    elif it[0] == "op":
                        it[1](engobj).then_inc(sems[it[2]], 1)
                    else:
                        it[1](engobj).then_inc(sems[it[2]], 16)

            @block.tensor
            def _(e):
                run(e, self.items["pe"])

            @block.vector
            def _(e):
                run(e, self.items["dve"])

            @block.scalar
            def _(e):
                run(e, self.items["act"])

            @block.gpsimd
            def _(e):
                run(e, self.items["pool"])

            @block.sync
            def _(e):
                run(e, self.items["sp"])


def _b(*ts):
    out = []
    for t in ts:
        if isinstance(t, T):
            out.extend(t.bufs)
    return out


def _a(x):
    return x.ap if isinstance(x, T) else x


class K:
    def __init__(self, nlayers=DEPTH, debug=False):
        self.nlayers = nlayers
        self.debug = debug
        self.nc = bass.Bass("TRN2", target_bir_lowering=False)
        self.p = Prog(self.nc)
        self.es = ExitStack()
        self.dq = 0

    def act(self, out, in_, func, bias=None, scale=None, accum=None):
        kw = {}
        if bias is not None:
            kw["bias"] = _a(bias)
        if scale is not None:
            kw["scale"] = _a(scale)
        if accum is not None:
            kw["accum_out"] = _a(accum)
        o, i = out.ap, in_.ap
        self.p.op("act", lambda e: e.activation(out=o, in_=i, func=func, **kw),
                  _b(in_, bias, scale), _b(out, accum))

    def ts(self, eng, out, in0, s1, s2=None, op0=ALU.mult, op1=None, accum=None):
        kw = {}
        if op1 is not None:
            kw["op1"] = op1
        if accum is not None:
            kw["accum_out"] = _a(accum)
        o, i, a1, a2 = out.ap, in0.ap, _a(s1), _a(s2)
        ename = eng
        self.p.op(ename, lambda e: e.tensor_scalar(out=o, in0=i, scalar1=a1, scalar2=a2, op0=op0, **kw),
                  _b(in0, s1, s2), _b(out, accum))

    def tt(self, eng, out, in0, in1, op):
        o, a, b = out.ap, in0.ap, in1.ap
        self.p.op(eng, lambda e: e.tensor_tensor(out=o, in0=a, in1=b, op=op), _b(in0, in1), _b(out))

    def stt(self, out, in0, scalar, in1, op0, op1):
        o, a, s, b = out.ap, in0.ap, _a(scalar), in1.ap
        self.p.op("dve", lambda e: e.scalar_tensor_tensor(out=o, in0=a, scalar=s, in1=b, op0=op0, op1=op1),
                  _b(in0, scalar, in1), _b(out))

    def copy(self, eng, out, in_):
        o, i = out.ap, in_.ap
        if eng == "act":
            self.p.op("act", lambda e: e.activation(out=o, in_=i, func=AF.Copy), _b(in_), _b(out))
        else:
            self.p.op(eng, lambda e: e.tensor_copy(out=o, in_=i), _b(in_), _b(out))

    def memset(self, eng, out, val):
        o = out.ap
        self.p.op(eng, lambda e: e.memset(o, val), (), _b(out))

    def recip(self, out, in_):
        o, i = out.ap, in_.ap
        self.p.op("dve", lambda e: e.reciprocal(out=o, in_=i), _b(in_), _b(out))

    def mm(self, out, pairs, extra_reads=()):
        o = out.ap
        ps = [(l.ap, r.ap) for l, r in pairs]
        n = len(ps)

        def fn(e):
            ins = None
            for i, (l, r) in enumerate(ps):
                ins = e.matmul(o, l, r, start=(i == 0), stop=(i == n - 1))
            return ins
        rd = []
        for l, r in pairs:
            rd += _b(l, r)
        self.p.op("pe", fn, rd + list(extra_reads), _b(out))

    def tr(self, out, in_, ident):
        o, i = out.ap, in_.ap
        P = i.shape[0]
        d = ident.ap[0:P, 0:P]
        self.p.op("pe", lambda e: e.transpose(out=o, in_=i, identity=d), _b(in_, ident), _b(out))

    def trs(self, items, ident):
        lst = [(o.ap, i.ap) for o, i in items]
        d = ident.ap

        def fn(e):
            ins = None
            for o, i in lst:
                P = i.shape[0]
                ins = e.transpose(out=o, in_=i, identity=d[0:P, 0:P])
            return ins
        rd, wr = _b(ident), []
        for o, i in items:
            rd += _b(i)
            wr += _b(o)
        self.p.op("pe", fn, rd, wr)

    def dma(self, out, in_, q=None, **kw):
        if q is None:
            q = "sp"
        o, i = out.ap, in_.ap
        self.p.dma(q, lambda e: e.dma_start(out=o, in_=i, **kw), _b(in_), _b(out))

    def arena_reset(self, off=None):
        self.aoff = self.persist_end if off is None else off

    def alloc(self, shape, dt, name=""):
        esz = {F32: 4, BF16: 2, F16: 2, I32: 4}[dt]
        n = int(np.prod(shape[1:])) * esz
        n4 = (n + 3) // 4
        off = self.aoff
        self.aoff += n4 + (-n4) % 8
        assert self.aoff <= self.arena_n, ("SBUF arena overflow", name, self.aoff * 4)
        ap = self.arena[0:shape[0], off:off + n4]
        if dt != F32:
            ap = ap.bitcast(dt)
        ap = ap[:, 0:int(np.prod(shape[1:]))]
        if len(shape) > 2:
            names = " ".join("d%d" % i for i in range(len(shape) - 1))
            ap = ap.rearrange("p (%s) -> p %s" % (names, names), **{"d%d" % i: shape[i + 1] for i in range(len(shape) - 1)})
        return T(ap, Buf(name))

    def build(self):
        nc, es = self.nc, self.es
        dbg = self.debug

        def din(name, shape, dt=F32):
            return nc.dram_tensor(name, list(shape), dt, kind="ExternalInput").ap()

        def dscr(name, shape, dt=F32):
            kind = "ExternalOutput" if (dbg and name in dbg) else "Internal"
            return nc.dram_tensor(name, list(shape), dt, kind=kind).ap()

        I = {}
        I["x"] = din("x", [NL, D])
        I["ctx"] = din("ctx", [NCX, D])
        I["cvec"] = din("cvec", [2, D])
        I["ada_w"] = din("ada_w", [DEPTH, D, 6 * D])
        I["ada_b"] = din("ada_b", [DEPTH, 6 * D])
        I["norm_mix_g"] = din("norm_mix_g", [DEPTH, D])
        I["norm_ffn_g"] = din("norm_ffn_g", [DEPTH, D])
        I["a_w_in"] = din("a_w_in", [2, D, 2 * D])
        I["a_ln_g"] = din("a_ln_g", [2, D])
        I["a_ln_b"] = din("a_ln_b", [2, D])
        I["a_w_s"] = din("a_w_s", [2, 8, 128, 128])
        I["a_b_s"] = din("a_b_s", [2, 8, 128])
        I["a_w_out"] = din("a_w_out", [2, D, D])
        I["r_w_in"] = din("r_w_in", [2, D, 5 * D])
        I["r_decay_f"] = din("r_decay_f", [2, 4])
        I["r_decay_b"] = din("r_decay_b", [2, 4])
        I["r_w_out"] = din("r_w_out", [2, D, D])
        I["moe_w_router"] = din("moe_w_router", [DEPTH, D, NE])
        I["moe_w_gate"] = din("moe_w_gate", [DEPTH, NE, D, D])
        I["moe_w_up"] = din("moe_w_up", [DEPTH, NE, D, D])
        I["moe_w_down"] = din("moe_w_down", [DEPTH, NE, D, D])
        I["final_norm_g"] = din("final_norm_g", [D])
        I["ropecs"] = din("ropecs", [2, 128, NL])
        self.I = I
        self.out = nc.dram_tensor("out", [NL, D], F32, kind="ExternalOutput").ap()
        self.outB = [Buf("out%d" % t) for t in range(NLT)]

        self.Xd = dscr("X", [NT * 128, D])
        self.XB = [Buf("X%d" % t) for t in range(NT)]
        self.H2d = dscr("H2", [NT * 128, D], BF16)
        self.H2B = [Buf("H2_%d" % t) for t in range(NT)]
        self.BCRd = dscr("BCR", [DEPTH, 2, 6 * D])
        self.BCRB = [Buf("BCR%d" % i) for i in range(DEPTH)]
        self.QTd = dscr("QT", [NT, 128, 8, 128], BF16)
        self.KTd = dscr("KT", [NT, 128, 8, 128], BF16)
        self.KKd = dscr("KK", [NT, 128, D], BF16)
        self.VVd = dscr("VV", [NT, 128, D], BF16)
        self.GFd = dscr("GF", [NT, 128, D], BF16)
        self.GBd = dscr("GB", [NT, 128, D], BF16)
        self.HNd = dscr("HN", [NT, 128, D])
        self.YYd = dscr("YY", [NT, 128, D], BF16)
        self.RB = {n: [Buf("%s%d" % (n, t)) for t in range(NT)] for n in ("QT", "KT", "KK", "VV", "GF", "GB", "HN", "YY")}
        if dbg and "AFF" in dbg:
            self.AFFd = dscr("AFF", [128, NT * NE])
            self.IDXd = dscr("IDX", [128, NE * 5], I32)
            self.GATd = dscr("GAT", [128, NE * 5])

        self.arena_n = 49 * 1024
        self.arena = es.enter_context(nc.sbuf_tensor("arena", [128, self.arena_n], F32))
        self.aoff = 0
        self.persist_end = 0
        self.PS = []
        for b in range(8):
            t = es.enter_context(nc.psum_tensor("ps%d" % b, [128, 512], F32))
            self.PS.append(T(t[:, :], Buf("ps%d" % b)))

        self.identF = self.alloc([128, 128], F32, "identF")
        self.identB = self.alloc([128, 128], BF16, "identB")
        self.iota512 = self.alloc([128, 512], F16, "iota512")
        self.affTok = self.alloc([128, NT, NE], F32, "affTok")
        self.csT = self.alloc([128, 8, 2], F32, "csT")
        self.idxAll = self.alloc([128, NE, 5], I32, "idxAll")
        self.gateAll = self.alloc([128, NE, 5], F32, "gateAll")
        self.epsT = self.alloc([128, 1], F32, "eps")
        self.persist_end = self.aoff

        self.setup_consts()
        self.phase_mods()
        for i in range(self.nlayers):
            last = i == DEPTH - 1
            if i % 2 == 0:
                self.phase_gmlp(i)
            else:
                self.phase_ret(i)
            self.phase_routing(i)
            self.phase_experts(i)
        if self.nlayers == DEPTH:
            self.phase_final()
        self.p.barrier()
        self.p.emit()
        return self.nc

    def setup_consts(self):
        p = self.p
        iF, iB = self.identF, self.identB
        self.memset("pool", iF, 1.0)
        o = iF.ap
        p.op("pool", lambda e, o=o: e.affine_select(out=o, in_=o, pattern=[[-1, 128]], base=0, channel_multiplier=1,
                                               compare_op=ALU.is_equal, fill=0.0), _b(iF), _b(iF))
        self.copy("dve", iB, iF)
        io = self.iota512.ap
        p.op("pool", lambda e, io=io: e.iota(io, pattern=[[1, 512]], base=0, channel_multiplier=0,
                                      allow_small_or_imprecise_dtypes=True), (), _b(self.iota512))
        self.memset("dve", self.epsT, EPS)
        self.arena_reset()
        cv2 = self.alloc([2, D], F32, "cv2")
        self.dma(cv2, T(self.I["cvec"], Buf("cvec")))
        self.act(cv2, cv2, AF.Silu)
        pp = self.PS[0]
        self.trs([(pp[:, k * 2:(k + 1) * 2], cv2[:, k * 128:(k + 1) * 128]) for k in range(8)], self.identF)
        self.copy("dve", self.csT, pp[:, 0:16].re("p (k r) -> p k r", k=8))

    def load_bc(self, dst, src_ap1d, buf=None, q="sp"):
        P = dst.shape[0]
        self.dma(dst, T(src_ap1d.partition_broadcast(P), buf if buf is not None else Buf("const")), q=q)

    def cast_pieces(self, dst, src_ap, stage, nk=8):
        ncols = dst.shape[2]
        sv = src_ap.rearrange("(kc p) n -> p kc n", p=128)
        out = []
        for c0 in range(0, ncols, 1024):
            cw = min(1024, ncols - c0)
            for k0 in range(0, nk, 2):
                def piece(c0=c0, cw=cw, k0=k0):
                    st = stage[self.dq % len(stage)]
                    self.dq += 1
                    s = st[:, :, 0:cw]
                    self.dma(s, T(sv[:, k0:k0 + 2, c0:c0 + cw], Buf("w")))
                    d = dst[:, k0:k0 + 2, c0:c0 + cw]
                    m = self.dq % 8
                    eng = "dve" if m < 4 else "act"
                    self.copy(eng, d, s)
                out.append(piece)
        return out

    def load_cast(self, dst, src_ap, stage, nk=8):
        for f in self.cast_pieces(dst, src_ap, stage, nk):
            f()

    def phase_mods(self):
        p = self.p
        p.barrier()
        self.arena_reset()
        I = self.I
        stg = [self.alloc([128, 8, 512], F32, "adastg%d" % s) for s in range(2)]
        modv = self.alloc([2, 6 * D], F32, "modv")
        adab = self.alloc([2, 6 * D], F32, "adab")
        ng = self.alloc([2, 2, D], F32, "ng")
        for i in range(self.nlayers):
            self.load_bc(adab, I["ada_b"][i])
            self.load_bc(ng[:, 0, :], I["norm_mix_g"][i])
            self.load_bc(ng[:, 1, :], I["norm_ffn_g"][i])
            wv = I["ada_w"][i].rearrange("(kc p) n -> p kc n", p=128)
            for cg in range(12):
                st = stg[cg % 2]
                self.dma(st, T(wv[:, :, cg * 512:(cg + 1) * 512], Buf("adaw")))
                ps = self.PS[cg % 2][0:2, :]
                self.mm(ps, [(self.csT[:, kc, :], st[:, kc, :]) for kc in range(8)])
                self.tt("dve", modv[:, cg * 512:(cg + 1) * 512], ps, adab[:, cg * 512:(cg + 1) * 512], ALU.add)
            for s, g in ((1, 0), (4, 1)):
                v = modv[:, s * D:(s + 1) * D]
                self.stt(v, v, 1.0, ng[:, g, :], ALU.add, ALU.mult)
            self.dma(T(self.BCRd[i], self.BCRB[i]), modv)

    def rms_mod(self, xt, junk, A, B, out32, ss, rstd, out_eng="pool", outb=None):
        self.act(junk, xt, AF.Square, accum=ss)
        self.act(rstd, ss, AF.Sqrt, bias=self.epsT, scale=1.0 / D)
        self.recip(rstd, rstd)
        self.stt(out32, xt, rstd, A, ALU.mult, ALU.mult)
        if outb is None:
            self.tt(out_eng, out32, out32, B, ALU.add)
        else:
            self.tt(out_eng, outb, out32, B, ALU.add)

    def store_xh(self, t, xn, W, k):
        self.dma(T(self.Xd[t * 128:(t + 1) * 128, :], self.XB[t]), xn)
        self.dma(T(self.H2d[t * 128:(t + 1) * 128, :], self.H2B[t]), W["h2b"][k % 3])

    def moe_prep(self, i, t, xn, W, k):
        A2, B2 = W["A2"], W["B2"]
        h2, junk, h2b, h2T = W["h2"][k % 2], W["junk"], W["h2b"][k % 3], W["h2T"][k % 2]
        ss, rstd = W["ss2"][k % 2], W["rstd2"][k % 2]
        self.rms_mod(xn, junk, A2, B2, h2, ss, rstd)
        self.copy("act", h2b, h2)
        pa, pb = self.PS[5], self.PS[6]
        for half, pp in ((0, pa), (1, pb)):
            self.trs([(pp[:, q * 128:(q + 1) * 128], h2[:, (half * 4 + q) * 128:(half * 4 + q + 1) * 128]) for q in range(4)],
                     self.identF)
            self.copy("act" if half == 0 else "dve", h2T[:, half * 4:(half + 1) * 4, :],
                      pp.re("p (k n) -> p k n", k=4))
        lg = self.PS[7][:, 0:NE]
        self.mm(lg, [(h2T[:, kc, :], W["WR"][:, kc, :]) for kc in range(8)])
        mx, sm, ex = W["mx"][k % 2], W["sm"][k % 2], W["ex"][k % 2]
        o, a = mx.ap, lg.ap
        self.p.op("dve", lambda e, o=o, a=a: e.tensor_reduce(out=o, in_=a, axis=AX.X, op=ALU.max), _b(lg), _b(mx))
        self.ts("dve", mx, mx, -1.0, op0=ALU.mult)
        self.act(ex, lg, AF.Exp, bias=mx, scale=1.0, accum=sm)
        self.recip(sm, sm)
        self.ts("dve", self.affTok[:, t, :], ex, sm, op0=ALU.mult)

    def alloc_n(self, n, shape, dt, name):
        return [self.alloc(shape, dt, "%s_%d" % (name, q)) for q in range(n)]

    def alloc_prep(self, i):
        W = {}
        W["h2"] = self.alloc_n(2, [128, D], F32, "h2")
        W["junk"] = self.alloc([128, D], BF16, "junk")
        W["h2b"] = self.alloc_n(3, [128, D], BF16, "h2b")
        W["h2T"] = self.alloc_n(2, [128, 8, 128], F32, "h2T")
        W["WR"] = self.alloc([128, 8, NE], F32, "WR")
        for n in ("ss2", "rstd2", "mx", "sm"):
            W[n] = self.alloc_n(2, [128, 1], F32, n)
        W["ex"] = self.alloc_n(2, [128, NE], F32, "ex")
        self.dma(W["WR"], T(self.I["moe_w_router"][i].rearrange("(kc p) e -> p kc e", p=128), Buf("wr")))
        return W

    def pipeline(self, stages, items, weights=None):
        n, S = len(items), len(stages)
        weights = weights or [1] * S
        prog = self.p

        class Coop:
            def __init__(self, fn, w):
                self.fn, self.w = fn, w
                self.go = threading.Semaphore(0)
                self.back = threading.Semaphore(0)
                self.done = False
                self.exc = None
                self.left = 0
                self.th = threading.Thread(target=self.run)
                self.th.start()

            def run(self):
                self.go.acquire()
                try:
                    self.fn()
                except BaseException as e:
                    self.exc = e
                self.done = True
                self.back.release()

            def turn(self):
                self.left = self.w
                prog.hook = self.hook
                self.go.release()
                self.back.acquire()
                prog.hook = None
                if self.exc is not None:
                    raise self.exc

            def hook(self):
                self.left -= 1
                if self.left <= 0:
                    self.back.release()
                    self.go.acquire()

        for step in range(n + S - 1):
            act = []
            for si in [0] + list(range(S - 1, 0, -1)):
                k = step - si
                if 0 <= k < n:
                    act.append(Coop(lambda si=si, k=k: stages[si](k, items[k]), weights[si]))
            while act:
                for c in list(act):
                    c.turn()
                    if c.done:
                        c.th.join()
                        act.remove(c)

    def load_bcr(self, W, i, r, names):
        idx = {"B1": 0, "A1": 1, "G1": 2, "B2": 3, "A2": 4, "G2": 5}
        for n in names:
            k = idx[n]
            self.load_bc(W[n], self.BCRd[i, r, k * D:(k + 1) * D], self.BCRB[i])

    def xsrc(self, i, t):
        if i == 0:
            if t < NLT:
                return T(self.I["x"][t * 128:(t + 1) * 128, :], Buf("xin"))
            return T(self.I["ctx"][(t - NLT) * 128:(t - NLT + 1) * 128, :], Buf("cin"))
        return T(self.Xd[t * 128:(t + 1) * 128, :], self.XB[t])

    def phase_gmlp(self, i):
        p = self.p
        j = i // 2
        I = self.I
        p.barrier()
        self.arena_reset()
        stage = self.alloc_n(2, [128, 2, 1024], F32, "stg")
        WIN = self.alloc([128, 8, 2 * D], BF16, "WIN")
        WOUT = self.alloc([128, 8, D], BF16, "WOUT")
        WST = self.alloc([128, 8, 128], BF16, "WST")
        bsB = self.alloc([128, 8, 128], F32, "bsB")
        W = self.alloc_prep(i)
        for n in ("A1", "B1", "G1", "A2", "B2", "LNG", "LNB"):
            W[n] = self.alloc([128, D], F32, n)
        xt = self.alloc_n(2, [128, D], F32, "xt") + [T(stage[q].ap[:, r, :], stage[q].bufs) for q in range(2) for r in range(2)][:2]
        h32 = self.alloc([128, D], F32, "h32")
        hb = self.alloc_n(2, [128, D], BF16, "hb")
        hT = self.alloc_n(2, [128, 8, 128], BF16, "hT")
        uT = self.alloc_n(2, [128, 8, 128], F32, "uT")
        v = self.alloc_n(2, [128, D], F32, "v")
        vlb = self.alloc_n(2, [128, D], BF16, "vlb")
        t1 = self.alloc([128, 8, 128], F32, "t1")
        prodT = self.alloc_n(2, [128, 8, 128], BF16, "prodT")
        xn = self.alloc_n(2, [128, D], F32, "xn") + [T(stage[1].ap[:, 1, :], Buf("xn2"))]
        bst = self.alloc_n(2, [128, 2, 6], F32, "bst")
        mv = self.alloc_n(2, [128, 2], F32, "mv")
        rs = self.alloc_n(2, [128, 1], F32, "rs")
        ss = self.alloc_n(2, [128, 1], F32, "ss")
        rstd = self.alloc_n(2, [128, 1], F32, "rstd")
        wsl = t1

        self.load_cast(WIN, I["a_w_in"][j], stage)
        self.load_cast(WOUT, I["a_w_out"][j], stage)
        self.dma(wsl, T(I["a_w_s"][j].rearrange("g n m -> n g m"), Buf("ws")))
        for g in range(8):
            pp = self.PS[g % 2][:, 0:128]
            self.tr(pp, wsl[:, g, :], self.identF)
            self.copy("dve", WST[:, g, :], pp)
        self.load_bc(bsB.re("p g n -> p (g n)"), I["a_b_s"][j].rearrange("g n -> (g n)"))
        self.load_bc(W["LNG"], I["a_ln_g"][j])
        self.load_bc(W["LNB"], I["a_ln_b"][j])
        PS = self.PS

        def bc_reload(t, names):
            if t == 0:
                self.load_bcr(W, i, 0, names)
            if t == NLT:
                self.load_bcr(W, i, 1, names)

        def S0(k, t):
            self.dma(xt[k % 4], self.xsrc(i, t))

        def S5(k, t):
            self.store_xh(t, xn[k % 3], W, k)

        def S1(k, t):
            bc_reload(t, ("A1", "B1"))
            x = xt[k % 4]
            self.rms_mod(x, W["junk"], W["A1"], W["B1"], h32, ss[k % 2], rstd[k % 2], outb=hb[k % 2])
            pb = PS[0].bitcast(BF16)
            self.trs([(pb[:, q * 128:(q + 1) * 128], hb[k % 2][:, q * 128:(q + 1) * 128]) for q in range(8)], self.identB)
            self.copy("act", hT[k % 2], pb.re("p (k n) -> p k n", k=8))

        def S2(k, t):
            h, u, vv_ = hT[k % 2], uT[k % 2], v[k % 2]
            for half in range(2):
                pu = PS[1 + half]
                for q4 in range(4):
                    oc = half * 4 + q4
                    self.mm(pu[:, q4 * 128:(q4 + 1) * 128],
                            [(WIN[:, kc, oc * 128:(oc + 1) * 128], h[:, kc, :]) for kc in range(8)])
                self.act(u[:, half * 4:(half + 1) * 4, :], pu.re("p (k n) -> p k n", k=4), AF.Gelu_apprx_tanh)
            for half in range(2):
                pv = PS[1 + half]
                self.mm(pv, [(h[:, kc, :], WIN[:, kc, D + half * 512:D + (half + 1) * 512]) for kc in range(8)])
                self.act(vv_[:, half * 512:(half + 1) * 512], pv, AF.Gelu_apprx_tanh)
            b_, m_, r_ = bst[k % 2], mv[k % 2], rs[k % 2]
            for half in range(2):
                o, a = b_[:, half, :].ap, vv_[:, half * 512:(half + 1) * 512].ap
                p.op("dve", lambda e, o=o, a=a: e.bn_stats(out=o, in_=a), _b(vv_), _b(b_))
            o, a = m_.ap, b_.re("p a b -> p (a b)").ap
            p.op("dve", lambda e, o=o, a=a: e.bn_aggr(out=o, in_=a), _b(b_), _b(m_))
            self.act(r_, m_[:, 1:2], AF.Sqrt, bias=self.epsT, scale=1.0)
            self.recip(r_, r_)
            self.ts("dve", vv_, vv_, m_[:, 0:1], r_, op0=ALU.subtract, op1=ALU.mult)
            self.tt("pool", vv_, vv_, W["LNG"], ALU.mult)
            self.tt("pool", vlb[k % 2], vv_, W["LNB"], ALU.add)

        def S3(k, t):
            bc_reload(t, ("G1",))
            for half in range(2):
                psm = PS[3 + half]
                for q4 in range(4):
                    g = half * 4 + q4
                    self.mm(psm[:, q4 * 128:(q4 + 1) * 128], [(vlb[k % 2][:, g * 128:(g + 1) * 128], WST[:, g, :])])
                self.tt("dve", t1[:, half * 4:(half + 1) * 4, :], psm.re("p (k n) -> p k n", k=4),
                        bsB[:, half * 4:(half + 1) * 4, :], ALU.add)
            self.tt("pool", prodT[k % 2], t1, uT[k % 2], ALU.mult)
            x_n = xn[k % 3]
            for half in range(2):
                py = PS[3 + half]
                self.mm(py, [(prodT[k % 2][:, g, :], WOUT[:, g, half * 512:(half + 1) * 512]) for g in range(8)])
                self.tt("dve", x_n[:, half * 512:(half + 1) * 512], py, W["G1"][:, half * 512:(half + 1) * 512], ALU.mult)
            self.tt("pool", x_n, x_n, xt[k % 4], ALU.add)

        def S4(k, t):
            bc_reload(t, ("A2", "B2"))
            self.moe_prep(i, t, xn[k % 3], W, k)

        self.pipeline([S0, S1, S2, S3, S4, S5], list(range(NT)))

    def phase_ret(self, i):
        p = self.p
        j = i // 2
        I = self.I
        PS = self.PS
        last = i == DEPTH - 1
        RB = self.RB
        p.barrier()
        self.arena_reset()
        stage = self.alloc_n(2, [128, 2, 1024], F32, "stg")
        WQ, WK, WV, WGF, WGB = [self.alloc([128, 8, D], BF16, "Wr%d" % m) for m in range(5)]
        for m, Wm in enumerate((WQ, WK, WV, WGF, WGB)):
            self.load_cast(Wm, I["r_w_in"][j][:, m * D:(m + 1) * D], stage)
        A1 = self.alloc([128, D], F32, "A1")
        B1 = self.alloc([128, D], F32, "B1")
        Wb = {"A1": A1, "B1": B1}
        xt = self.alloc_n(2, [128, D], F32, "xt")
        junk = self.alloc([128, D], BF16, "junk")
        h32 = self.alloc([128, D], F32, "h32")
        hb = self.alloc_n(2, [128, D], BF16, "hb")
        hT = self.alloc_n(2, [128, 8, 128], BF16, "hT")
        ss = self.alloc_n(2, [128, 1], F32, "ss")
        rstd = self.alloc_n(2, [128, 1], F32, "rstd")
        cs = self.alloc_n(3, [128, 2, 128], F32, "cs")
        cs16 = self.alloc_n(2, [128, 2, 128], F32, "cs16")
        qr = self.alloc_n(2, [128, 8, 128], BF16, "qr")
        kr = self.alloc_n(2, [128, 8, 128], BF16, "kr")
        ta = [self.alloc_n(2, [128, 2, 128], F32, "ta%d" % q) for q in range(4)]
        kk = self.alloc_n(2, [128, D], BF16, "kk")
        vv = self.alloc_n(2, [128, D], BF16, "vv")
        gf = self.alloc_n(2, [128, D], BF16, "gf")
        gb = self.alloc_n(2, [128, D], BF16, "gb")
        csd = I["ropecs"].rearrange("c p n -> p c n")
        cnt = [0]

        def A1s(k, t):
            if t == 0:
                self.load_bcr(Wb, i, 0, ("A1", "B1"))
            if t == NLT:
                self.load_bcr(Wb, i, 1, ("A1", "B1"))
            x = xt[k % 2]
            self.rms_mod(x, junk, A1, B1, h32, ss[k % 2], rstd[k % 2], outb=hb[k % 2])
            pb = PS[0].bitcast(BF16)
            self.trs([(pb[:, q * 128:(q + 1) * 128], hb[k % 2][:, q * 128:(q + 1) * 128]) for q in range(8)], self.identB)
            self.copy("act", hT[k % 2], pb.re("p (k n) -> p k n", k=8))

        def A0s(k, t):
            self.dma(xt[k % 2], self.xsrc(i, t))
            if t < NLT:
                self.dma(cs[k % 3], T(csd[:, :, t * 128:(t + 1) * 128], Buf("ropecs")))

        def A4s(k, t):
            is_ctx = t >= NLT
            need_q = not (last and is_ctx)
            if need_q:
                self.dma(T(self.QTd[t], RB["QT"][t]), qr[k % 2])
            self.dma(T(self.KTd[t], RB["KT"][t]), kr[k % 2])
            self.dma(T(self.KKd[t], RB["KK"][t]), kk[k % 2])
            self.dma(T(self.VVd[t], RB["VV"][t]), vv[k % 2])
            if not (last and is_ctx):
                self.dma(T(self.GFd[t], RB["GF"][t]), gf[k % 2])
                self.dma(T(self.GBd[t], RB["GB"][t]), gb[k % 2])

        def A2s(k, t):
            is_ctx = t >= NLT
            need_q = not (last and is_ctx)
            h = hT[k % 2]
            if not is_ctx:
                self.ts("pool", cs16[k % 2], cs[k % 3], 0.0625, op0=ALU.mult)
            for (Wm, dst, base, tab) in ((WQ, qr[k % 2], 1, cs[k % 3]), (WK, kr[k % 2], 3, cs16[k % 2])):
                if Wm is WQ and not need_q:
                    continue
                for half in range(2):
                    bank = PS[base + half]
                    for q4 in range(4):
                        oc = half * 4 + q4
                        self.mm(bank[:, q4 * 128:(q4 + 1) * 128],
                                [(Wm[:, kc, oc * 128:(oc + 1) * 128], h[:, kc, :]) for kc in range(8)])
                    dview = dst[:, half * 4:(half + 1) * 4, :]
                    if is_ctx:
                        if Wm is WQ:
                            self.copy("act", dview, bank.re("p (k n) -> p k n", k=4))
                        else:
                            self.ts("dve", dview, bank.re("p (k n) -> p k n", k=4), 0.0625, op0=ALU.mult)
                    else:
                        bv = bank.re("p (h c n) -> p h c n", h=2, c=2)
                        t1, t2 = bv[:, :, 0, :], bv[:, :, 1, :]
                        cosb = tab[:, 0, :].bc(1, [128, 2, 128])
                        sinb = tab[:, 1, :].bc(1, [128, 2, 128])
                        dv4 = dview.re("p (h c) n -> p h c n", c=2)
                        q_ = cnt[0] % 2
                        cnt[0] += 1
                        self.tt("dve", ta[0][q_], t1, cosb, ALU.mult)
                        self.tt("dve", ta[1][q_], t2, sinb, ALU.mult)
                        self.tt("pool", dv4[:, :, 0, :], ta[0][q_], ta[1][q_], ALU.subtract)
                        self.tt("dve", ta[2][q_], t1, sinb, ALU.mult)
                        self.tt("dve", ta[3][q_], t2, cosb, ALU.mult)
                        self.tt("pool", dv4[:, :, 1, :], ta[2][q_], ta[3][q_], ALU.add)

        def A3s(k, t):
            is_ctx = t >= NLT
            need_q = not (last and is_ctx)
            h = hT[k % 2]
            nb = 0
            for (Wm, dst, fn) in ((WV, vv[k % 2], AF.Copy), (WGF, gf[k % 2], AF.Silu), (WGB, gb[k % 2], AF.Silu)):
                if last and is_ctx and Wm is not WV:
                    continue
                for half in range(2):
                    bank = PS[5 + nb % 2]
                    nb += 1
                    self.mm(bank, [(h[:, kc, :], Wm[:, kc, half * 512:(half + 1) * 512]) for kc in range(8)])
                    self.act(dst[:, half * 512:(half + 1) * 512], bank, fn)
            pb7 = PS[7].bitcast(BF16)
            self.trs([(pb7[:, oc * 128:(oc + 1) * 128], kr[k % 2][:, oc, :]) for oc in range(8)], self.identB)
            self.copy("dve", kk[k % 2], pb7)

        def A23(k, t):
            A2s(k, t)
            A3s(k, t)
        self.pipeline([A0s, A1s, A23, A4s], list(range(NT)))

        p.barrier()
        self.arena_reset()
        dcy = self.alloc([128, 8], F32, "dcy")
        lg = self.alloc([128, 8], F32, "lg")
        nlg = self.alloc([128, 8], F32, "nlg")
        one = self.alloc([128, 1], F32, "one")
        maskT = self.alloc([128, 8, 128], F32, "maskT")
        qdec = self.alloc([128, 8, 128], F32, "qdec")
        kdec = self.alloc([128, 8], F32, "kdec")
        cd = self.alloc([128, 8], F32, "cd")
        diff = self.alloc([128, 128], F32, "diff")
        rowf = self.alloc([128, 128], F32, "rowf")
        rowb = self.alloc([128, 128], F32, "rowb")
        colf = self.alloc([128, 1], F32, "colf")
        colb = self.alloc([128, 1], F32, "colb")
        self.load_bc(dcy[:, 0:4], I["r_decay_f"][j])
        self.load_bc(dcy[:, 4:8], I["r_decay_b"][j])
        self.memset("dve", one, 1.0)
        self.act(nlg, dcy, AF.Exp, scale=-1.0)
        self.act(nlg, nlg, AF.Ln, bias=one, scale=1.0)
        self.ts("dve", lg, nlg, -1.0, op0=ALU.mult)

        def iota(tile, pattern, base, cm):
            o = tile.ap
            p.op("pool", lambda e, o=o: e.iota(o, pattern=pattern, base=base, channel_multiplier=cm,
                                               allow_small_or_imprecise_dtypes=True), (), _b(tile))
        iota(diff, [[1, 128]], 0, -1)
        iota(rowf, [[1, 128]], 1, 0)
        iota(rowb, [[-1, 128]], 128, 0)
        iota(colf, [[0, 1]], 127, -1)
        iota(colb, [[0, 1]], 0, 1)
        for h in range(4):
            mf, mb = maskT[:, h, :], maskT[:, 4 + h, :]
            self.act(mf, diff, AF.Exp, scale=lg[:, h:h + 1])
            o = mf.ap
            p.op("pool", lambda e, o=o: e.affine_select(out=o, in_=o, pattern=[[1, 128]], base=0, channel_multiplier=-1,
                                                        compare_op=ALU.is_ge, fill=0.0), _b(mf), _b(mf))
            self.act(mb, diff, AF.Exp, scale=nlg[:, 4 + h:5 + h])
            o = mb.ap
            p.op("pool", lambda e, o=o: e.affine_select(out=o, in_=o, pattern=[[-1, 128]], base=0, channel_multiplier=1,
                                                        compare_op=ALU.is_gt, fill=0.0), _b(mb), _b(mb))
            self.act(qdec[:, h, :], rowf, AF.Exp, scale=lg[:, h:h + 1])
            self.act(qdec[:, 4 + h, :], rowb, AF.Exp, scale=lg[:, 4 + h:5 + h])
            self.act(kdec[:, h:h + 1], colf, AF.Exp, scale=lg[:, h:h + 1])
            self.act(kdec[:, 4 + h:5 + h], colb, AF.Exp, scale=lg[:, 4 + h:5 + h])
        self.act(cd, lg, AF.Exp, scale=128.0)
        keep = self.aoff

        def scan_pass(d):
            p.barrier()
            self.arena_reset(keep)
            ring = [{n: self.alloc(([128, 8, 128] if n in ("QT", "KT") else [128, D]), BF16, "%s%d" % (n, q))
                     for n in ("QT", "KT", "KK", "VV")} for q in range(3)]
            Qd = self.alloc_n(2, [128, 8, 128], BF16, "Qd")
            Kd = self.alloc_n(2, [128, D], BF16, "Kd")
            attm = [self.alloc_n(2, [128, 128], BF16, "attm%d" % h) for h in range(4)]
            S32 = [self.alloc([128, 2, 256], F32, "S32_%d" % h) for h in range(4)]
            Sbf = [self.alloc([128, 2, 256], BF16, "Sbf_%d" % h) for h in range(4)]
            HN = self.alloc_n(2, [128, D], F32, "HN")
            bst = [self.alloc_n(2, [128, 6], F32, "bst%d" % h) for h in range(4)]
            mv = [self.alloc_n(2, [128, 2], F32, "mv%d" % h) for h in range(4)]
            rs = [self.alloc_n(2, [128, 1], F32, "rs%d" % h) for h in range(4)]
            nb_ = [self.alloc_n(2, [128, 1], F32, "nb%d" % h) for h in range(4)]
            for h in range(4):
                self.memset("pool", S32[h], 0.0)
                self.memset("pool", Sbf[h], 0.0)
            if d == 1:
                HNf = self.alloc_n(3, [128, D], F32, "HNf")
                GFc = self.alloc_n(3, [128, D], BF16, "GFc")
                GBc = self.alloc_n(3, [128, D], BF16, "GBc")
                yb = self.alloc_n(2, [128, D], BF16, "yb")
            order = [NLT, NLT + 1] + list(range(NLT)) if d == 0 else [NLT + 1, NLT] + list(range(NLT - 1, -1, -1))

            def L(k, c):
                is_ctx = c >= NLT
                want_out = not (last and is_ctx)
                R_ = ring[k % 3]
                if want_out:
                    self.dma(R_["QT"], T(self.QTd[c], RB["QT"][c]))
                    self.dma(R_["KT"], T(self.KTd[c], RB["KT"][c]))
                self.dma(R_["KK"], T(self.KKd[c], RB["KK"][c]))
                self.dma(R_["VV"], T(self.VVd[c], RB["VV"][c]))
                if d == 1 and want_out:
                    self.dma(HNf[k % 3], T(self.HNd[c], RB["HN"][c]))
                    self.dma(GFc[k % 3], T(self.GFd[c], RB["GF"][c]))
                    self.dma(GBc[k % 3], T(self.GBd[c], RB["GB"][c]))

            def C(k, c):
                is_ctx = c >= NLT
                want_out = not (last and is_ctx)
                R_ = ring[k % 3]
                QTc, KTc, KKc, VVc = R_["QT"], R_["KT"], R_["KK"], R_["VV"]
                qd, kd, hn = Qd[k % 2], Kd[k % 2], HN[k % 2]
                if want_out:
                    self.tt("pool", qd.re("p (h c) n -> p h c n", c=2), QTc.re("p (h c) n -> p h c n", c=2),
                            qdec[:, d * 4:(d + 1) * 4, :].bc(2, [128, 4, 2, 128]), ALU.mult)
                self.tt("pool", kd.re("p (h k) -> p h k", h=4), KKc.re("p (h k) -> p h k", h=4),
                        kdec[:, d * 4:(d + 1) * 4].bc(2, [128, 4, 256]), ALU.mult)
                attb = PS[0] if k % 2 == 0 else PS[7]
                psos = [PS[1 + h // 2][:, (h % 2) * 256:(h % 2 + 1) * 256] for h in range(4)]
                vhs = [VVc[:, h * 256:(h + 1) * 256] for h in range(4)]
                if want_out:
                    for h in range(4):
                        att = attb[:, h * 128:(h + 1) * 128]
                        self.mm(att, [(KTc[:, 2 * h + jj, :], QTc[:, 2 * h + jj, :]) for jj in range(2)])
                    for h in range(4):
                        self.tt("dve", attm[h][k % 2], attb[:, h * 128:(h + 1) * 128], maskT[:, d * 4 + h, :], ALU.mult)
                    for h in range(4):
                        self.mm(psos[h], [(attm[h][k % 2], vhs[h]), (qd[:, 2 * h, :], Sbf[h][:, 0, :]),
                                          (qd[:, 2 * h + 1, :], Sbf[h][:, 1, :])])
                for h in range(4):
                    pss = PS[3 + h]
                    for jj in range(2):
                        self.mm(pss[:, jj * 256:(jj + 1) * 256], [(kd[:, h * 256 + jj * 128:h * 256 + (jj + 1) * 128], vhs[h])])
                for h in range(4):
                    s32 = S32[h].re("p a b -> p (a b)")
                    self.stt(s32, s32, cd[:, d * 4 + h:d * 4 + h + 1], PS[3 + h], ALU.mult, ALU.add)
                for h in range(4):
                    self.copy("act", Sbf[h].re("p a b -> p (a b)"), S32[h].re("p a b -> p (a b)"))
                if not want_out:
                    return
                for h in range(4):
                    o_, a_ = bst[h][k % 2].ap, psos[h].ap
                    p.op("dve", lambda e, o_=o_, a_=a_: e.bn_stats(out=o_, in_=a_), _b(psos[h]), _b(bst[h][k % 2]))
                for h in range(4):
                    o_, a_ = mv[h][k % 2].ap, bst[h][k % 2].ap
                    p.op("dve", lambda e, o_=o_, a_=a_: e.bn_aggr(out=o_, in_=a_), _b(bst[h][k % 2]), _b(mv[h][k % 2]))
                for h in range(4):
                    self.act(rs[h][k % 2], mv[h][k % 2][:, 1:2], AF.Sqrt, bias=self.epsT, scale=1.0)
                for h in range(4):
                    self.recip(rs[h][k % 2], rs[h][k % 2])
                for h in range(4):
                    self.stt(nb_[h][k % 2], mv[h][k % 2][:, 0:1], -1.0, rs[h][k % 2], ALU.mult, ALU.mult)
                for h in range(4):
                    self.act(hn[:, h * 256:(h + 1) * 256], psos[h], AF.Identity, bias=nb_[h][k % 2], scale=rs[h][k % 2])

            def O(k, c):
                is_ctx = c >= NLT
                want_out = not (last and is_ctx)
                if not want_out:
                    return
                hn = HN[k % 2]
                if d == 0:
                    self.dma(T(self.HNd[c], RB["HN"][c]), hn)
                    return
                self.tt("pool", HNf[k % 3], HNf[k % 3], GFc[k % 3], ALU.mult)
                self.tt("pool", hn, hn, GBc[k % 3], ALU.mult)
                self.tt("pool", yb[k % 2], HNf[k % 3], hn, ALU.add)
                self.dma(T(self.YYd[c], RB["YY"][c]), yb[k % 2])

            self.pipeline([L, C, O], order)

        scan_pass(0)
        scan_pass(1)

        p.barrier()
        self.arena_reset()
        stage = self.alloc_n(2, [128, 2, 1024], F32, "stg")
        WO = self.alloc([128, 8, D], BF16, "WO")
        self.load_cast(WO, I["r_w_out"][j], stage)
        W = self.alloc_prep(i)
        for n in ("G1", "A2", "B2"):
            W[n] = self.alloc([128, D], F32, n)
        ybc = self.alloc_n(2, [128, D], BF16, "ybc")
        yT = self.alloc_n(2, [128, 8, 128], BF16, "yT")
        xt = self.alloc_n(4, [128, D], F32, "xt")
        xn = self.alloc_n(3, [128, D], F32, "xn")
        tiles = list(range(NLT)) + ([] if last else [NLT, NLT + 1])

        def bc_reload(t, names):
            if t == 0:
                self.load_bcr(W, i, 0, names)
            if t == NLT:
                self.load_bcr(W, i, 1, names)

        def C0(k, t):
            self.dma(ybc[k % 2], T(self.YYd[t], RB["YY"][t]))
            self.dma(xt[k % 4], self.xsrc(i, t))

        def C4(k, t):
            self.store_xh(t, xn[k % 3], W, k)

        def C1(k, t):
            pb = PS[0].bitcast(BF16)
            self.trs([(pb[:, q * 128:(q + 1) * 128], ybc[k % 2][:, q * 128:(q + 1) * 128]) for q in range(8)], self.identB)
            self.copy("act", yT[k % 2], pb.re("p (k n) -> p k n", k=8))

        def C2(k, t):
            bc_reload(t, ("G1",))
            x_n = xn[k % 3]
            for half in range(2):
                py = PS[1 + half]
                self.mm(py, [(yT[k % 2][:, kc, :], WO[:, kc, half * 512:(half + 1) * 512]) for kc in range(8)])
                self.tt("dve", x_n[:, half * 512:(half + 1) * 512], py, W["G1"][:, half * 512:(half + 1) * 512], ALU.mult)
            self.tt("pool", x_n, x_n, xt[k % 4], ALU.add)

        def C3(k, t):
            bc_reload(t, ("A2", "B2"))
            self.moe_prep(i, t, xn[k % 3], W, k)

        self.pipeline([C0, C1, C2, C3, C4], tiles)

    def phase_routing(self, i):
        p = self.p
        p.barrier()
        self.arena_reset()
        last = i == DEPTH - 1
        PS = self.PS
        NP = 64
        affT = self.alloc([NP, NL], F32, "affT")
        msk = self.alloc([NP, NL], F32, "msk")
        cum = self.alloc([NP, NL], F32, "cum")
        junk = self.alloc([NP, NL], BF16, "rjunk")
        posT = self.alloc([128, NT, NP], F32, "posT")
        rhs5 = self.alloc([128, NT, NE, 5], BF16, "rhs5")
        affC = self.alloc([128, 2, 48], F32, "affC")
        sm = {n: self.alloc([NP, 1], F32, n) for n in ("lo", "hi", "mid", "cnt", "ge", "d", "cap", "zero")}
        r1 = self.alloc([128, NT, NE], F32, "r1")
        pc = self.alloc([128, NT, NE], F32, "pc")
        ohs = [self.alloc([128, 512], BF16, "oh%d" % s) for s in range(4)]
        ohc = [self.alloc([128, 32], BF16, "ohc%d" % s) for s in range(2)]
        res = self.alloc([128, 5, 5], F32, "res")
        idf = self.alloc([128, 5], F32, "idf")
        self.memset("dve", res, 0.0)

        if self.debug and "AFF" in self.debug:
            self.dma(T(self.AFFd, Buf("affd")), self.affTok.re("p t e -> p (t e)"))
        self.memset("pool", affT, -1.0)
        for g in range(8):
            pp = PS[g % 2]
            self.trs([(pp[0:NE, k * 128:(k + 1) * 128], self.affTok[:, g * 4 + k, :]) for k in range(4)], self.identF)
            self.copy("dve" if g % 2 == 0 else "act", affT[0:NE, g * 512:(g + 1) * 512], pp[0:NE, :])
        if not last:
            self.memset("dve", affC, 0.0)
            self.copy("dve", affC[:, :, 32:48], self.affTok[:, NLT:NT, :])
            pp = PS[2]
            self.trs([(pp[0:48, k * 128:(k + 1) * 128], affC[:, k, :]) for k in range(2)], self.identF)
            self.copy("dve", affT[32:48, 0:256], pp[32:48, 0:256])
        self.memset("dve", sm["cap"][0:32, :], float(CAP))
        self.memset("dve", sm["cap"][32:64, :], float(CAPC))
        self.memset("dve", sm["lo"], 0.0)
        self.memset("dve", sm["hi"], 1.0)
        lo, hi, mid, cnt, ge, d, cap = (sm[n] for n in ("lo", "hi", "mid", "cnt", "ge", "d", "cap"))
        for it in range(30):
            self.tt("dve", mid, lo, hi, ALU.add)
            self.ts("dve", mid, mid, 0.5, op0=ALU.mult)
            self.ts("dve", junk, affT, mid, 0.0, op0=ALU.is_ge, op1=ALU.add, accum=cnt)
            self.tt("dve", ge, cnt, cap, ALU.is_ge)
            self.tt("dve", d, mid, lo, ALU.subtract)
            self.stt(lo, d, ge, lo, ALU.mult, ALU.add)
            self.tt("dve", d, hi, mid, ALU.subtract)
            self.stt(hi, d, ge, mid, ALU.mult, ALU.add)
        self.ts("dve", msk, affT, lo, op0=ALU.is_ge)
        o, a = cum.ap, msk.ap
        self.memset("dve", sm["zero"], 0.0)
        z = sm["zero"].ap
        p.op("dve", lambda e, o=o, a=a, z=z: e.tensor_tensor_scan(out=o, data0=a, data1=a, initial=z, op0=ALU.add, op1=ALU.max),
             _b(msk, sm["zero"]), _b(cum))
        self.tt("dve", cum, cum, msk, ALU.mult)
        self.ts("dve", cum, cum, -1.0, op0=ALU.add)
        for g in range(4):
            pp = PS[3 + g % 2]
            self.trs([(pp[:, k * NP:(k + 1) * NP], cum[:, (g * 8 + k) * 128:(g * 8 + k + 1) * 128]) for k in range(8)],
                     self.identF)
            self.copy("act" if g % 2 else "dve", posT[:, g * 8:(g + 1) * 8, :], pp.re("p (k n) -> p k n", k=8))
        if not last:
            pp = PS[5]
            self.trs([(pp[:, k * NP:(k + 1) * NP], cum[:, k * 128:(k + 1) * 128]) for k in range(2)], self.identF)
            self.copy("dve", posT[:, NLT:NT, :], pp[:, 0:2 * NP].re("p (k n) -> p k n", k=2))
        o = pc.ap
        p.op("pool", lambda e, o=o: e.iota(o, pattern=[[0, NT * NE]], base=0, channel_multiplier=1,
                                      allow_small_or_imprecise_dtypes=True), (), _b(pc))
        self.copy("pool", rhs5[:, :, :, 0], pc)
        p.op("pool", lambda e, o=o: e.iota(o, pattern=[[1, NT], [0, NE]], base=0, channel_multiplier=0,
                                      allow_small_or_imprecise_dtypes=True), _b(rhs5), _b(pc))
        self.copy("pool", rhs5[:, :, :, 1], pc)
        self.copy("dve", rhs5[:, :, :, 2], self.affTok)
        self.tt("dve", r1, self.affTok, rhs5[:, :, :, 2], ALU.subtract)
        self.copy("dve", rhs5[:, :, :, 3], r1)
        self.tt("dve", r1, r1, rhs5[:, :, :, 3], ALU.subtract)
        self.copy("dve", rhs5[:, :, :, 4], r1)
        k = 0
        for e in range(NE):
            banks = [PS[(e % 2) * 4 + c] for c in range(4)]
            for t in range(NLT):
                oh = ohs[k % 4]
                k += 1
                self.ts("dve", oh, self.iota512, posT[:, t, e:e + 1], op0=ALU.is_equal)
                for c in range(4):
                    o_, l_, r_ = banks[c][:, 0:5].ap, oh[:, c * 128:(c + 1) * 128].ap, rhs5[:, t, e, :].ap
                    st, sp_ = (t == 0), (t == NLT - 1)
                    p.op("pe", lambda en, o_=o_, l_=l_, r_=r_, st=st, sp_=sp_: en.matmul(o_, l_, r_, start=st, stop=sp_),
                         _b(oh, rhs5), _b(banks[c]))
            for c in range(4):
                self.copy("act", res[:, c, :], banks[c][:, 0:5])
            if not last:
                for tt_ in range(2):
                    oc_ = ohc[tt_]
                    self.ts("dve", oc_, self.iota512[:, 0:32], posT[:, NLT + tt_, 32 + e:33 + e], op0=ALU.is_equal)
                bk = banks[0]
                self.mm(bk[0:32, 8:13], [(ohc[tt_], rhs5[:, NLT + tt_, e, :]) for tt_ in range(2)])
                self.copy("act", res[0:32, 4, :], bk[0:32, 8:13])
            nch = 4 if last else 5
            self.stt(idf[:, 0:nch], res[:, 0:nch, 1], 128.0, res[:, 0:nch, 0], ALU.mult, ALU.add)
            self.copy("dve", self.idxAll[:, e, 0:nch], idf[:, 0:nch])
            self.tt("dve", idf[:, 0:nch], res[:, 0:nch, 2], res[:, 0:nch, 3], ALU.add)
            self.tt("dve", self.gateAll[:, e, 0:nch], idf[:, 0:nch], res[:, 0:nch, 4], ALU.add)
        if self.debug and "AFF" in self.debug:
            self.dma(T(self.IDXd, Buf("idxd")), self.idxAll.re("p e c -> p (e c)"))
            self.dma(T(self.GATd, Buf("gatd")), self.gateAll.re("p e c -> p (e c)"))

    def phase_experts(self, i):
        p = self.p
        p.barrier()
        self.arena_reset()
        last = i == DEPTH - 1
        I = self.I
        PS = self.PS
        stage = self.alloc_n(4, [128, 2, 1024], F32, "stg")
        WS = [[self.alloc([128, 8, D], BF16, "W%d_%d" % (s, m)) for m in range(3)] for s in range(2)]
        NS = 512 if last else 544
        nch = 4 if last else 5
        xs = [self.alloc([128, 5, D], BF16, "xs%d" % s) for s in range(2)]
        xsT = self.alloc([128, 8, 544], BF16, "xsT")
        hidT = self.alloc([128, 8, 544], BF16, "hidT")
        sg = self.alloc([128, 544], F32, "sg")
        ye = [self.alloc([128, D], F32, "ye%d" % s) for s in range(2)]
        G2 = self.alloc([128, D], F32, "G2")
        G2c = self.alloc([128, D], F32, "G2c")
        self.load_bcr({"G2": G2}, i, 0, ("G2",))
        if not last:
            self.load_bcr({"G2": G2c}, i, 1, ("G2",))
        XSC = Buf("xscatter")
        halves = [(0, NS // 2), (NS // 2, NS)]
        nye = 0
        allX = self.XB

        def gather(e):
            x = xs[e % 2]
            for c in range(nch):
                rows = 128 if c < 4 else 32
                o_ = x[0:rows, c, :].ap
                ix = self.idxAll[0:rows, e, c:c + 1].ap
                src = self.H2d
                p.dma("pool", lambda en, o_=o_, ix=ix, src=src: en.indirect_dma_start(
                    out=o_, out_offset=None, in_=src, in_offset=bass.IndirectOffsetOnAxis(ap=ix, axis=0)),
                    _b(self.idxAll) + self.H2B, _b(x))

        def wpieces(e):
            s = e % 2
            return (self.cast_pieces(WS[s][0], I["moe_w_gate"][i, e], stage)
                    + self.cast_pieces(WS[s][1], I["moe_w_up"][i, e], stage)
                    + self.cast_pieces(WS[s][2], I["moe_w_down"][i, e], stage))

        gather(0)
        for f in wpieces(0):
            f()
        for e in range(NE):
            pend = []
            if e + 1 < NE:
                gather(e + 1)
                pend = wpieces(e + 1)
            x = xs[e % 2]
            WG, WU, WD = WS[e % 2]
            for c in range(nch):
                rows = 128 if c < 4 else 32
                pb = PS[c % 2].bitcast(BF16)
                self.trs([(pb[:, k * 128:k * 128 + rows], x[0:rows, c, k * 128:(k + 1) * 128]) for k in range(8)],
                         self.identB)
                self.copy("act" if c % 2 else "dve", xsT[:, :, c * 128:c * 128 + rows],
                          pb.re("p (k n) -> p k n", k=8)[:, :, 0:rows])
            for fc in range(8):
                if pend:
                    pend.pop(0)()
                for hi_, (a0, a1) in enumerate(halves):
                    pg, pu = PS[2 + hi_], PS[4 + hi_]
                    n = a1 - a0
                    self.mm(pg[:, 0:n], [(WG[:, kc, fc * 128:(fc + 1) * 128], xsT[:, kc, a0:a1]) for kc in range(8)])
                    self.mm(pu[:, 0:n], [(WU[:, kc, fc * 128:(fc + 1) * 128], xsT[:, kc, a0:a1]) for kc in range(8)])
                    self.act(sg[:, a0:a1], pg[:, 0:n], AF.Silu)
                    self.tt("dve", hidT[:, fc, a0:a1], pu[:, 0:n], sg[:, a0:a1], ALU.mult)
            for c in range(nch):
                if pend:
                    pend.pop(0)()
                rows = 128 if c < 4 else 32
                y = ye[nye % 2]
                nye += 1
                g2 = G2 if c < 4 else G2c
                for half in range(2):
                    py = PS[6 + half]
                    self.mm(py[0:rows, :], [(hidT[:, fc, c * 128:c * 128 + rows], WD[:, fc, half * 512:(half + 1) * 512])
                                            for fc in range(8)])
                    self.stt(y[0:rows, half * 512:(half + 1) * 512], py[0:rows, :], self.gateAll[0:rows, e, c:c + 1],
                             g2[0:rows, half * 512:(half + 1) * 512], ALU.mult, ALU.mult)
                o_ = self.Xd
                ix = self.idxAll[0:rows, e, c:c + 1].ap
                i_ = y[0:rows, :].ap
                p.dma("pool", lambda en, ix=ix, i_=i_, o_=o_: en.indirect_dma_start(
                    out=o_, out_offset=bass.IndirectOffsetOnAxis(ap=ix, axis=0), in_=i_, in_offset=None,
                    compute_op=ALU.add), _b(self.idxAll, y), [XSC] + allX)
            for f in pend:
                f()

    def phase_final(self):
        p = self.p
        p.barrier()
        self.arena_reset()
        FG = self.alloc([128, D], F32, "FG")
        self.load_bc(FG, self.I["final_norm_g"])
        xts = [self.alloc([128, D], F32, "fx%d" % s) for s in range(2)]
        outs = [self.alloc([128, D], F32, "fo%d" % s) for s in range(2)]
        junk = self.alloc([128, D], BF16, "fjunk")
        ss = self.alloc([128, 1], F32, "fss")
        rstd = self.alloc([128, 1], F32, "frstd")
        for t in range(NLT):
            xt, o = xts[t % 2], outs[t % 2]
            self.dma(xt, T(self.Xd[t * 128:(t + 1) * 128, :], self.XB[t]))
            self.act(junk, xt, AF.Square, accum=ss)
            self.act(rstd, ss, AF.Sqrt, bias=self.epsT, scale=1.0 / D)
            self.recip(rstd, rstd)
            self.stt(o, xt, rstd, FG, ALU.mult, ALU.mult)
            self.dma(T(self.out[t * 128:(t + 1) * 128, :], self.outB[t]), o)


def rope_tables():
    n = np.arange(NL)
    pos_r = (n // 64).astype(np.float32)
    pos_c = (n % 64).astype(np.float32)
    inv = np.power(np.float32(10000.0), -np.arange(64, dtype=np.float32) / np.float32(64)).astype(np.float32)
    ang = np.concatenate([pos_r[:, None] * inv[None], pos_c[:, None] * inv[None]], axis=-1)
    cs = np.stack([np.cos(ang).T, np.sin(ang).T]).astype(np.float32)
    return np.ascontiguousarray(cs)


_CACHE = {}


def make_in_maps(inputs, cores):
    f = lambda a: np.ascontiguousarray(np.asarray(a, dtype=np.float32))
    shared = {k: f(inputs[k]) for k in ("ada_w", "ada_b", "norm_mix_g", "norm_ffn_g", "a_w_in", "a_ln_g", "a_ln_b",
                                        "a_w_s", "a_b_s", "a_w_out", "r_w_in", "r_decay_f", "r_decay_b", "r_w_out",
                                        "moe_w_router", "moe_w_gate", "moe_w_up", "moe_w_down", "final_norm_g")}
    shared["ropecs"] = rope_tables()
    x, c, ctx, c_ctx = f(inputs["x"]), f(inputs["c"]), f(inputs["ctx"]), f(inputs["c_ctx"])
    maps = []
    for b in cores:
        m = dict(shared)
        m["x"] = x[b]
        m["ctx"] = ctx[b]
        m["cvec"] = np.ascontiguousarray(np.stack([c[b], c_ctx]))
        maps.append(m)
    return maps


def kernel(**inputs):
    if "nc" not in _CACHE:
        _CACHE["nc"] = K().build()
    nc = _CACHE["nc"]
    maps = make_in_maps(inputs, list(range(8)))
    res = run_bass_kernel_spmd(nc, maps, core_ids=list(range(8)))
    out = np.stack([np.asarray(r["out"], dtype=np.float32) for r in res.results], axis=0)
    return out
```

```python
import numpy as np
import threading
from contextlib import ExitStack
import concourse.bass as bass
import concourse.mybir as mybir
from concourse.bass_utils import run_bass_kernel_spmd

F32 = mybir.dt.float32
BF16 = mybir.dt.bfloat16
F16 = mybir.dt.float16
I32 = mybir.dt.int32
AF = mybir.ActivationFunctionType
ALU = mybir.AluOpType
AX = mybir.AxisListType

D = 1024
NL = 4096
NCX = 256
NT = 34
NLT = 32
DEPTH = 4
NE = 16
CAP = 512
CAPC = 32
EPS = 1e-6


class Buf:
    __slots__ = ("name", "w", "r")

    def __init__(self, name=""):
        self.name = name
        self.w = None
        self.r = {}


class T:
    __slots__ = ("ap", "bufs")

    def __init__(self, ap, bufs):
        self.ap = ap
        self.bufs = bufs if isinstance(bufs, (list, tuple)) else [bufs]

    def __getitem__(self, k):
        return T(self.ap[k], self.bufs)

    def re(self, s, **kw):
        return T(self.ap.rearrange(s, **kw), self.bufs)

    def bitcast(self, dt):
        return T(self.ap.bitcast(dt), self.bufs)

    def bc(self, axis, shape):
        return T(self.ap.unsqueeze(axis).to_broadcast(list(shape)), self.bufs)

    @property
    def shape(self):
        return self.ap.shape


class Prog:
    ENGS = ("pe", "dve", "act", "pool", "sp")
    NDMA = {"sp": 24, "pool": 12, "act": 8}

    def __init__(self, nc, same_engine_sync=True):
        self.nc = nc
        self.items = {e: [] for e in self.ENGS}
        self.cnt = {e: 0 for e in self.ENGS}
        self.dma_idx = {q: 0 for q in self.NDMA}
        self.seen = {e: {} for e in self.ENGS}
        self.same_engine_sync = same_engine_sync
        self.sems = {}
        self.last_dma = {}
        self.hook = None

    def _need(self, eng, dep):
        if dep is None:
            return
        key, val = dep
        if key == eng and (eng == "pe" or not self.same_engine_sync):
            return
        if self.seen[eng].get(key, 0) >= val:
            return
        self.seen[eng][key] = val
        self.items[eng].append(("wait", key, val))

    def _deps(self, eng, reads, writes):
        for b in reads:
            self._need(eng, b.w)
        for b in writes:
            self._need(eng, b.w)
            for d in b.r.items():
                self._need(eng, d)

    def _mark(self, me, reads, writes):
        for b in reads:
            if b.r.get(me[0], 0) < me[1]:
                b.r[me[0]] = me[1]
        for b in writes:
            b.w = me
            b.r = {}

    def op(self, eng, fn, reads=(), writes=()):
        self._deps(eng, reads, writes)
        self.cnt[eng] += 1
        me = (eng, self.cnt[eng])
        self.items[eng].append(("op", fn, eng))
        self._mark(me, reads, writes)
        if self.hook is not None:
            self.hook()
        return me

    def dma(self, q, fn, reads=(), writes=()):
        R = self.NDMA[q]
        i = self.dma_idx[q]
        self.dma_idx[q] += 1
        key = ("dma", q, i % R)
        if i >= R:
            self._need(q, (key, 16 * (i // R)))
        self._deps(q, reads, writes)
        me = (key, 16 * (i // R + 1))
        self.items[q].append(("dma", fn, key))
        self._mark(me, reads, writes)
        self.last_dma[key] = me
        if self.hook is not None:
            self.hook()
        return me

    def barrier(self):
        for e in self.ENGS:
            for e2 in ("pe", "dve", "act", "pool"):
                if self.cnt[e2] > 0:
                    self._need(e, (e2, self.cnt[e2]))
            for key, me in self.last_dma.items():
                self._need(e, me)

    def emit(self):
        nc = self.nc
        with ExitStack() as es:
            for e in ("pe", "dve", "act", "pool"):
                self.sems[e] = es.enter_context(nc.semaphore("s_" + e))
            for q, R in self.NDMA.items():
                for j in range(R):
                    self.sems[("dma", q, j)] = es.enter_context(nc.semaphore("d_%s_%d" % (q, j)))
            block = es.enter_context(nc.Block())
            sems = self.sems

            def run(engobj, items):
                for it in items:
                    if it[0] == "wait":
                        engobj.wait_ge(sems[it[1]], it[2])
                    elif it[0] == "op":
                        it[1](engobj).then_inc(sems[it[2]], 1)
                    else:
                        it[1](engobj).then_inc(sems[it[2]], 16)

            @block.tensor
            def _(e):
                run(e, self.items["pe"])

            @block.vector
            def _(e):
                run(e, self.items["dve"])

            @block.scalar
            def _(e):
                run(e, self.items["act"])

            @block.gpsimd
            def _(e):
                run(e, self.items["pool"])

            @block.sync
            def _(e):
                run(e, self.items["sp"])


def _b(*ts):
    out = []
    for t in ts:
        if isinstance(t, T):
            out.extend(t.bufs)
    return out


def _a(x):
    return x.ap if isinstance(x, T) else x


class K:
    def __init__(self, nlayers=DEPTH, debug=False):
        self.nlayers = nlayers
        self.debug = debug
        self.nc = bass.Bass("TRN2", target_bir_lowering=False)
        self.p = Prog(self.nc)
        self.es = ExitStack()
        self.dq = 0
        self.pref = None
        self._atomic = 0
        self._cur = None

    def act(self, out, in_, func, bias=None, scale=None, accum=None):
        kw = {}
        if bias is not None:
            kw["bias"] = _a(bias)
        if scale is not None:
            kw["scale"] = _a(scale)
        if accum is not None:
            kw["accum_out"] = _a(accum)
        o, i = out.ap, in_.ap
        self.p.op("act", lambda e: e.activation(out=o, in_=i, func=func, **kw),
                  _b(in_, bias, scale), _b(out, accum))

    def ts(self, eng, out, in0, s1, s2=None, op0=ALU.mult, op1=None, accum=None):
        kw = {}
        if op1 is not None:
            kw["op1"] = op1
        if accum is not None:
            kw["accum_out"] = _a(accum)
        o, i, a1, a2 = out.ap, in0.ap, _a(s1), _a(s2)
        ename = eng
        self.p.op(ename, lambda e: e.tensor_scalar(out=o, in0=i, scalar1=a1, scalar2=a2, op0=op0, **kw),
                  _b(in0, s1, s2), _b(out, accum))

    def tt(self, eng, out, in0, in1, op):
        o, a, b = out.ap, in0.ap, in1.ap
        self.p.op(eng, lambda e: e.tensor_tensor(out=o, in0=a, in1=b, op=op), _b(in0, in1), _b(out))

    def stt(self, out, in0, scalar, in1, op0, op1):
        o, a, s, b = out.ap, in0.ap, _a(scalar), in1.ap
        self.p.op("dve", lambda e: e.scalar_tensor_tensor(out=o, in0=a, scalar=s, in1=b, op0=op0, op1=op1),
                  _b(in0, scalar, in1), _b(out))

    def copy(self, eng, out, in_):
        o, i = out.ap, in_.ap
        if eng == "act":
            self.p.op("act", lambda e: e.activation(out=o, in_=i, func=AF.Copy), _b(in_), _b(out))
        else:
            self.p.op(eng, lambda e: e.tensor_copy(out=o, in_=i), _b(in_), _b(out))

    def memset(self, eng, out, val):
        o = out.ap
        self.p.op(eng, lambda e: e.memset(o, val), (), _b(out))

    def recip(self, out, in_):
        o, i = out.ap, in_.ap
        self.p.op("dve", lambda e: e.reciprocal(out=o, in_=i), _b(in_), _b(out))

    def mm(self, out, pairs, extra_reads=()):
        o = out.ap
        ps = [(l.ap, r.ap) for l, r in pairs]
        n = len(ps)

        def fn(e):
            ins = None
            for i, (l, r) in enumerate(ps):
                ins = e.matmul(o, l, r, start=(i == 0), stop=(i == n - 1))
            return ins
        rd = []
        for l, r in pairs:
            rd += _b(l, r)
        self.p.op("pe", fn, rd + list(extra_reads), _b(out))

    def tr(self, out, in_, ident):
        o, i = out.ap, in_.ap
        P = i.shape[0]
        d = ident.ap[0:P, 0:P]
        self.p.op("pe", lambda e: e.transpose(out=o, in_=i, identity=d), _b(in_, ident), _b(out))

    def trs(self, items, ident):
        lst = [(o.ap, i.ap) for o, i in items]
        d = ident.ap

        def fn(e):
            ins = None
            for o, i in lst:
                P = i.shape[0]
                ins = e.transpose(out=o, in_=i, identity=d[0:P, 0:P])
            return ins
        rd, wr = _b(ident), []
        for o, i in items:
            rd += _b(i)
            wr += _b(o)
        self.p.op("pe", fn, rd, wr)

    def dma(self, out, in_, q=None, **kw):
        if q is None:
            q = "sp"
        o, i = out.ap, in_.ap
        self.p.dma(q, lambda e: e.dma_start(out=o, in_=i, **kw), _b(in_), _b(out))

    def arena_reset(self, off=None):
        self.aoff = self.persist_end if off is None else off

    def alloc(self, shape, dt, name=""):
        esz = {F32: 4, BF16: 2, F16: 2, I32: 4}[dt]
        n = int(np.prod(shape[1:])) * esz
        n4 = (n + 3) // 4
        off = self.aoff
        self.aoff += n4 + (-n4) % 8
        assert self.aoff <= self.arena_n, ("SBUF arena overflow", name, self.aoff * 4)
        ap = self.arena[0:shape[0], off:off + n4]
        if dt != F32:
            ap = ap.bitcast(dt)
        ap = ap[:, 0:int(np.prod(shape[1:]))]
        if len(shape) > 2:
            names = " ".join("d%d" % i for i in range(len(shape) - 1))
            ap = ap.rearrange("p (%s) -> p %s" % (names, names), **{"d%d" % i: shape[i + 1] for i in range(len(shape) - 1)})
        return T(ap, Buf(name))

    def build(self):
        nc, es = self.nc, self.es
        dbg = self.debug

        def din(name, shape, dt=F32):
            return nc.dram_tensor(name, list(shape), dt, kind="ExternalInput").ap()

        def dscr(name, shape, dt=F32):
            kind = "ExternalOutput" if (dbg and name in dbg) else "Internal"
            return nc.dram_tensor(name, list(shape), dt, kind=kind).ap()

        I = {}
        I["x"] = din("x", [NL, D])
        I["ctx"] = din("ctx", [NCX, D])
        I["cvec"] = din("cvec", [2, D])
        I["ada_w"] = din("ada_w", [DEPTH, D, 6 * D])
        I["ada_b"] = din("ada_b", [DEPTH, 6 * D])
        I["norm_mix_g"] = din("norm_mix_g", [DEPTH, D])
        I["norm_ffn_g"] = din("norm_ffn_g", [DEPTH, D])
        I["a_w_in"] = din("a_w_in", [2, D, 2 * D])
        I["a_ln_g"] = din("a_ln_g", [2, D])
        I["a_ln_b"] = din("a_ln_b", [2, D])
        I["a_w_s"] = din("a_w_s", [2, 8, 128, 128])
        I["a_b_s"] = din("a_b_s", [2, 8, 128])
        I["a_w_out"] = din("a_w_out", [2, D, D])
        I["r_w_in"] = din("r_w_in", [2, D, 5 * D])
        I["r_decay_f"] = din("r_decay_f", [2, 4])
        I["r_decay_b"] = din("r_decay_b", [2, 4])
        I["r_w_out"] = din("r_w_out", [2, D, D])
        I["moe_w_router"] = din("moe_w_router", [DEPTH, D, NE])
        I["moe_w_gate"] = din("moe_w_gate", [DEPTH, NE, D, D])
        I["moe_w_up"] = din("moe_w_up", [DEPTH, NE, D, D])
        I["moe_w_down"] = din("moe_w_down", [DEPTH, NE, D, D])
        I["final_norm_g"] = din("final_norm_g", [D])
        I["ropecs"] = din("ropecs", [2, 128, NL])
        self.I = I
        self.out = nc.dram_tensor("out", [NL, D], F32, kind="ExternalOutput").ap()
        self.outB = [Buf("out%d" % t) for t in range(NLT)]

        self.Xd = dscr("X", [NT * 128, D])
        self.XB = [Buf("X%d" % t) for t in range(NT)]
        self.H2d = dscr("H2", [NT * 128, D], BF16)
        self.H2B = [Buf("H2_%d" % t) for t in range(NT)]
        self.BCRd = dscr("BCR", [DEPTH, 2, 6 * D])
        self.BCRB = [[Buf("BCR%d_%d" % (i, g)) for g in range(24)] for i in range(DEPTH)]
        self.QTd = dscr("QT", [NT, 128, 8, 128], BF16)
        self.KTd = dscr("KT", [NT, 128, 8, 128], BF16)
        self.KKd = dscr("KK", [NT, 128, D], BF16)
        self.VVd = dscr("VV", [NT, 128, D], BF16)
        self.GFd = dscr("GF", [NT, 128, D], BF16)
        self.GBd = dscr("GB", [NT, 128, D], BF16)
        self.HNd = dscr("HN", [NT, 128, D])
        self.YYd = dscr("YY", [NT, 128, D], BF16)
        self.RB = {n: [Buf("%s%d" % (n, t)) for t in range(NT)] for n in ("QT", "KT", "KK", "VV", "GF", "GB", "HN", "YY")}
        if dbg and "AFF" in dbg:
            self.AFFd = dscr("AFF", [128, NT * NE])
            self.IDXd = dscr("IDX", [128, NE * 5], I32)
            self.GATd = dscr("GAT", [128, NE * 5])

        self.arena_n = 49 * 1024
        self.arena = es.enter_context(nc.sbuf_tensor("arena", [128, self.arena_n], F32))
        self.aoff = 0
        self.persist_end = 0
        self.PS = []
        for b in range(8):
            t = es.enter_context(nc.psum_tensor("ps%d" % b, [128, 512], F32))
            self.PS.append(T(t[:, :], Buf("ps%d" % b)))

        self.identF = self.alloc([128, 128], F32, "identF")
        self.identB = self.alloc([128, 128], BF16, "identB")
        self.iota512 = self.alloc([128, 512], F16, "iota512")
        self.affTok = self.alloc([128, NT, NE], F32, "affTok")
        self.csT = self.alloc([128, 8, 2], F32, "csT")
        self.idxAll = self.alloc([128, NE, 5], I32, "idxAll")
        self.gateAll = self.alloc([128, NE, 5], F32, "gateAll")
        self.epsT = self.alloc([128, 1], F32, "eps")
        self.mhalf = self.alloc([128, 4], F32, "mhalf")
        self.persist_end = self.aoff

        self.setup_consts()
        self.phase_mods_fast()
        for i in range(self.nlayers):
            last = i == DEPTH - 1
            if i % 2 == 0:
                self.phase_gmlp(i)
            else:
                self.phase_ret(i)
            self.phase_routing(i)
            self.phase_experts(i)
        if self.nlayers == DEPTH:
            self.phase_final()
        self.p.barrier()
        self.p.emit()
        return self.nc

    def setup_consts(self):
        p = self.p
        iF, iB = self.identF, self.identB
        self.memset("pool", iF, 1.0)
        o = iF.ap
        p.op("pool", lambda e, o=o: e.affine_select(out=o, in_=o, pattern=[[-1, 128]], base=0, channel_multiplier=1,
                                               compare_op=ALU.is_equal, fill=0.0), _b(iF), _b(iF))
        self.copy("dve", iB, iF)
        io = self.iota512.ap
        p.op("pool", lambda e, io=io: e.iota(io, pattern=[[1, 512]], base=0, channel_multiplier=0,
                                      allow_small_or_imprecise_dtypes=True), (), _b(self.iota512))
        self.memset("dve", self.epsT, EPS)
        self.memset("pool", self.mhalf, -0.5)
        self.arena_reset()
        cv2 = self.alloc([2, D], F32, "cv2")
        self.dma(cv2, T(self.I["cvec"], Buf("cvec")))
        self.act(cv2, cv2, AF.Silu)
        pp = self.PS[0]
        self.trs([(pp[:, k * 2:(k + 1) * 2], cv2[:, k * 128:(k + 1) * 128]) for k in range(8)], self.identF)
        self.copy("dve", self.csT, pp[:, 0:16].re("p (k r) -> p k r", k=8))

    def load_bc(self, dst, src_ap1d, buf=None, q="sp"):
        P = dst.shape[0]
        self.dma(dst, T(src_ap1d.partition_broadcast(P), buf if buf is not None else Buf("const")), q=q)

    def cast_pieces(self, dst, src_ap, stage, nk=8, engs=("dve", "act")):
        ncols = dst.shape[2]
        sv = src_ap.rearrange("(kc p) n -> p kc n", p=128)
        out = []
        for c0 in range(0, ncols, 1024):
            cw = min(1024, ncols - c0)
            for k0 in range(0, nk, 2):
                def piece(c0=c0, cw=cw, k0=k0):
                    st = stage[self.dq % len(stage)]
                    self.dq += 1
                    s = st[:, :, 0:cw]
                    self.dma(s, T(sv[:, k0:k0 + 2, c0:c0 + cw], Buf("w")))
                    d = dst[:, k0:k0 + 2, c0:c0 + cw]
                    m = self.dq % 8
                    eng = engs[0] if m < 4 else engs[-1]
                    self.copy(eng, d, s)
                out.append(piece)
        return out

    def load_cast(self, dst, src_ap, stage, nk=8):
        for f in self.cast_pieces(dst, src_ap, stage, nk):
            f()

    def mods_alloc(self):
        M = {}
        M["stg"] = self.alloc_n(2, [128, 8, 256], F32, "adastg")
        M["adab"] = self.alloc_n(2, [2, 256], F32, "adab")
        M["ngp"] = self.alloc_n(2, [2, 256], F32, "ngp")
        M["mod"] = self.alloc_n(2, [2, 256], F32, "modp")
        return M

    def mods_layer(self, i, M, banks):
        I = self.I
        wv = I["ada_w"][i].rearrange("(kc p) n -> p kc n", p=128)
        for cg in range(24):
            c0 = cg * 256
            st, ab, ngp, md = M["stg"][cg % 2], M["adab"][cg % 2], M["ngp"][cg % 2], M["mod"][cg % 2]
            self.dma(st, T(wv[:, :, c0:c0 + 256], Buf("adaw")))
            self.load_bc(ab, I["ada_b"][i][c0:c0 + 256])
            slot = c0 // D
            if slot in (1, 4):
                gsrc = I["norm_mix_g"][i] if slot == 1 else I["norm_ffn_g"][i]
                self.load_bc(ngp, gsrc[c0 - slot * D:c0 - slot * D + 256])
            ps = banks[cg % 2][0:2, 0:256]
            self.mm(ps, [(self.csT[:, kc, :], st[:, kc, :]) for kc in range(8)] + [(self.identF[0:2, 0:2], ab)])
            self.copy("act", md, ps)
            if slot in (1, 4):
                self.ts("pool", md, md, 1.0, op0=ALU.add)
                self.tt("pool", md, md, ngp, ALU.mult)
            self.dma(T(self.BCRd[i, :, c0:c0 + 256], self.BCRB[i][cg]), md)

    def phase_mods_fast(self):
        p = self.p
        p.barrier()
        self.arena_reset()
        I = self.I
        stg = [self.alloc([128, 8, 512], F32, "adastg%d" % s) for s in range(2)]
        modv = self.alloc([2, 6 * D], F32, "modv")
        adab = self.alloc([2, 6 * D], F32, "adab")
        ng = self.alloc([2, 2, D], F32, "ng")
        for i in range(self.nlayers):
            self.load_bc(adab, I["ada_b"][i])
            self.load_bc(ng[:, 0, :], I["norm_mix_g"][i])
            self.load_bc(ng[:, 1, :], I["norm_ffn_g"][i])
            wv = I["ada_w"][i].rearrange("(kc p) n -> p kc n", p=128)
            for cg in range(12):
                st = stg[cg % 2]
                self.dma(st, T(wv[:, :, cg * 512:(cg + 1) * 512], Buf("adaw")))
                ps = self.PS[cg % 2][0:2, :]
                self.mm(ps, [(self.csT[:, kc, :], st[:, kc, :]) for kc in range(8)])
                self.tt("dve", modv[:, cg * 512:(cg + 1) * 512], ps, adab[:, cg * 512:(cg + 1) * 512], ALU.add)
            for s, g in ((1, 0), (4, 1)):
                v = modv[:, s * D:(s + 1) * D]
                self.stt(v, v, 1.0, ng[:, g, :], ALU.add, ALU.mult)
            self.dma(T(self.BCRd[i], self.BCRB[i]), modv)

    def phase_mods(self):
        self.p.barrier()
        self.arena_reset()
        M = self.mods_alloc()
        self.mods_layer(0, M, [self.PS[6], self.PS[7]])

    def interleave(self, fns):
        items = list(range(len(fns)))
        self.pipeline2(lambda k, it: fns[it](), items, all_at_once=True)

    def rsqrt(self, out, in_, scale):
        n = out.shape[1]
        self.ts("dve", out, in_, scale, EPS, op0=ALU.mult, op1=ALU.add)
        self.tt("pool", out, out, self.mhalf[0:out.shape[0], 0:n], ALU.pow)

    def rms_mod(self, xt, junk, A, B, out32, ss, rstd, out_eng="pool", outb=None):
        self.act(junk, xt, AF.Square, accum=ss)
        self.rsqrt(rstd, ss, 1.0 / D)
        self.stt(out32, xt, rstd, A, ALU.mult, ALU.mult)
        if outb is None:
            self.tt(out_eng, out32, out32, B, ALU.add)
        else:
            self.tt(out_eng, outb, out32, B, ALU.add)

    def store_xh(self, t, xn, W, k):
        self.dma(T(self.Xd[t * 128:(t + 1) * 128, :], self.XB[t]), xn)
        self.dma(T(self.H2d[t * 128:(t + 1) * 128, :], self.H2B[t]), W["h2b"][k % 3])

    def moe_prep(self, i, t, xn, W, k, cuts=False):
        A2, B2 = W["A2"], W["B2"]
        h2, junk, h2b, h2T = W["h2"][k % 2], W["junk"], W["h2b"][k % 3], W["h2T"][k % 2]
        ss, rstd = W["ss2"][k % 2], W["rstd2"][k % 2]
        self.rms_mod(xn, junk, A2, B2, h2, ss, rstd, out_eng="dve")
        self.copy("act", h2b, h2)
        if cuts:
            self.cut()
        pa, pb = self.PS[5], self.PS[6]
        for half, pp in ((0, pa), (1, pb)):
            self.trs([(pp[:, q * 128:(q + 1) * 128], h2[:, (half * 4 + q) * 128:(half * 4 + q + 1) * 128]) for q in range(4)],
                     self.identF)
            self.copy("act", h2T[:, half * 4:(half + 1) * 4, :], pp.re("p (k n) -> p k n", k=4))
        if cuts:
            self.cut()
        lg = self.PS[7][:, 0:NE]
        self.mm(lg, [(h2T[:, kc, :], W["WR"][:, kc, :]) for kc in range(8)])
        self.copy("act", self.affTok[:, t, :], lg)

    def alloc_n(self, n, shape, dt, name):
        return [self.alloc(shape, dt, "%s_%d" % (name, q)) for q in range(n)]

    def alloc_prep(self, i):
        W = {}
        W["h2"] = self.alloc_n(2, [128, D], F32, "h2")
        W["junk"] = self.alloc([128, D], BF16, "junk")
        W["h2b"] = self.alloc_n(3, [128, D], BF16, "h2b")
        W["h2T"] = self.alloc_n(2, [128, 8, 128], F32, "h2T")
        W["WR"] = self.alloc([128, 8, NE], F32, "WR")
        for n in ("ss2", "rstd2", "mx", "sm"):
            W[n] = self.alloc_n(2, [128, 1], F32, n)
        W["ex"] = self.alloc_n(2, [128, NE], F32, "ex")
        self.dma(W["WR"], T(self.I["moe_w_router"][i].rearrange("(kc p) e -> p kc e", p=128), Buf("wr")))
        return W

    def pipeline(self, stages, items, weights=None):
        n, S = len(items), len(stages)
        weights = weights or [1] * S
        prog = self.p

        class Coop:
            def __init__(self, fn, w):
                self.fn, self.w = fn, w
                self.go = threading.Semaphore(0)
                self.back = threading.Semaphore(0)
                self.done = False
                self.exc = None
                self.left = 0
                self.th = threading.Thread(target=self.run)
                self.th.start()

            def run(self):
                self.go.acquire()
                try:
                    self.fn()
                except BaseException as e:
                    self.exc = e
                self.done = True
                self.back.release()

            def turn(self):
                self.left = self.w
                prog.hook = self.hook
                self.go.release()
                self.back.acquire()
                prog.hook = None
                if self.exc is not None:
                    raise self.exc

            def hook(self):
                self.left -= 1
                if self.left <= 0:
                    self.back.release()
                    self.go.acquire()

        for step in range(n + S - 1):
            act = []
            for si in [0] + list(range(S - 1, 0, -1)):
                k = step - si
                if 0 <= k < n:
                    act.append(Coop(lambda si=si, k=k: stages[si](k, items[k]), weights[si]))
            while act:
                for c in list(act):
                    c.turn()
                    if c.done:
                        c.th.join()
                        act.remove(c)

    class _Atomic:
        def __init__(self, k):
            self.k = k

        def __enter__(self):
            self.k._atomic += 1

        def __exit__(self, *a):
            self.k._atomic -= 1

    def atomic(self):
        return K._Atomic(self)

    def cut(self):
        c = self._cur
        c.parked = True
        c.back.release()
        c.go.acquire()

    def pipeline2(self, fn, items, all_at_once=False):
        prog = self.p
        outer = self

        class Coop:
            def __init__(self, f):
                self.f = f
                self.go = threading.Semaphore(0)
                self.back = threading.Semaphore(0)
                self.done = False
                self.parked = False
                self.exc = None
                self.th = threading.Thread(target=self.run)
                self.th.start()

            def run(self):
                self.go.acquire()
                try:
                    self.f()
                except BaseException as e:
                    self.exc = e
                self.done = True
                self.back.release()

            def turn(self):
                outer._cur = self
                prog.hook = self.hook
                self.go.release()
                self.back.acquire()
                prog.hook = None
                if self.exc is not None:
                    raise self.exc

            def hook(self):
                if outer._atomic:
                    return
                self.back.release()
                self.go.acquire()

        live = []
        nxt = 0
        while nxt < len(items) or live:
            while nxt < len(items):
                live.insert(0, Coop(lambda k=nxt: fn(k, items[k])))
                nxt += 1
                if not all_at_once:
                    break
            running = list(live)
            while running:
                for c in list(running):
                    c.turn()
                    if c.done:
                        c.th.join()
                        running.remove(c)
                        live.remove(c)
                    elif c.parked:
                        c.parked = False
                        running.remove(c)

    def load_bcr(self, W, i, r, names):
        idx = {"B1": 0, "A1": 1, "G1": 2, "B2": 3, "A2": 4, "G2": 5}
        for n in names:
            k = idx[n]
            self.load_bc(W[n], self.BCRd[i, r, k * D:(k + 1) * D], self.BCRB[i][4 * k:4 * k + 4])

    def xsrc(self, i, t):
        if i == 0:
            if t < NLT:
                return T(self.I["x"][t * 128:(t + 1) * 128, :], Buf("xin"))
            return T(self.I["ctx"][(t - NLT) * 128:(t - NLT + 1) * 128, :], Buf("cin"))
        return T(self.Xd[t * 128:(t + 1) * 128, :], self.XB[t])

    def phase_gmlp(self, i):
        p = self.p
        j = i // 2
        I = self.I
        p.barrier()
        self.arena_reset()
        stage = self.alloc_n(2, [128, 2, 1024], F32, "stg")
        WIN = self.alloc([128, 8, 2 * D], BF16, "WIN")
        WOUT = self.alloc([128, 8, D], BF16, "WOUT")
        WST = self.alloc([128, 8, 128], BF16, "WST")
        bsB = self.alloc([128, 8, 128], F32, "bsB")
        W = self.alloc_prep(i)
        for n in ("A1", "B1", "G1", "A2", "B2"):
            W[n] = self.alloc([128, D], F32, n)
        xt = self.alloc_n(2, [128, D], F32, "xt")
        h32 = self.alloc([128, D], F32, "h32")
        hb = self.alloc_n(2, [128, D], BF16, "hb")
        hT = self.alloc_n(2, [128, 8, 128], BF16, "hT")
        uT = self.alloc_n(2, [128, 8, 128], F32, "uT")
        v = self.alloc_n(2, [128, D], F32, "v")
        vlb = self.alloc_n(2, [128, D], BF16, "vlb")
        t1 = self.alloc([128, 8, 128], F32, "t1")
        prodT = self.alloc_n(2, [128, 8, 128], BF16, "prodT")
        bst = self.alloc_n(2, [128, 2, 6], F32, "bst")
        mv = self.alloc_n(2, [128, 2], F32, "mv")
        rs = self.alloc_n(2, [128, 1], F32, "rs")
        ss = self.alloc_n(2, [128, 1], F32, "ss")
        rstd = self.alloc_n(2, [128, 1], F32, "rstd")
        wsl = t1

        self.load_cast(WIN, I["a_w_in"][j], stage)
        self.load_cast(WOUT, I["a_w_out"][j], stage)
        self.dma(wsl, T(I["a_w_s"][j].rearrange("g n m -> n g m"), Buf("ws")))
        for g in range(8):
            pp = self.PS[g % 2][:, 0:128]
            self.tr(pp, wsl[:, g, :], self.identF)
            self.copy("dve", WST[:, g, :], pp)
        self.load_bc(bsB.re("p g n -> p (g n)"), I["a_b_s"][j].rearrange("g n -> (g n)"))
        PS = self.PS
        lngT = self.alloc([128, 8], F32, "lngT")
        lnbT = self.alloc([128, 8], F32, "lnbT")
        self.dma(lngT, T(I["a_ln_g"][j].rearrange("(g d) -> d g", d=128), Buf("lng")), allow_slow_non_contiguous=True)
        self.dma(lnbT, T(I["a_ln_b"][j].rearrange("(g d) -> d g", d=128), Buf("lnb")), allow_slow_non_contiguous=True)
        onesB = self.alloc([128, 128], BF16, "onesB")
        self.memset("dve", onesB, 1.0)
        for half in range(2):
            pr = PS[half]
            for q4 in range(4):
                g = half * 4 + q4
                self.mm(pr[:, q4 * 128:(q4 + 1) * 128], [(onesB, WST[:, g, :])])
            for q4 in range(4):
                g = half * 4 + q4
                self.stt(bsB[:, g, :], pr[:, q4 * 128:(q4 + 1) * 128], lnbT[:, g:g + 1], bsB[:, g, :], ALU.mult, ALU.add)

        def bc_reload(t, names):
            if t == 0:
                self.load_bcr(W, i, 0, names)
            if t == NLT:
                self.load_bcr(W, i, 1, names)

        uT3 = uT + [self.alloc([128, 8, 128], F32, "uT_2")]
        t1s = [t1, self.alloc([128, 8, 128], F32, "t1_1")]
        xt2 = [T(stage[0].ap[:, r, :], Buf("xt2_%d" % r)) for r in range(2)]
        xn4 = self.alloc_n(3, [128, D], F32, "xn") + [T(stage[1].ap[:, 0, :], Buf("xn3"))]

        def tile(k, t):
            x = xt[k % 2]
            self.dma(x, self.xsrc(i, t))
            self.cut()
            bc_reload(t, ("A1", "B1"))
            self.rms_mod(x, W["junk"], W["A1"], W["B1"], h32, ss[k % 2], rstd[k % 2], outb=hb[k % 2])
            self.cut()
            pb = PS[0].bitcast(BF16)
            self.trs([(pb[:, q * 128:(q + 1) * 128], hb[k % 2][:, q * 128:(q + 1) * 128]) for q in range(8)], self.identB)
            self.copy("act", hT[k % 2], pb.re("p (k n) -> p k n", k=8))
            self.cut()
            h, u, vv_ = hT[k % 2], uT3[k % 3], v[k % 2]
            for half in range(2):
                pu = PS[1 + half]
                for q4 in range(4):
                    oc = half * 4 + q4
                    self.mm(pu[:, q4 * 128:(q4 + 1) * 128],
                            [(WIN[:, kc, oc * 128:(oc + 1) * 128], h[:, kc, :]) for kc in range(8)])
                self.act(u[:, half * 4:(half + 1) * 4, :], pu.re("p (k n) -> p k n", k=4), AF.Gelu_apprx_tanh)
            for half in range(2):
                pv = PS[1 + half]
                self.mm(pv, [(h[:, kc, :], WIN[:, kc, D + half * 512:D + (half + 1) * 512]) for kc in range(8)])
                self.act(vv_[:, half * 512:(half + 1) * 512], pv, AF.Gelu_apprx_tanh)
            self.cut()
            b_, m_, r_ = bst[k % 2], mv[k % 2], rs[k % 2]
            for half in range(2):
                o, a = b_[:, half, :].ap, vv_[:, half * 512:(half + 1) * 512].ap
                p.op("dve", lambda e, o=o, a=a: e.bn_stats(out=o, in_=a), _b(vv_), _b(b_))
            o, a = m_.ap, b_.re("p a b -> p (a b)").ap
            p.op("dve", lambda e, o=o, a=a: e.bn_aggr(out=o, in_=a), _b(b_), _b(m_))
            self.rsqrt(r_, m_[:, 1:2], 1.0)
            self.ts("dve", vlb[k % 2], vv_, m_[:, 0:1], r_, op0=ALU.subtract, op1=ALU.mult)
            self.cut()
            t1_ = t1s[k % 2]
            for half in range(2):
                psm = PS[3 + half]
                with self.atomic():
                    for q4 in range(4):
                        g = half * 4 + q4
                        self.mm(psm[:, q4 * 128:(q4 + 1) * 128], [(vlb[k % 2][:, g * 128:(g + 1) * 128], WST[:, g, :])])
                    for q4 in range(4):
                        g = half * 4 + q4
                        self.stt(t1_[:, g, :], psm[:, q4 * 128:(q4 + 1) * 128], lngT[:, g:g + 1], bsB[:, g, :], ALU.mult, ALU.add)
            self.tt("pool", prodT[k % 2], t1_, u, ALU.mult)
            x2 = xt2[k % 2]
            self.dma(x2, self.xsrc(i, t))
            self.cut()
            bc_reload(t, ("G1",))
            x_n = xn4[k % 4]
            for half in range(2):
                py = PS[3 + half]
                with self.atomic():
                    self.mm(py, [(prodT[k % 2][:, g, :], WOUT[:, g, half * 512:(half + 1) * 512]) for g in range(8)])
                    self.tt("dve", x_n[:, half * 512:(half + 1) * 512], py, W["G1"][:, half * 512:(half + 1) * 512], ALU.mult)
            self.tt("pool", x_n, x_n, x2, ALU.add)
            self.cut()
            bc_reload(t, ("A2", "B2"))
            self.moe_prep(i, t, x_n, W, k, cuts=True)
            self.store_xh(t, x_n, W, k)

        self.pipeline2(tile, list(range(NT)))

    def phase_ret(self, i):
        p = self.p
        j = i // 2
        I = self.I
        PS = self.PS
        last = i == DEPTH - 1
        RB = self.RB
        p.barrier()
        self.arena_reset()
        stage = self.alloc_n(2, [128, 2, 1024], F32, "stg")
        WQ, WK, WV, WGF, WGB = [self.alloc([128, 8, D], BF16, "Wr%d" % m) for m in range(5)]
        for m, Wm in enumerate((WQ, WK, WV, WGF, WGB)):
            self.load_cast(Wm, I["r_w_in"][j][:, m * D:(m + 1) * D], stage)
        A1 = self.alloc([128, D], F32, "A1")
        B1 = self.alloc([128, D], F32, "B1")
        Wb = {"A1": A1, "B1": B1}
        xt = self.alloc_n(2, [128, D], F32, "xt")
        junk = self.alloc([128, D], BF16, "junk")
        h32 = self.alloc([128, D], F32, "h32")
        hb = self.alloc_n(2, [128, D], BF16, "hb")
        hT = self.alloc_n(2, [128, 8, 128], BF16, "hT")
        ss = self.alloc_n(2, [128, 1], F32, "ss")
        rstd = self.alloc_n(2, [128, 1], F32, "rstd")
        cs = self.alloc_n(3, [128, 2, 128], F32, "cs")
        cs16 = self.alloc_n(3, [128, 2, 128], F32, "cs16")
        qr = self.alloc_n(2, [128, 8, 128], BF16, "qr")
        kr = self.alloc_n(2, [128, 8, 128], BF16, "kr")
        ta = [self.alloc_n(2, [128, 2, 128], F32, "ta%d" % q) for q in range(4)]
        kk = self.alloc_n(2, [128, D], BF16, "kk")
        vv = self.alloc_n(2, [128, D], BF16, "vv")
        gf = self.alloc_n(2, [128, D], BF16, "gf")
        gb = self.alloc_n(2, [128, D], BF16, "gb")
        csd = I["ropecs"].rearrange("c p n -> p c n")
        cnt = [0]

        hT4 = hT + self.alloc_n(2, [128, 8, 128], BF16, "hTx")
        qr4 = qr + self.alloc_n(2, [128, 8, 128], BF16, "qrx")
        kr3 = kr + self.alloc_n(1, [128, 8, 128], BF16, "krx")
        tak = [self.alloc_n(2, [128, 2, 128], F32, "tak%d" % q) for q in range(4)]

        def rope_group(Wm, dst, base, tab, h, is_ctx, tmp):
            for half in range(2):
                bank = PS[base + half]
                for q4 in range(4):
                    oc = half * 4 + q4
                    self.mm(bank[:, q4 * 128:(q4 + 1) * 128],
                            [(Wm[:, kc, oc * 128:(oc + 1) * 128], h[:, kc, :]) for kc in range(8)])
                dview = dst[:, half * 4:(half + 1) * 4, :]
                if is_ctx:
                    if Wm is WQ:
                        self.copy("act", dview, bank.re("p (k n) -> p k n", k=4))
                    else:
                        self.ts("dve", dview, bank.re("p (k n) -> p k n", k=4), 0.0625, op0=ALU.mult)
                else:
                    bv = bank.re("p (h c n) -> p h c n", h=2, c=2)
                    t1, t2 = bv[:, :, 0, :], bv[:, :, 1, :]
                    cosb = tab[:, 0, :].bc(1, [128, 2, 128])
                    sinb = tab[:, 1, :].bc(1, [128, 2, 128])
                    dv4 = dview.re("p (h c) n -> p h c n", c=2)
                    self.tt("dve", tmp[0][half], t1, cosb, ALU.mult)
                    self.tt("dve", tmp[1][half], t2, sinb, ALU.mult)
                    self.tt("pool", dv4[:, :, 0, :], tmp[0][half], tmp[1][half], ALU.subtract)
                    self.tt("dve", tmp[2][half], t1, sinb, ALU.mult)
                    self.tt("dve", tmp[3][half], t2, cosb, ALU.mult)
                    self.tt("pool", dv4[:, :, 1, :], tmp[2][half], tmp[3][half], ALU.add)

        def tileA(k, t):
            is_ctx = t >= NLT
            need_q = not (last and is_ctx)
            x = xt[k % 2]
            self.dma(x, self.xsrc(i, t))
            self.cut()
            if t == 0:
                self.load_bcr(Wb, i, 0, ("A1", "B1"))
            if t == NLT:
                self.load_bcr(Wb, i, 1, ("A1", "B1"))
            self.rms_mod(x, junk, A1, B1, h32, ss[k % 2], rstd[k % 2], outb=hb[k % 2])
            self.cut()
            h = hT4[k % 4]
            pb = PS[0].bitcast(BF16)
            self.trs([(pb[:, q * 128:(q + 1) * 128], hb[k % 2][:, q * 128:(q + 1) * 128]) for q in range(8)], self.identB)
            self.copy("act", h, pb.re("p (k n) -> p k n", k=8))
            if not is_ctx:
                self.dma(cs[k % 3], T(csd[:, :, t * 128:(t + 1) * 128], Buf("ropecs")))
            self.cut()
            if not is_ctx:
                self.ts("pool", cs16[k % 3], cs[k % 3], 0.0625, op0=ALU.mult)
            if need_q:
                rope_group(WQ, qr4[k % 4], 1, cs[k % 3], h, is_ctx, ta)
            self.cut()
            rope_group(WK, kr3[k % 3], 3, cs16[k % 3], h, is_ctx, tak)
            self.cut()
            nb = 0
            for (Wm, dst, fn) in ((WV, vv[k % 2], AF.Copy), (WGF, gf[k % 2], AF.Silu), (WGB, gb[k % 2], AF.Silu)):
                if last and is_ctx and Wm is not WV:
                    continue
                for half in range(2):
                    bank = PS[5 + nb % 2]
                    nb += 1
                    self.mm(bank, [(h[:, kc, :], Wm[:, kc, half * 512:(half + 1) * 512]) for kc in range(8)])
                    self.act(dst[:, half * 512:(half + 1) * 512], bank, fn)
            pb7 = PS[7].bitcast(BF16)
            self.trs([(pb7[:, oc * 128:(oc + 1) * 128], kr3[k % 3][:, oc, :]) for oc in range(8)], self.identB)
            self.copy("dve", kk[k % 2], pb7)
            self.cut()
            if need_q:
                self.dma(T(self.QTd[t], RB["QT"][t]), qr4[k % 4])
            self.dma(T(self.KTd[t], RB["KT"][t]), kr3[k % 3])
            self.dma(T(self.KKd[t], RB["KK"][t]), kk[k % 2])
            self.dma(T(self.VVd[t], RB["VV"][t]), vv[k % 2])
            if not (last and is_ctx):
                self.dma(T(self.GFd[t], RB["GF"][t]), gf[k % 2])
                self.dma(T(self.GBd[t], RB["GB"][t]), gb[k % 2])

        self.pipeline2(tileA, list(range(NT)))

        p.barrier()
        self.arena_reset()
        dcy = self.alloc([128, 8], F32, "dcy")
        lg = self.alloc([128, 8], F32, "lg")
        nlg = self.alloc([128, 8], F32, "nlg")
        one = self.alloc([128, 1], F32, "one")
        maskT = self.alloc([128, 8, 128], F32, "maskT")
        qdec = self.alloc([128, 8, 128], F32, "qdec")
        kdec = self.alloc([128, 8], F32, "kdec")
        cd = self.alloc([128, 8], F32, "cd")
        diff = self.alloc([128, 128], F32, "diff")
        rowf = self.alloc([128, 128], F32, "rowf")
        rowb = self.alloc([128, 128], F32, "rowb")
        colf = self.alloc([128, 1], F32, "colf")
        colb = self.alloc([128, 1], F32, "colb")
        self.load_bc(dcy[:, 0:4], I["r_decay_f"][j])
        self.load_bc(dcy[:, 4:8], I["r_decay_b"][j])
        self.memset("dve", one, 1.0)
        self.act(nlg, dcy, AF.Exp, scale=-1.0)
        self.act(nlg, nlg, AF.Ln, bias=one, scale=1.0)
        self.ts("dve", lg, nlg, -1.0, op0=ALU.mult)

        def iota(tile, pattern, base, cm):
            o = tile.ap
            p.op("pool", lambda e, o=o: e.iota(o, pattern=pattern, base=base, channel_multiplier=cm,
                                               allow_small_or_imprecise_dtypes=True), (), _b(tile))
        iota(diff, [[1, 128]], 0, -1)
        iota(rowf, [[1, 128]], 1, 0)
        iota(rowb, [[-1, 128]], 128, 0)
        iota(colf, [[0, 1]], 127, -1)
        iota(colb, [[0, 1]], 0, 1)
        for h in range(4):
            mf, mb = maskT[:, h, :], maskT[:, 4 + h, :]
            self.act(mf, diff, AF.Exp, scale=lg[:, h:h + 1])
            o = mf.ap
            p.op("pool", lambda e, o=o: e.affine_select(out=o, in_=o, pattern=[[1, 128]], base=0, channel_multiplier=-1,
                                                        compare_op=ALU.is_ge, fill=0.0), _b(mf), _b(mf))
            self.act(mb, diff, AF.Exp, scale=nlg[:, 4 + h:5 + h])
            o = mb.ap
            p.op("pool", lambda e, o=o: e.affine_select(out=o, in_=o, pattern=[[-1, 128]], base=0, channel_multiplier=1,
                                                        compare_op=ALU.is_gt, fill=0.0), _b(mb), _b(mb))
            self.act(qdec[:, h, :], rowf, AF.Exp, scale=lg[:, h:h + 1])
            self.act(qdec[:, 4 + h, :], rowb, AF.Exp, scale=lg[:, 4 + h:5 + h])
            self.act(kdec[:, h:h + 1], colf, AF.Exp, scale=lg[:, h:h + 1])
            self.act(kdec[:, 4 + h:5 + h], colb, AF.Exp, scale=lg[:, 4 + h:5 + h])
        self.act(cd, lg, AF.Exp, scale=128.0)
        keep = self.aoff

        def scan_pass(d):
            p.barrier()
            self.arena_reset(keep)
            ring = [{n: self.alloc(([128, 8, 128] if n in ("QT", "KT") else [128, D]), BF16, "%s%d" % (n, q))
                     for n in ("QT", "KT", "KK", "VV")} for q in range(5)]
            Qd = self.alloc_n(3, [128, 8, 128], BF16, "Qd")
            Kd = self.alloc_n(3, [128, D], BF16, "Kd")
            attm = [self.alloc_n(2, [128, 128], BF16, "attm%d" % h) for h in range(4)]
            S32 = [self.alloc([128, 2, 256], F32, "S32_%d" % h) for h in range(4)]
            Sbf = [self.alloc([128, 2, 256], BF16, "Sbf_%d" % h) for h in range(4)]
            o32 = self.alloc_n(2, [128, D], F32, "o32")
            HN = self.alloc_n(2, [128, D], F32, "HN")
            bst = [self.alloc_n(2, [128, 6], F32, "bst%d" % h) for h in range(4)]
            mvA = self.alloc_n(2, [128, 4, 2], F32, "mvA")
            rsA = self.alloc_n(2, [128, 4], F32, "rsA")
            nbA = self.alloc_n(2, [128, 4], F32, "nbA")
            for h in range(4):
                self.memset("pool", S32[h], 0.0)
                self.memset("pool", Sbf[h], 0.0)
            if d == 1:
                HNf = self.alloc_n(2, [128, D], F32, "HNf")
                GFc = self.alloc_n(2, [128, D], BF16, "GFc")
                GBc = self.alloc_n(2, [128, D], BF16, "GBc")
                yb = self.alloc_n(2, [128, D], BF16, "yb")
            order = [NLT, NLT + 1] + list(range(NLT)) if d == 0 else [NLT + 1, NLT] + list(range(NLT - 1, -1, -1))

            def chunk(k, c):
                is_ctx = c >= NLT
                want_out = not (last and is_ctx)
                R_ = ring[k % 5]
                if want_out:
                    self.dma(R_["QT"], T(self.QTd[c], RB["QT"][c]))
                    self.dma(R_["KT"], T(self.KTd[c], RB["KT"][c]))
                self.dma(R_["KK"], T(self.KKd[c], RB["KK"][c]))
                self.dma(R_["VV"], T(self.VVd[c], RB["VV"][c]))
                self.cut()
                QTc, KTc, KKc, VVc = R_["QT"], R_["KT"], R_["KK"], R_["VV"]
                qd, kd = Qd[k % 3], Kd[k % 3]
                if want_out:
                    self.tt("dve", qd.re("p (h c) n -> p h c n", c=2), QTc.re("p (h c) n -> p h c n", c=2),
                            qdec[:, d * 4:(d + 1) * 4, :].bc(2, [128, 4, 2, 128]), ALU.mult)
                for h in range(4):
                    self.act(kd[:, h * 256:(h + 1) * 256], KKc[:, h * 256:(h + 1) * 256], AF.Identity,
                             scale=kdec[:, d * 4 + h:d * 4 + h + 1])
                self.cut()
                attb = PS[0] if k % 2 == 0 else PS[7]
                if want_out:
                    for h in range(4):
                        att = attb[:, h * 128:(h + 1) * 128]
                        self.mm(att, [(KTc[:, 2 * h + jj, :], QTc[:, 2 * h + jj, :]) for jj in range(2)])
                    for h in range(4):
                        self.tt("dve", attm[h][k % 2], attb[:, h * 128:(h + 1) * 128], maskT[:, d * 4 + h, :], ALU.mult)
                self.cut()
                psos = [PS[1 + h // 2][:, (h % 2) * 256:(h % 2 + 1) * 256] for h in range(4)]
                vhs = [VVc[:, h * 256:(h + 1) * 256] for h in range(4)]
                o_sb = o32[k % 2]
                if want_out:
                    for h in range(4):
                        self.mm(psos[h], [(attm[h][k % 2], vhs[h]), (qd[:, 2 * h, :], Sbf[h][:, 0, :]),
                                          (qd[:, 2 * h + 1, :], Sbf[h][:, 1, :])])
                for h in range(4):
                    pss = PS[3 + h]
                    for jj in range(2):
                        self.mm(pss[:, jj * 256:(jj + 1) * 256], [(kd[:, h * 256 + jj * 128:h * 256 + (jj + 1) * 128], vhs[h])])
                for h in range(4):
                    s32 = S32[h].re("p a b -> p (a b)")
                    self.stt(s32, s32, cd[:, d * 4 + h:d * 4 + h + 1], PS[3 + h], ALU.mult, ALU.add)
                for h in range(4):
                    self.copy("act", Sbf[h].re("p a b -> p (a b)"), S32[h].re("p a b -> p (a b)"))
                if not want_out:
                    return
                for half in range(2):
                    self.copy("act", o_sb[:, half * 512:(half + 1) * 512], PS[1 + half])
                if d == 1:
                    self.dma(HNf[k % 2], T(self.HNd[c], RB["HN"][c]))
                    self.dma(GFc[k % 2], T(self.GFd[c], RB["GF"][c]))
                    self.dma(GBc[k % 2], T(self.GBd[c], RB["GB"][c]))
                self.cut()
                hn = HN[k % 2]
                mva, rsa, nba = mvA[k % 2], rsA[k % 2], nbA[k % 2]
                for h in range(4):
                    o_, a_ = bst[h][k % 2].ap, o_sb[:, h * 256:(h + 1) * 256].ap
                    p.op("dve", lambda e, o_=o_, a_=a_: e.bn_stats(out=o_, in_=a_), _b(o_sb), _b(bst[h][k % 2]))
                for h in range(4):
                    o_, a_ = mva[:, h, :].ap, bst[h][k % 2].ap
                    p.op("dve", lambda e, o_=o_, a_=a_: e.bn_aggr(out=o_, in_=a_), _b(bst[h][k % 2]), _b(mva))
                self.rsqrt(rsa, mva[:, :, 1], 1.0)
                self.stt(nba, mva[:, :, 0], -1.0, rsa, ALU.mult, ALU.mult)
                for h in range(4):
                    self.act(hn[:, h * 256:(h + 1) * 256], o_sb[:, h * 256:(h + 1) * 256], AF.Identity,
                             bias=nba[:, h:h + 1], scale=rsa[:, h:h + 1])
                self.cut()
                if d == 0:
                    self.dma(T(self.HNd[c], RB["HN"][c]), hn)
                    return
                self.tt("pool", HNf[k % 2], HNf[k % 2], GFc[k % 2], ALU.mult)
                self.tt("dve", hn, hn, GBc[k % 2], ALU.mult)
                self.tt("pool", yb[k % 2], HNf[k % 2], hn, ALU.add)
                self.dma(T(self.YYd[c], RB["YY"][c]), yb[k % 2])

            self.pipeline2(chunk, order)

        scan_pass(0)
        scan_pass(1)

        p.barrier()
        self.arena_reset()
        stage = self.alloc_n(2, [128, 2, 1024], F32, "stg")
        WO = self.alloc([128, 8, D], BF16, "WO")
        self.load_cast(WO, I["r_w_out"][j], stage)
        W = self.alloc_prep(i)
        for n in ("G1", "A2", "B2"):
            W[n] = self.alloc([128, D], F32, n)
        ybc = self.alloc_n(2, [128, D], BF16, "ybc")
        yT = self.alloc_n(2, [128, 8, 128], BF16, "yT")
        xt = self.alloc_n(4, [128, D], F32, "xt")
        xn = self.alloc_n(3, [128, D], F32, "xn")
        tiles = list(range(NLT)) + ([] if last else [NLT, NLT + 1])

        def bc_reload(t, names):
            if t == 0:
                self.load_bcr(W, i, 0, names)
            if t == NLT:
                self.load_bcr(W, i, 1, names)

        xn4 = xn + self.alloc_n(1, [128, D], F32, "xnx")

        def tileC(k, t):
            self.dma(ybc[k % 2], T(self.YYd[t], RB["YY"][t]))
            self.dma(xt[k % 4], self.xsrc(i, t))
            self.cut()
            pb = PS[0].bitcast(BF16)
            self.trs([(pb[:, q * 128:(q + 1) * 128], ybc[k % 2][:, q * 128:(q + 1) * 128]) for q in range(8)], self.identB)
            self.copy("act", yT[k % 2], pb.re("p (k n) -> p k n", k=8))
            self.cut()
            bc_reload(t, ("G1",))
            x_n = xn4[k % 4]
            for half in range(2):
                py = PS[1 + half]
                self.mm(py, [(yT[k % 2][:, kc, :], WO[:, kc, half * 512:(half + 1) * 512]) for kc in range(8)])
                self.tt("dve", x_n[:, half * 512:(half + 1) * 512], py, W["G1"][:, half * 512:(half + 1) * 512], ALU.mult)
            self.tt("pool", x_n, x_n, xt[k % 4], ALU.add)
            self.cut()
            bc_reload(t, ("A2", "B2"))
            self.moe_prep(i, t, x_n, W, k, cuts=True)
            self.store_xh(t, x_n, W, k)

        self.pipeline2(tileC, tiles)

    def phase_routing(self, i):
        p = self.p
        p.barrier()
        self.arena_reset()
        last = i == DEPTH - 1
        PS = self.PS
        NP = 64
        pstage = self.alloc_n(3, [128, 2, 1024], F32, "stg")
        pWS0 = [self.alloc([128, 8, D], BF16, "W0_%d" % m) for m in range(3)]
        self.pref = (pstage, pWS0)
        Mm = None
        affT = self.alloc([NP, NL], F32, "affT")
        msk = self.alloc([NP, NL], F32, "msk")
        cum = self.alloc([NP, NL], F32, "cum")
        junk = self.alloc([NP, NL], BF16, "rjunk")
        posT = self.alloc([128, NT, NP], F32, "posT")
        rhs5 = self.alloc([128, NT, NE, 5], BF16, "rhs5")
        affC = self.alloc([128, 2, 48], F32, "affC")
        sm = {n: self.alloc([NP, 1], F32, n) for n in ("lo", "hi", "mid", "cnt", "ge", "d", "cap", "zero")}
        r1 = self.alloc([128, NT, NE], F32, "r1")
        pc = self.alloc([128, NT, NE], F32, "pc")
        ohs = [self.alloc([128, 512], BF16, "oh%d" % s) for s in range(4)]
        ohc = [self.alloc([128, 32], BF16, "ohc%d" % s) for s in range(2)]
        res = self.alloc([128, 5, 5], F32, "res")
        idf = self.alloc([128, 5], F32, "idf")
        self.memset("dve", res, 0.0)

        mxs = self.alloc([128, NT], F32, "mxs")
        aT = self.affTok
        o_, a_ = mxs.ap, aT.ap
        p.op("dve", lambda e, o_=o_, a_=a_: e.tensor_reduce(out=o_, in_=a_, axis=AX.X, op=ALU.max), _b(aT), _b(mxs))
        self.tt("dve", aT, aT, mxs.bc(2, [128, NT, NE]), ALU.subtract)
        self.act(aT, aT, AF.Exp)
        p.op("dve", lambda e, o_=o_, a_=a_: e.tensor_reduce(out=o_, in_=a_, axis=AX.X, op=ALU.add), _b(aT), _b(mxs))
        self.recip(mxs, mxs)
        self.tt("dve", aT, aT, mxs.bc(2, [128, NT, NE]), ALU.mult)
        if self.debug and "AFF" in self.debug:
            self.dma(T(self.AFFd, Buf("affd")), self.affTok.re("p t e -> p (t e)"))
        def fg():
            self.memset("pool", affT, -1.0)
            for g in range(8):
                pp = PS[g % 2]
                self.trs([(pp[0:NE, k * 128:(k + 1) * 128], self.affTok[:, g * 4 + k, :]) for k in range(4)], self.identF)
                self.copy("dve", affT[0:NE, g * 512:(g + 1) * 512], pp[0:NE, :])
            if not last:
                self.memset("dve", affC, 0.0)
                self.copy("dve", affC[:, :, 32:48], self.affTok[:, NLT:NT, :])
                pp = PS[2]
                self.trs([(pp[0:48, k * 128:(k + 1) * 128], affC[:, k, :]) for k in range(2)], self.identF)
                self.copy("dve", affT[32:48, 0:256], pp[32:48, 0:256])
            self.memset("dve", sm["cap"][0:32, :], float(CAP))
            self.memset("dve", sm["cap"][32:64, :], float(CAPC))
            self.memset("dve", sm["lo"], 0.0)
            self.memset("dve", sm["hi"], 1.0)
            lo, hi, mid, cnt, ge, d, cap = (sm[n] for n in ("lo", "hi", "mid", "cnt", "ge", "d", "cap"))
            for it in range(30):
                self.tt("dve", mid, lo, hi, ALU.add)
                self.ts("dve", mid, mid, 0.5, op0=ALU.mult)
                self.ts("dve", junk, affT, mid, 0.0, op0=ALU.is_ge, op1=ALU.add, accum=cnt)
                self.tt("dve", ge, cnt, cap, ALU.is_ge)
                self.tt("dve", d, mid, lo, ALU.subtract)
                self.stt(lo, d, ge, lo, ALU.mult, ALU.add)
                self.tt("dve", d, hi, mid, ALU.subtract)
                self.stt(hi, d, ge, mid, ALU.mult, ALU.add)
            self.ts("dve", msk, affT, lo, op0=ALU.is_ge)
            o, a = cum.ap, msk.ap
            self.memset("dve", sm["zero"], 0.0)
            z = sm["zero"].ap
            p.op("dve", lambda e, o=o, a=a, z=z: e.tensor_tensor_scan(out=o, data0=a, data1=a, initial=z, op0=ALU.add, op1=ALU.max),
                 _b(msk, sm["zero"]), _b(cum))
            self.tt("dve", cum, cum, msk, ALU.mult)
            self.ts("dve", cum, cum, -1.0, op0=ALU.add)
            for g in range(4):
                pp = PS[3 + g % 2]
                self.trs([(pp[:, k * NP:(k + 1) * NP], cum[:, (g * 8 + k) * 128:(g * 8 + k + 1) * 128]) for k in range(8)],
                         self.identF)
                self.copy("dve", posT[:, g * 8:(g + 1) * 8, :], pp.re("p (k n) -> p k n", k=8))
            if not last:
                pp = PS[5]
                self.trs([(pp[:, k * NP:(k + 1) * NP], cum[:, k * 128:(k + 1) * 128]) for k in range(2)], self.identF)
                self.copy("dve", posT[:, NLT:NT, :], pp[:, 0:2 * NP].re("p (k n) -> p k n", k=2))
            o = pc.ap
            p.op("pool", lambda e, o=o: e.iota(o, pattern=[[0, NT * NE]], base=0, channel_multiplier=1,
                                          allow_small_or_imprecise_dtypes=True), (), _b(pc))
            self.copy("pool", rhs5[:, :, :, 0], pc)
            p.op("pool", lambda e, o=o: e.iota(o, pattern=[[1, NT], [0, NE]], base=0, channel_multiplier=0,
                                          allow_small_or_imprecise_dtypes=True), _b(rhs5), _b(pc))
            self.copy("pool", rhs5[:, :, :, 1], pc)
            self.copy("dve", rhs5[:, :, :, 2], self.affTok)
            self.tt("dve", r1, self.affTok, rhs5[:, :, :, 2], ALU.subtract)
            self.copy("dve", rhs5[:, :, :, 3], r1)
            self.tt("dve", r1, r1, rhs5[:, :, :, 3], ALU.subtract)
            self.copy("dve", rhs5[:, :, :, 4], r1)
            k = 0
            for e in range(NE):
                banks = [PS[(e % 2) * 4 + c] for c in range(4)]
                for t in range(NLT):
                    oh = ohs[k % 4]
                    k += 1
                    self.ts("dve", oh, self.iota512, posT[:, t, e:e + 1], op0=ALU.is_equal)
                    for c in range(4):
                        o_, l_, r_ = banks[c][:, 0:5].ap, oh[:, c * 128:(c + 1) * 128].ap, rhs5[:, t, e, :].ap
                        st, sp_ = (t == 0), (t == NLT - 1)
                        p.op("pe", lambda en, o_=o_, l_=l_, r_=r_, st=st, sp_=sp_: en.matmul(o_, l_, r_, start=st, stop=sp_),
                             _b(oh, rhs5), _b(banks[c]))
                for c in range(4):
                    self.copy("dve", res[:, c, :], banks[c][:, 0:5])
                if not last:
                    for tt_ in range(2):
                        oc_ = ohc[tt_]
                        self.ts("dve", oc_, self.iota512[:, 0:32], posT[:, NLT + tt_, 32 + e:33 + e], op0=ALU.is_equal)
                    bk = banks[0]
                    self.mm(bk[0:32, 8:13], [(ohc[tt_], rhs5[:, NLT + tt_, e, :]) for tt_ in range(2)])
                    self.copy("dve", res[0:32, 4, :], bk[0:32, 8:13])
                nch = 4 if last else 5
                self.stt(idf[:, 0:nch], res[:, 0:nch, 1], 128.0, res[:, 0:nch, 0], ALU.mult, ALU.add)
                self.copy("dve", self.idxAll[:, e, 0:nch], idf[:, 0:nch])
                self.tt("dve", idf[:, 0:nch], res[:, 0:nch, 2], res[:, 0:nch, 3], ALU.add)
                self.tt("dve", self.gateAll[:, e, 0:nch], idf[:, 0:nch], res[:, 0:nch, 4], ALU.add)
            if self.debug and "AFF" in self.debug:
                self.dma(T(self.IDXd, Buf("idxd")), self.idxAll.re("p e c -> p (e c)"))
                self.dma(T(self.GATd, Buf("gatd")), self.gateAll.re("p e c -> p (e c)"))

        def bg():
            I = self.I
            if Mm is not None:
                self.mods_layer(i + 1, Mm, [PS[6], PS[7]])
            for m_, nm in enumerate(("moe_w_gate", "moe_w_up", "moe_w_down")):
                for f in self.cast_pieces(pWS0[m_], I[nm][i, 0], pstage, engs=("act",)):
                    f()

        self.interleave([fg, bg])

    def phase_experts(self, i):
        p = self.p
        p.barrier()
        self.arena_reset()
        last = i == DEPTH - 1
        I = self.I
        PS = self.PS
        stage = self.alloc_n(3, [128, 2, 1024], F32, "stg")
        WS = [[self.alloc([128, 8, D], BF16, "W%d_%d" % (s, m)) for m in range(3)] for s in range(2)]
        if self.pref is not None:
            stage, WS[0] = self.pref
            self.pref = None
            pre0 = True
        else:
            pre0 = False
        NS = 512 if last else 544
        nch = 4 if last else 5
        xs = [self.alloc([128, 5, D], BF16, "xs%d" % s) for s in range(2)]
        xsT = self.alloc([128, 8, 544], BF16, "xsT")
        hidT = self.alloc([128, 8, 544], BF16, "hidT")
        sg = self.alloc([128, 544], F32, "sg")
        ye = self.alloc_n(4, [128, D], F32, "ye")
        fence = self.alloc([128, 1], F32, "fence")
        GATE = [Buf("gate%d" % q) for q in range(NE + 1)]
        G2 = self.alloc([128, D], F32, "G2")
        G2c = self.alloc([128, D], F32, "G2c")
        self.load_bcr({"G2": G2}, i, 0, ("G2",))
        if not last:
            self.load_bcr({"G2": G2c}, i, 1, ("G2",))
        XSC = Buf("xscatter")
        halves = [(0, NS // 2), (NS // 2, NS)]
        nye = 0
        allX = self.XB

        def gather(e):
            x = xs[e % 2]
            for c in range(nch):
                rows = 128 if c < 4 else 32
                o_ = x[0:rows, c, :].ap
                ix = self.idxAll[0:rows, e, c:c + 1].ap
                src = self.H2d
                p.dma("pool", lambda en, o_=o_, ix=ix, src=src: en.indirect_dma_start(
                    out=o_, out_offset=None, in_=src, in_offset=bass.IndirectOffsetOnAxis(ap=ix, axis=0)),
                    _b(self.idxAll) + self.H2B, _b(x))

        def wpieces(e):
            s = e % 2
            return (self.cast_pieces(WS[s][0], I["moe_w_gate"][i, e], stage)
                    + self.cast_pieces(WS[s][1], I["moe_w_up"][i, e], stage)
                    + self.cast_pieces(WS[s][2], I["moe_w_down"][i, e], stage))

        gather(0)
        if not pre0:
            for f in wpieces(0):
                f()
        for e in range(NE):
            pend = []
            if e + 1 < NE:
                gather(e + 1)
                pend = wpieces(e + 1)
            x = xs[e % 2]
            WG, WU, WD = WS[e % 2]
            for c in range(nch):
                rows = 128 if c < 4 else 32
                pb = PS[c % 2].bitcast(BF16)
                self.trs([(pb[:, k * 128:k * 128 + rows], x[0:rows, c, k * 128:(k + 1) * 128]) for k in range(8)],
                         self.identB)
                self.copy("act" if c % 2 else "dve", xsT[:, :, c * 128:c * 128 + rows],
                          pb.re("p (k n) -> p k n", k=8)[:, :, 0:rows])
            for fc in range(8):
                if pend:
                    pend.pop(0)()
                for hi_, (a0, a1) in enumerate(halves):
                    pg, pu = PS[2 + hi_], PS[4 + hi_]
                    n = a1 - a0
                    self.mm(pg[:, 0:n], [(WG[:, kc, fc * 128:(fc + 1) * 128], xsT[:, kc, a0:a1]) for kc in range(8)])
                    self.mm(pu[:, 0:n], [(WU[:, kc, fc * 128:(fc + 1) * 128], xsT[:, kc, a0:a1]) for kc in range(8)])
                    self.act(sg[:, a0:a1], pg[:, 0:n], AF.Silu)
                    self.tt("dve", hidT[:, fc, a0:a1], pu[:, 0:n], sg[:, a0:a1], ALU.mult)
            self.p.op("pool", lambda en, f=fence.ap: en.memset(f, 0.0), (), _b(fence) + [GATE[e]])
            for c in range(nch):
                if pend:
                    pend.pop(0)()
                rows = 128 if c < 4 else 32
                y = ye[nye % 4]
                nye += 1
                g2 = G2 if c < 4 else G2c
                for half in range(2):
                    py = PS[6 + half]
                    self.mm(py[0:rows, :], [(hidT[:, fc, c * 128:c * 128 + rows], WD[:, fc, half * 512:(half + 1) * 512])
                                            for fc in range(8)])
                    self.stt(y[0:rows, half * 512:(half + 1) * 512], py[0:rows, :], self.gateAll[0:rows, e, c:c + 1],
                             g2[0:rows, half * 512:(half + 1) * 512], ALU.mult, ALU.mult)
                o_ = self.Xd
                ix = self.idxAll[0:rows, e, c:c + 1].ap
                i_ = y[0:rows, :].ap
                p.dma("pool", lambda en, ix=ix, i_=i_, o_=o_: en.indirect_dma_start(
                    out=o_, out_offset=bass.IndirectOffsetOnAxis(ap=ix, axis=0), in_=i_, in_offset=None,
                    compute_op=ALU.add), _b(self.idxAll, y, fence) + [GATE[e + 1]], ())
            for f in pend:
                f()

    def phase_final(self):
        p = self.p
        p.barrier()
        self.arena_reset()
        FG = self.alloc([128, D], F32, "FG")
        self.load_bc(FG, self.I["final_norm_g"])
        xts = self.alloc_n(3, [128, D], F32, "fx")
        outs = self.alloc_n(3, [128, D], F32, "fo")
        junk = self.alloc([128, D], BF16, "fjunk")
        ss = self.alloc_n(2, [128, 1], F32, "fss")
        rstd = self.alloc_n(2, [128, 1], F32, "frstd")

        def tile(k, t):
            xt, o = xts[k % 3], outs[k % 3]
            self.dma(xt, T(self.Xd[t * 128:(t + 1) * 128, :], self.XB[t]))
            self.cut()
            self.act(junk, xt, AF.Square, accum=ss[k % 2])
            self.rsqrt(rstd[k % 2], ss[k % 2], 1.0 / D)
            self.stt(o, xt, rstd[k % 2], FG, ALU.mult, ALU.mult)
            self.cut()
            self.dma(T(self.out[t * 128:(t + 1) * 128, :], self.outB[t]), o)

        self.pipeline2(tile, list(range(NLT)))


def rope_tables():
    n = np.arange(NL)
    pos_r = (n // 64).astype(np.float32)
    pos_c = (n % 64).astype(np.float32)
    inv = np.power(np.float32(10000.0), -np.arange(64, dtype=np.float32) / np.float32(64)).astype(np.float32)
    ang = np.concatenate([pos_r[:, None] * inv[None], pos_c[:, None] * inv[None]], axis=-1)
    cs = np.stack([np.cos(ang).T, np.sin(ang).T]).astype(np.float32)
    return np.ascontiguousarray(cs)


_CACHE = {}


def make_in_maps(inputs, cores):
    f = lambda a: np.ascontiguousarray(np.asarray(a, dtype=np.float32))
    shared = {k: f(inputs[k]) for k in ("ada_w", "ada_b", "norm_mix_g", "norm_ffn_g", "a_w_in", "a_ln_g", "a_ln_b",
                                        "a_w_s", "a_b_s", "a_w_out", "r_w_in", "r_decay_f", "r_decay_b", "r_w_out",
                                        "moe_w_router", "moe_w_gate", "moe_w_up", "moe_w_down", "final_norm_g")}
    shared["ropecs"] = rope_tables()
    x, c, ctx, c_ctx = f(inputs["x"]), f(inputs["c"]), f(inputs["ctx"]), f(inputs["c_ctx"])
    maps = []
    for b in cores:
        m = dict(shared)
        m["x"] = x[b]
        m["ctx"] = ctx[b]
        m["cvec"] = np.ascontiguousarray(np.stack([c[b], c_ctx]))
        maps.append(m)
    return maps


def kernel(**inputs):
    if "nc" not in _CACHE:
        _CACHE["nc"] = K().build()
    nc = _CACHE["nc"]
    maps = make_in_maps(inputs, list(range(8)))
    res = run_bass_kernel_spmd(nc, maps, core_ids=list(range(8)))
    out = np.stack([np.asarray(r["out"], dtype=np.float32) for r in res.results], axis=0)
    return out
```

```python
import numpy as np
import threading
from contextlib import ExitStack
import concourse.bass as bass
import concourse.mybir as mybir
from concourse.bass_utils import run_bass_kernel_spmd

F32 = mybir.dt.float32
BF16 = mybir.dt.bfloat16
F16 = mybir.dt.float16
I32 = mybir.dt.int32
AF = mybir.ActivationFunctionType
ALU = mybir.AluOpType
AX = mybir.AxisListType

D = 1024
NL = 4096
NCX = 256
NT = 34
NLT = 32
DEPTH = 4
NE = 16
CAP = 512
CAPC = 32
EPS = 1e-6


class Buf:
    __slots__ = ("name", "w", "r")

    def __init__(self, name=""):
        self.name = name
        self.w = None
        self.r = {}


class T:
    __slots__ = ("ap", "bufs")

    def __init__(self, ap, bufs):
        self.ap = ap
        self.bufs = bufs if isinstance(bufs, (list, tuple)) else [bufs]

    def __getitem__(self, k):
        return T(self.ap[k], self.bufs)

    def re(self, s, **kw):
        return T(self.ap.rearrange(s, **kw), self.bufs)

    def bitcast(self, dt):
        return T(self.ap.bitcast(dt), self.bufs)

    def bc(self, axis, shape):
        return T(self.ap.unsqueeze(axis).to_broadcast(list(shape)), self.bufs)

    @property
    def shape(self):
        return self.ap.shape


class Prog:
    ENGS = ("pe", "dve", "act", "pool", "sp")
    NDMA = {"sp": 24, "pool": 12, "act": 8}

    def __init__(self, nc, same_engine_sync=True):
        self.nc = nc
        self.items = {e: [] for e in self.ENGS}
        self.cnt = {e: 0 for e in self.ENGS}
        self.dma_idx = {q: 0 for q in self.NDMA}
        self.seen = {e: {} for e in self.ENGS}
        self.same_engine_sync = same_engine_sync
        self.sems = {}
        self.last_dma = {}
        self.hook = None

    def _need(self, eng, dep):
        if dep is None:
            return
        key, val = dep
        if key == eng and (eng == "pe" or not self.same_engine_sync):
            return
        if self.seen[eng].get(key, 0) >= val:
            return
        self.seen[eng][key] = val
        self.items[eng].append(("wait", key, val))

    def _deps(self, eng, reads, writes):
        for b in reads:
            self._need(eng, b.w)
        for b in writes:
            self._need(eng, b.w)
            for d in b.r.items():
                self._need(eng, d)

    def _mark(self, me, reads, writes):
        for b in reads:
            if b.r.get(me[0], 0) < me[1]:
                b.r[me[0]] = me[1]
        for b in writes:
            b.w = me
            b.r = {}

    def op(self, eng, fn, reads=(), writes=()):
        self._deps(eng, reads, writes)
        self.cnt[eng] += 1
        me = (eng, self.cnt[eng])
        self.items[eng].append(("op", fn, eng))
        self._mark(me, reads, writes)
        if self.hook is not None:
            self.hook()
        return me

    def dma(self, q, fn, reads=(), writes=()):
        R = self.NDMA[q]
        i = self.dma_idx[q]
        self.dma_idx[q] += 1
        key = ("dma", q, i % R)
        if i >= R:
            self._need(q, (key, 16 * (i // R)))
        self._deps(q, reads, writes)
        me = (key, 16 * (i // R + 1))
        self.items[q].append(("dma", fn, key))
        self._mark(me, reads, writes)
        self.last_dma[key] = me
        if self.hook is not None:
            self.hook()
        return me

    def barrier(self):
        for e in self.ENGS:
            for e2 in ("pe", "dve", "act", "pool"):
                if self.cnt[e2] > 0:
                    self._need(e, (e2, self.cnt[e2]))
            for key, me in self.last_dma.items():
                self._need(e, me)

    def emit(self):
        nc = self.nc
        with ExitStack() as es:
            for e in ("pe", "dve", "act", "pool"):
                self.sems[e] = es.enter_context(nc.semaphore("s_" + e))
            for q, R in self.NDMA.items():
                for j in range(R):
                    self.sems[("dma", q, j)] = es.enter_context(nc.semaphore("d_%s_%d" % (q, j)))
            block = es.enter_context(nc.Block())
            sems = self.sems

            def run(engobj, items):
                for it in items:
                    if it[0] == "wait":
                        engobj.wait_ge(sems[it[1]], it[2])
                    elif it[0] == "op":
                        it[1](engobj).then_inc(sems[it[2]], 1)
                    else:
                        it[1](engobj).then_inc(sems[it[2]], 16)

            @block.tensor
            def _(e):
                run(e, self.items["pe"])

            @block.vector
            def _(e):
                run(e, self.items["dve"])

            @block.scalar
            def _(e):
                run(e, self.items["act"])

            @block.gpsimd
            def _(e):
                run(e, self.items["pool"])

            @block.sync
            def _(e):
                run(e, self.items["sp"])


def _b(*ts):
    out = []
    for t in ts:
        if isinstance(t, T):
            out.extend(t.bufs)
    return out


def _a(x):
    return x.ap if isinstance(x, T) else x


class K:
    def __init__(self, nlayers=DEPTH, debug=False):
        self.nlayers = nlayers
        self.debug = debug
        self.nc = bass.Bass("TRN2", target_bir_lowering=False)
        self.p = Prog(self.nc)
        self.es = ExitStack()
        self.dq = 0
        self.pref = None
        self._atomic = 0
        self._cur = None

    def act(self, out, in_, func, bias=None, scale=None, accum=None):
        kw = {}
        if bias is not None:
            kw["bias"] = _a(bias)
        if scale is not None:
            kw["scale"] = _a(scale)
        if accum is not None:
            kw["accum_out"] = _a(accum)
        o, i = out.ap, in_.ap
        self.p.op("act", lambda e: e.activation(out=o, in_=i, func=func, **kw),
                  _b(in_, bias, scale), _b(out, accum))

    def ts(self, eng, out, in0, s1, s2=None, op0=ALU.mult, op1=None, accum=None):
        kw = {}
        if op1 is not None:
            kw["op1"] = op1
        if accum is not None:
            kw["accum_out"] = _a(accum)
        o, i, a1, a2 = out.ap, in0.ap, _a(s1), _a(s2)
        ename = eng
        self.p.op(ename, lambda e: e.tensor_scalar(out=o, in0=i, scalar1=a1, scalar2=a2, op0=op0, **kw),
                  _b(in0, s1, s2), _b(out, accum))

    def tt(self, eng, out, in0, in1, op):
        o, a, b = out.ap, in0.ap, in1.ap
        self.p.op(eng, lambda e: e.tensor_tensor(out=o, in0=a, in1=b, op=op), _b(in0, in1), _b(out))

    def stt(self, out, in0, scalar, in1, op0, op1):
        o, a, s, b = out.ap, in0.ap, _a(scalar), in1.ap
        self.p.op("dve", lambda e: e.scalar_tensor_tensor(out=o, in0=a, scalar=s, in1=b, op0=op0, op1=op1),
                  _b(in0, scalar, in1), _b(out))

    def copy(self, eng, out, in_):
        o, i = out.ap, in_.ap
        if eng == "act":
            self.p.op("act", lambda e: e.activation(out=o, in_=i, func=AF.Copy), _b(in_), _b(out))
        else:
            self.p.op(eng, lambda e: e.tensor_copy(out=o, in_=i), _b(in_), _b(out))

    def memset(self, eng, out, val):
        o = out.ap
        self.p.op(eng, lambda e: e.memset(o, val), (), _b(out))

    def recip(self, out, in_):
        o, i = out.ap, in_.ap
        self.p.op("dve", lambda e: e.reciprocal(out=o, in_=i), _b(in_), _b(out))

    def mm(self, out, pairs, extra_reads=()):
        o = out.ap
        ps = [(l.ap, r.ap) for l, r in pairs]
        n = len(ps)

        def fn(e):
            ins = None
            for i, (l, r) in enumerate(ps):
                ins = e.matmul(o, l, r, start=(i == 0), stop=(i == n - 1))
            return ins
        rd = []
        for l, r in pairs:
            rd += _b(l, r)
        self.p.op("pe", fn, rd + list(extra_reads), _b(out))

    def tr(self, out, in_, ident):
        o, i = out.ap, in_.ap
        P = i.shape[0]
        d = ident.ap[0:P, 0:P]
        self.p.op("pe", lambda e: e.transpose(out=o, in_=i, identity=d), _b(in_, ident), _b(out))

    def trs(self, items, ident):
        lst = [(o.ap, i.ap) for o, i in items]
        d = ident.ap

        def fn(e):
            ins = None
            for o, i in lst:
                P = i.shape[0]
                ins = e.transpose(out=o, in_=i, identity=d[0:P, 0:P])
            return ins
        rd, wr = _b(ident), []
        for o, i in items:
            rd += _b(i)
            wr += _b(o)
        self.p.op("pe", fn, rd, wr)

    def dma(self, out, in_, q=None, **kw):
        if q is None:
            q = "sp"
        o, i = out.ap, in_.ap
        self.p.dma(q, lambda e: e.dma_start(out=o, in_=i, **kw), _b(in_), _b(out))

    def arena_reset(self, off=None):
        self.aoff = self.persist_end if off is None else off

    def alloc(self, shape, dt, name=""):
        esz = {F32: 4, BF16: 2, F16: 2, I32: 4}[dt]
        n = int(np.prod(shape[1:])) * esz
        n4 = (n + 3) // 4
        off = self.aoff
        self.aoff += n4 + (-n4) % 8
        assert self.aoff <= self.arena_n, ("SBUF arena overflow", name, self.aoff * 4)
        ap = self.arena[0:shape[0], off:off + n4]
        if dt != F32:
            ap = ap.bitcast(dt)
        ap = ap[:, 0:int(np.prod(shape[1:]))]
        if len(shape) > 2:
            names = " ".join("d%d" % i for i in range(len(shape) - 1))
            ap = ap.rearrange("p (%s) -> p %s" % (names, names), **{"d%d" % i: shape[i + 1] for i in range(len(shape) - 1)})
        return T(ap, Buf(name))

    def build(self):
        nc, es = self.nc, self.es
        dbg = self.debug

        def din(name, shape, dt=F32):
            return nc.dram_tensor(name, list(shape), dt, kind="ExternalInput").ap()

        def dscr(name, shape, dt=F32):
            kind = "ExternalOutput" if (dbg and name in dbg) else "Internal"
            return nc.dram_tensor(name, list(shape), dt, kind=kind).ap()

        I = {}
        I["x"] = din("x", [NL, D])
        I["ctx"] = din("ctx", [NCX, D])
        I["cvec"] = din("cvec", [2, D])
        I["ada_w"] = din("ada_w", [DEPTH, D, 6 * D])
        I["ada_b"] = din("ada_b", [DEPTH, 6 * D])
        I["norm_mix_g"] = din("norm_mix_g", [DEPTH, D])
        I["norm_ffn_g"] = din("norm_ffn_g", [DEPTH, D])
        I["a_w_in"] = din("a_w_in", [2, D, 2 * D])
        I["a_ln_g"] = din("a_ln_g", [2, D])
        I["a_ln_b"] = din("a_ln_b", [2, D])
        I["a_w_s"] = din("a_w_s", [2, 8, 128, 128])
        I["a_b_s"] = din("a_b_s", [2, 8, 128])
        I["a_w_out"] = din("a_w_out", [2, D, D])
        I["r_w_in"] = din("r_w_in", [2, D, 5 * D])
        I["r_decay_f"] = din("r_decay_f", [2, 4])
        I["r_decay_b"] = din("r_decay_b", [2, 4])
        I["r_w_out"] = din("r_w_out", [2, D, D])
        I["moe_w_router"] = din("moe_w_router", [DEPTH, D, NE])
        I["moe_w_gate"] = din("moe_w_gate", [DEPTH, NE, D, D])
        I["moe_w_up"] = din("moe_w_up", [DEPTH, NE, D, D])
        I["moe_w_down"] = din("moe_w_down", [DEPTH, NE, D, D])
        I["final_norm_g"] = din("final_norm_g", [D])
        I["ropecs"] = din("ropecs", [2, 128, NL])
        self.I = I
        self.out = nc.dram_tensor("out", [NL, D], F32, kind="ExternalOutput").ap()
        self.outB = [Buf("out%d" % t) for t in range(NLT)]

        self.Xd = dscr("X", [NT * 128, D])
        self.XB = [Buf("X%d" % t) for t in range(NT)]
        self.H2d = dscr("H2", [NT * 128, D], BF16)
        self.H2B = [Buf("H2_%d" % t) for t in range(NT)]
        self.BCRd = dscr("BCR", [DEPTH, 2, 6 * D])
        self.BCRB = [[Buf("BCR%d_%d" % (i, g)) for g in range(24)] for i in range(DEPTH)]
        self.QTd = dscr("QT", [NT, 128, 8, 128], BF16)
        self.KTd = dscr("KT", [NT, 128, 8, 128], BF16)
        self.KKd = dscr("KK", [NT, 128, D], BF16)
        self.VVd = dscr("VV", [NT, 128, D], BF16)
        self.GFd = dscr("GF", [NT, 128, D], BF16)
        self.GBd = dscr("GB", [NT, 128, D], BF16)
        self.HNd = dscr("HN", [NT, 128, D])
        self.YYd = dscr("YY", [NT, 128, D], BF16)
        self.RB = {n: [Buf("%s%d" % (n, t)) for t in range(NT)] for n in ("QT", "KT", "KK", "VV", "GF", "GB", "HN", "YY")}
        if dbg and "AFF" in dbg:
            self.AFFd = dscr("AFF", [128, NT * NE])
            self.IDXd = dscr("IDX", [128, NE * 5], I32)
            self.GATd = dscr("GAT", [128, NE * 5])

        self.arena_n = 49 * 1024
        self.arena = es.enter_context(nc.sbuf_tensor("arena", [128, self.arena_n], F32))
        self.aoff = 0
        self.persist_end = 0
        self.PS = []
        for b in range(8):
            t = es.enter_context(nc.psum_tensor("ps%d" % b, [128, 512], F32))
            self.PS.append(T(t[:, :], Buf("ps%d" % b)))

        self.identF = self.alloc([128, 128], F32, "identF")
        self.identB = self.alloc([128, 128], BF16, "identB")
        self.iota512 = self.alloc([128, 512], F16, "iota512")
        self.affTok = self.alloc([128, NT, NE], F32, "affTok")
        self.csT = self.alloc([128, 8, 2], F32, "csT")
        self.idxAll = self.alloc([128, NE, 5], I32, "idxAll")
        self.gateAll = self.alloc([128, NE, 5], F32, "gateAll")
        self.epsT = self.alloc([128, 1], F32, "eps")
        self.mhalf = self.alloc([128, 4], F32, "mhalf")
        self.Bm = self.alloc([128, 128], F32, "Bm")
        self.persist_end = self.aoff

        self.setup_consts()
        self.phase_mods_fast()
        for i in range(self.nlayers):
            last = i == DEPTH - 1
            if i % 2 == 0:
                self.phase_gmlp(i)
            else:
                self.phase_ret(i)
            self.phase_routing(i)
            self.phase_experts(i)
        if self.nlayers == DEPTH:
            self.phase_final()
        self.p.barrier()
        self.p.emit()
        return self.nc

    def setup_consts(self):
        p = self.p
        iF, iB = self.identF, self.identB
        self.memset("pool", iF, 1.0)
        o = iF.ap
        p.op("pool", lambda e, o=o: e.affine_select(out=o, in_=o, pattern=[[-1, 128]], base=0, channel_multiplier=1,
                                               compare_op=ALU.is_equal, fill=0.0), _b(iF), _b(iF))
        self.copy("dve", iB, iF)
        io = self.iota512.ap
        p.op("pool", lambda e, io=io: e.iota(io, pattern=[[1, 512]], base=0, channel_multiplier=0,
                                      allow_small_or_imprecise_dtypes=True), (), _b(self.iota512))
        self.memset("dve", self.epsT, EPS)
        self.memset("pool", self.mhalf, -0.5)
        self.arena_reset()
        Rt = self.alloc([NE, 128], F32, "Rt")
        for sg_ in range(8):
            self.copy("dve", Rt[:, sg_ * NE:(sg_ + 1) * NE], self.identF[0:NE, 0:NE])
        pbm = self.PS[1][:, 0:128]
        self.mm(pbm, [(Rt, Rt)])
        self.copy("dve", self.Bm, pbm)
        cv2 = self.alloc([2, D], F32, "cv2")
        self.dma(cv2, T(self.I["cvec"], Buf("cvec")))
        self.act(cv2, cv2, AF.Silu)
        pp = self.PS[0]
        self.trs([(pp[:, k * 2:(k + 1) * 2], cv2[:, k * 128:(k + 1) * 128]) for k in range(8)], self.identF)
        self.copy("dve", self.csT, pp[:, 0:16].re("p (k r) -> p k r", k=8))

    def load_bc(self, dst, src_ap1d, buf=None, q="sp"):
        P = dst.shape[0]
        self.dma(dst, T(src_ap1d.partition_broadcast(P), buf if buf is not None else Buf("const")), q=q)

    def cast_pieces(self, dst, src_ap, stage, nk=8, engs=("dve", "act")):
        ncols = dst.shape[2]
        sv = src_ap.rearrange("(kc p) n -> p kc n", p=128)
        out = []
        for c0 in range(0, ncols, 1024):
            cw = min(1024, ncols - c0)
            for k0 in range(0, nk, 2):
                def piece(c0=c0, cw=cw, k0=k0):
                    st = stage[self.dq % len(stage)]
                    self.dq += 1
                    s = st[:, :, 0:cw]
                    self.dma(s, T(sv[:, k0:k0 + 2, c0:c0 + cw], Buf("w")))
                    d = dst[:, k0:k0 + 2, c0:c0 + cw]
                    m = self.dq % 8
                    eng = engs[0] if m < 4 else engs[-1]
                    self.copy(eng, d, s)
                out.append(piece)
        return out

    def load_cast(self, dst, src_ap, stage, nk=8):
        for f in self.cast_pieces(dst, src_ap, stage, nk):
            f()

    def mods_alloc(self):
        M = {}
        M["stg"] = self.alloc_n(2, [128, 8, 256], F32, "adastg")
        M["adab"] = self.alloc_n(2, [2, 256], F32, "adab")
        M["ngp"] = self.alloc_n(2, [2, 256], F32, "ngp")
        M["mod"] = self.alloc_n(2, [2, 256], F32, "modp")
        return M

    def mods_layer(self, i, M, banks):
        I = self.I
        wv = I["ada_w"][i].rearrange("(kc p) n -> p kc n", p=128)
        for cg in range(24):
            c0 = cg * 256
            st, ab, ngp, md = M["stg"][cg % 2], M["adab"][cg % 2], M["ngp"][cg % 2], M["mod"][cg % 2]
            self.dma(st, T(wv[:, :, c0:c0 + 256], Buf("adaw")))
            self.load_bc(ab, I["ada_b"][i][c0:c0 + 256])
            slot = c0 // D
            if slot in (1, 4):
                gsrc = I["norm_mix_g"][i] if slot == 1 else I["norm_ffn_g"][i]
                self.load_bc(ngp, gsrc[c0 - slot * D:c0 - slot * D + 256])
            ps = banks[cg % 2][0:2, 0:256]
            self.mm(ps, [(self.csT[:, kc, :], st[:, kc, :]) for kc in range(8)] + [(self.identF[0:2, 0:2], ab)])
            self.copy("act", md, ps)
            if slot in (1, 4):
                self.ts("pool", md, md, 1.0, op0=ALU.add)
                self.tt("pool", md, md, ngp, ALU.mult)
            self.dma(T(self.BCRd[i, :, c0:c0 + 256], self.BCRB[i][cg]), md)

    def phase_mods_fast(self):
        p = self.p
        p.barrier()
        self.arena_reset()
        I = self.I
        stg = [self.alloc([128, 8, 512], F32, "adastg%d" % s) for s in range(2)]
        modv = self.alloc([2, 6 * D], F32, "modv")
        adab = self.alloc([2, 6 * D], F32, "adab")
        ng = self.alloc([2, 2, D], F32, "ng")
        for i in range(self.nlayers):
            self.load_bc(adab, I["ada_b"][i])
            self.load_bc(ng[:, 0, :], I["norm_mix_g"][i])
            self.load_bc(ng[:, 1, :], I["norm_ffn_g"][i])
            wv = I["ada_w"][i].rearrange("(kc p) n -> p kc n", p=128)
            for cg in range(12):
                st = stg[cg % 2]
                self.dma(st, T(wv[:, :, cg * 512:(cg + 1) * 512], Buf("adaw")))
                ps = self.PS[cg % 2][0:2, :]
                self.mm(ps, [(self.csT[:, kc, :], st[:, kc, :]) for kc in range(8)])
                self.tt("dve", modv[:, cg * 512:(cg + 1) * 512], ps, adab[:, cg * 512:(cg + 1) * 512], ALU.add)
            for s, g in ((1, 0), (4, 1)):
                v = modv[:, s * D:(s + 1) * D]
                self.stt(v, v, 1.0, ng[:, g, :], ALU.add, ALU.mult)
            self.dma(T(self.BCRd[i], self.BCRB[i]), modv)

    def phase_mods(self):
        self.p.barrier()
        self.arena_reset()
        M = self.mods_alloc()
        self.mods_layer(0, M, [self.PS[6], self.PS[7]])

    def interleave(self, fns):
        items = list(range(len(fns)))
        self.pipeline2(lambda k, it: fns[it](), items, all_at_once=True)

    def rsqrt(self, out, in_, scale):
        n = out.shape[1]
        self.ts("dve", out, in_, scale, EPS, op0=ALU.mult, op1=ALU.add)
        self.tt("pool", out, out, self.mhalf[0:out.shape[0], 0:n], ALU.pow)

    def rms_mod(self, xt, junk, A, B, out32, ss, rstd, out_eng="pool", outb=None):
        self.act(junk, xt, AF.Square, accum=ss)
        self.rsqrt(rstd, ss, 1.0 / D)
        self.stt(out32, xt, rstd, A, ALU.mult, ALU.mult)
        if outb is None:
            self.tt(out_eng, out32, out32, B, ALU.add)
        else:
            self.tt(out_eng, outb, out32, B, ALU.add)

    def store_xh(self, t, xn, W, k):
        self.dma(T(self.Xd[t * 128:(t + 1) * 128, :], self.XB[t]), xn)
        self.dma(T(self.H2d[t * 128:(t + 1) * 128, :], self.H2B[t]), W["h2b"][k % 3])

    def moe_prep(self, i, t, xn, W, k, cuts=False):
        A2, B2 = W["A2"], W["B2"]
        h2, junk, h2b, h2T = W["h2"][k % 2], W["junk"], W["h2b"][k % 3], W["h2T"][k % 2]
        ss, rstd = W["ss2"][k % 2], W["rstd2"][k % 2]
        self.rms_mod(xn, junk, A2, B2, h2, ss, rstd, out_eng="dve")
        self.copy("act", h2b, h2)
        if cuts:
            self.cut()
        pa, pb = self.PS[5], self.PS[6]
        for half, pp in ((0, pa), (1, pb)):
            self.trs([(pp[:, q * 128:(q + 1) * 128], h2[:, (half * 4 + q) * 128:(half * 4 + q + 1) * 128]) for q in range(4)],
                     self.identF)
            self.copy("act", h2T[:, half * 4:(half + 1) * 4, :], pp.re("p (k n) -> p k n", k=4))
        if cuts:
            self.cut()
        lg = self.PS[7][:, 0:NE]
        self.mm(lg, [(h2T[:, kc, :], W["WR"][:, kc, :]) for kc in range(8)])
        self.copy("act", self.affTok[:, t, :], lg)

    def alloc_n(self, n, shape, dt, name):
        return [self.alloc(shape, dt, "%s_%d" % (name, q)) for q in range(n)]

    def alloc_prep(self, i):
        W = {}
        W["h2"] = self.alloc_n(2, [128, D], F32, "h2")
        W["junk"] = self.alloc([128, D], BF16, "junk")
        W["h2b"] = self.alloc_n(3, [128, D], BF16, "h2b")
        W["h2T"] = self.alloc_n(2, [128, 8, 128], F32, "h2T")
        W["WR"] = self.alloc([128, 8, NE], F32, "WR")
        for n in ("ss2", "rstd2", "mx", "sm"):
            W[n] = self.alloc_n(2, [128, 1], F32, n)
        W["ex"] = self.alloc_n(2, [128, NE], F32, "ex")
        self.dma(W["WR"], T(self.I["moe_w_router"][i].rearrange("(kc p) e -> p kc e", p=128), Buf("wr")))
        return W

    def pipeline(self, stages, items, weights=None):
        n, S = len(items), len(stages)
        weights = weights or [1] * S
        prog = self.p

        class Coop:
            def __init__(self, fn, w):
                self.fn, self.w = fn, w
                self.go = threading.Semaphore(0)
                self.back = threading.Semaphore(0)
                self.done = False
                self.exc = None
                self.left = 0
                self.th = threading.Thread(target=self.run)
                self.th.start()

            def run(self):
                self.go.acquire()
                try:
                    self.fn()
                except BaseException as e:
                    self.exc = e
                self.done = True
                self.back.release()

            def turn(self):
                self.left = self.w
                prog.hook = self.hook
                self.go.release()
                self.back.acquire()
                prog.hook = None
                if self.exc is not None:
                    raise self.exc

            def hook(self):
                self.left -= 1
                if self.left <= 0:
                    self.back.release()
                    self.go.acquire()

        for step in range(n + S - 1):
            act = []
            for si in [0] + list(range(S - 1, 0, -1)):
                k = step - si
                if 0 <= k < n:
                    act.append(Coop(lambda si=si, k=k: stages[si](k, items[k]), weights[si]))
            while act:
                for c in list(act):
                    c.turn()
                    if c.done:
                        c.th.join()
                        act.remove(c)

    class _Atomic:
        def __init__(self, k):
            self.k = k

        def __enter__(self):
            self.k._atomic += 1

        def __exit__(self, *a):
            self.k._atomic -= 1

    def atomic(self):
        return K._Atomic(self)

    def cut(self):
        c = self._cur
        c.parked = True
        c.back.release()
        c.go.acquire()

    def pipeline2(self, fn, items, all_at_once=False):
        prog = self.p
        outer = self

        class Coop:
            def __init__(self, f):
                self.f = f
                self.go = threading.Semaphore(0)
                self.back = threading.Semaphore(0)
                self.done = False
                self.parked = False
                self.exc = None
                self.th = threading.Thread(target=self.run)
                self.th.start()

            def run(self):
                self.go.acquire()
                try:
                    self.f()
                except BaseException as e:
                    self.exc = e
                self.done = True
                self.back.release()

            def turn(self):
                outer._cur = self
                prog.hook = self.hook
                self.go.release()
                self.back.acquire()
                prog.hook = None
                if self.exc is not None:
                    raise self.exc

            def hook(self):
                if outer._atomic:
                    return
                self.back.release()
                self.go.acquire()

        live = []
        nxt = 0
        while nxt < len(items) or live:
            while nxt < len(items):
                live.insert(0, Coop(lambda k=nxt: fn(k, items[k])))
                nxt += 1
                if not all_at_once:
                    break
            running = list(live)
            while running:
                for c in list(running):
                    c.turn()
                    if c.done:
                        c.th.join()
                        running.remove(c)
                        live.remove(c)
                    elif c.parked:
                        c.parked = False
                        running.remove(c)

    def load_bcr(self, W, i, r, names):
        idx = {"B1": 0, "A1": 1, "G1": 2, "B2": 3, "A2": 4, "G2": 5}
        for n in names:
            k = idx[n]
            self.load_bc(W[n], self.BCRd[i, r, k * D:(k + 1) * D], self.BCRB[i][4 * k:4 * k + 4])

    def xsrc(self, i, t):
        if i == 0:
            if t < NLT:
                return T(self.I["x"][t * 128:(t + 1) * 128, :], Buf("xin"))
            return T(self.I["ctx"][(t - NLT) * 128:(t - NLT + 1) * 128, :], Buf("cin"))
        return T(self.Xd[t * 128:(t + 1) * 128, :], self.XB[t])

    def phase_gmlp(self, i):
        p = self.p
        j = i // 2
        I = self.I
        p.barrier()
        self.arena_reset()
        stage = self.alloc_n(2, [128, 2, 1024], F32, "stg")
        WIN = self.alloc([128, 8, 2 * D], BF16, "WIN")
        WOUT = self.alloc([128, 8, D], BF16, "WOUT")
        WST = self.alloc([128, 8, 128], BF16, "WST")
        bsB = self.alloc([128, 8, 128], F32, "bsB")
        W = self.alloc_prep(i)
        for n in ("A1", "B1", "G1", "A2", "B2"):
            W[n] = self.alloc([128, D], F32, n)
        xt = self.alloc_n(2, [128, D], F32, "xt")
        h32 = self.alloc([128, D], F32, "h32")
        hb = self.alloc_n(2, [128, D], BF16, "hb")
        hT = self.alloc_n(2, [128, 8, 128], BF16, "hT")
        uT = self.alloc_n(2, [128, 8, 128], F32, "uT")
        v = self.alloc_n(2, [128, D], F32, "v")
        vlb = self.alloc_n(2, [128, D], BF16, "vlb")
        t1 = self.alloc([128, 8, 128], F32, "t1")
        prodT = self.alloc_n(2, [128, 8, 128], BF16, "prodT")
        bst = self.alloc_n(2, [128, 2, 6], F32, "bst")
        mv = self.alloc_n(2, [128, 2], F32, "mv")
        rs = self.alloc_n(2, [128, 1], F32, "rs")
        ss = self.alloc_n(2, [128, 1], F32, "ss")
        rstd = self.alloc_n(2, [128, 1], F32, "rstd")
        wsl = t1

        self.load_cast(WIN, I["a_w_in"][j], stage)
        self.load_cast(WOUT, I["a_w_out"][j], stage)
        self.dma(wsl, T(I["a_w_s"][j].rearrange("g n m -> n g m"), Buf("ws")))
        for g in range(8):
            pp = self.PS[g % 2][:, 0:128]
            self.tr(pp, wsl[:, g, :], self.identF)
            self.copy("dve", WST[:, g, :], pp)
        self.load_bc(bsB.re("p g n -> p (g n)"), I["a_b_s"][j].rearrange("g n -> (g n)"))
        PS = self.PS
        lngT = self.alloc([128, 8], F32, "lngT")
        lnbT = self.alloc([128, 8], F32, "lnbT")
        self.dma(lngT, T(I["a_ln_g"][j].rearrange("(g d) -> d g", d=128), Buf("lng")), allow_slow_non_contiguous=True)
        self.dma(lnbT, T(I["a_ln_b"][j].rearrange("(g d) -> d g", d=128), Buf("lnb")), allow_slow_non_contiguous=True)
        onesB = self.alloc([128, 128], BF16, "onesB")
        self.memset("dve", onesB, 1.0)
        for half in range(2):
            pr = PS[half]
            for q4 in range(4):
                g = half * 4 + q4
                self.mm(pr[:, q4 * 128:(q4 + 1) * 128], [(onesB, WST[:, g, :])])
            for q4 in range(4):
                g = half * 4 + q4
                self.stt(bsB[:, g, :], pr[:, q4 * 128:(q4 + 1) * 128], lnbT[:, g:g + 1], bsB[:, g, :], ALU.mult, ALU.add)

        def bc_reload(t, names):
            if t == 0:
                self.load_bcr(W, i, 0, names)
            if t == NLT:
                self.load_bcr(W, i, 1, names)

        uT3 = uT + [self.alloc([128, 8, 128], F32, "uT_2")]
        t1s = [t1, self.alloc([128, 8, 128], F32, "t1_1")]
        xt2 = [T(stage[0].ap[:, r, :], Buf("xt2_%d" % r)) for r in range(2)]
        xn4 = self.alloc_n(3, [128, D], F32, "xn") + [T(stage[1].ap[:, 0, :], Buf("xn3"))]

        def tile(k, t):
            x = xt[k % 2]
            self.dma(x, self.xsrc(i, t))
            self.cut()
            bc_reload(t, ("A1", "B1"))
            self.rms_mod(x, W["junk"], W["A1"], W["B1"], h32, ss[k % 2], rstd[k % 2], outb=hb[k % 2])
            self.cut()
            pb = PS[0].bitcast(BF16)
            self.trs([(pb[:, q * 128:(q + 1) * 128], hb[k % 2][:, q * 128:(q + 1) * 128]) for q in range(8)], self.identB)
            self.copy("act", hT[k % 2], pb.re("p (k n) -> p k n", k=8))
            self.cut()
            h, u, vv_ = hT[k % 2], uT3[k % 3], v[k % 2]
            for half in range(2):
                pu = PS[1 + half]
                for q4 in range(4):
                    oc = half * 4 + q4
                    self.mm(pu[:, q4 * 128:(q4 + 1) * 128],
                            [(WIN[:, kc, oc * 128:(oc + 1) * 128], h[:, kc, :]) for kc in range(8)])
                self.act(u[:, half * 4:(half + 1) * 4, :], pu.re("p (k n) -> p k n", k=4), AF.Gelu_apprx_tanh)
            for half in range(2):
                pv = PS[1 + half]
                self.mm(pv, [(h[:, kc, :], WIN[:, kc, D + half * 512:D + (half + 1) * 512]) for kc in range(8)])
                self.act(vv_[:, half * 512:(half + 1) * 512], pv, AF.Gelu_apprx_tanh)
            self.cut()
            b_, m_, r_ = bst[k % 2], mv[k % 2], rs[k % 2]
            for half in range(2):
                o, a = b_[:, half, :].ap, vv_[:, half * 512:(half + 1) * 512].ap
                p.op("dve", lambda e, o=o, a=a: e.bn_stats(out=o, in_=a), _b(vv_), _b(b_))
            o, a = m_.ap, b_.re("p a b -> p (a b)").ap
            p.op("dve", lambda e, o=o, a=a: e.bn_aggr(out=o, in_=a), _b(b_), _b(m_))
            self.rsqrt(r_, m_[:, 1:2], 1.0)
            self.ts("dve", vlb[k % 2], vv_, m_[:, 0:1], r_, op0=ALU.subtract, op1=ALU.mult)
            self.cut()
            t1_ = t1s[k % 2]
            for half in range(2):
                psm = PS[3 + half]
                with self.atomic():
                    for q4 in range(4):
                        g = half * 4 + q4
                        self.mm(psm[:, q4 * 128:(q4 + 1) * 128], [(vlb[k % 2][:, g * 128:(g + 1) * 128], WST[:, g, :])])
                    for q4 in range(4):
                        g = half * 4 + q4
                        self.stt(t1_[:, g, :], psm[:, q4 * 128:(q4 + 1) * 128], lngT[:, g:g + 1], bsB[:, g, :], ALU.mult, ALU.add)
            self.tt("pool", prodT[k % 2], t1_, u, ALU.mult)
            x2 = xt2[k % 2]
            self.dma(x2, self.xsrc(i, t))
            self.cut()
            bc_reload(t, ("G1",))
            x_n = xn4[k % 4]
            for half in range(2):
                py = PS[3 + half]
                with self.atomic():
                    self.mm(py, [(prodT[k % 2][:, g, :], WOUT[:, g, half * 512:(half + 1) * 512]) for g in range(8)])
                    self.tt("dve", x_n[:, half * 512:(half + 1) * 512], py, W["G1"][:, half * 512:(half + 1) * 512], ALU.mult)
            self.tt("pool", x_n, x_n, x2, ALU.add)
            self.cut()
            bc_reload(t, ("A2", "B2"))
            self.moe_prep(i, t, x_n, W, k, cuts=True)
            self.store_xh(t, x_n, W, k)

        self.pipeline2(tile, list(range(NT)))

    def phase_ret(self, i):
        p = self.p
        j = i // 2
        I = self.I
        PS = self.PS
        last = i == DEPTH - 1
        RB = self.RB
        p.barrier()
        self.arena_reset()
        stage = self.alloc_n(2, [128, 2, 1024], F32, "stg")
        WQ, WK, WV, WGF, WGB = [self.alloc([128, 8, D], BF16, "Wr%d" % m) for m in range(5)]
        for m, Wm in enumerate((WQ, WK, WV, WGF, WGB)):
            self.load_cast(Wm, I["r_w_in"][j][:, m * D:(m + 1) * D], stage)
        A1 = self.alloc([128, D], F32, "A1")
        B1 = self.alloc([128, D], F32, "B1")
        Wb = {"A1": A1, "B1": B1}
        xt = self.alloc_n(2, [128, D], F32, "xt")
        junk = self.alloc([128, D], BF16, "junk")
        h32 = self.alloc([128, D], F32, "h32")
        hb = self.alloc_n(2, [128, D], BF16, "hb")
        hT = self.alloc_n(2, [128, 8, 128], BF16, "hT")
        ss = self.alloc_n(2, [128, 1], F32, "ss")
        rstd = self.alloc_n(2, [128, 1], F32, "rstd")
        cs = self.alloc_n(3, [128, 2, 128], F32, "cs")
        cs16 = self.alloc_n(3, [128, 2, 128], F32, "cs16")
        qr = self.alloc_n(2, [128, 8, 128], BF16, "qr")
        kr = self.alloc_n(2, [128, 8, 128], BF16, "kr")
        ta = [self.alloc_n(2, [128, 2, 128], F32, "ta%d" % q) for q in range(4)]
        kk = self.alloc_n(2, [128, D], BF16, "kk")
        vv = self.alloc_n(2, [128, D], BF16, "vv")
        gf = self.alloc_n(2, [128, D], BF16, "gf")
        gb = self.alloc_n(2, [128, D], BF16, "gb")
        csd = I["ropecs"].rearrange("c p n -> p c n")
        cnt = [0]

        hT4 = hT + self.alloc_n(2, [128, 8, 128], BF16, "hTx")
        qr4 = qr + self.alloc_n(2, [128, 8, 128], BF16, "qrx")
        kr3 = kr + self.alloc_n(1, [128, 8, 128], BF16, "krx")
        tak = [self.alloc_n(2, [128, 2, 128], F32, "tak%d" % q) for q in range(4)]

        def rope_group(Wm, dst, base, tab, h, is_ctx, tmp):
            for half in range(2):
                bank = PS[base + half]
                for q4 in range(4):
                    oc = half * 4 + q4
                    self.mm(bank[:, q4 * 128:(q4 + 1) * 128],
                            [(Wm[:, kc, oc * 128:(oc + 1) * 128], h[:, kc, :]) for kc in range(8)])
                dview = dst[:, half * 4:(half + 1) * 4, :]
                if is_ctx:
                    if Wm is WQ:
                        self.copy("act", dview, bank.re("p (k n) -> p k n", k=4))
                    else:
                        self.ts("dve", dview, bank.re("p (k n) -> p k n", k=4), 0.0625, op0=ALU.mult)
                else:
                    bv = bank.re("p (h c n) -> p h c n", h=2, c=2)
                    t1, t2 = bv[:, :, 0, :], bv[:, :, 1, :]
                    cosb = tab[:, 0, :].bc(1, [128, 2, 128])
                    sinb = tab[:, 1, :].bc(1, [128, 2, 128])
                    dv4 = dview.re("p (h c) n -> p h c n", c=2)
                    self.tt("dve", tmp[0][half], t1, cosb, ALU.mult)
                    self.tt("dve", tmp[1][half], t2, sinb, ALU.mult)
                    self.tt("pool", dv4[:, :, 0, :], tmp[0][half], tmp[1][half], ALU.subtract)
                    self.tt("dve", tmp[2][half], t1, sinb, ALU.mult)
                    self.tt("dve", tmp[3][half], t2, cosb, ALU.mult)
                    self.tt("pool", dv4[:, :, 1, :], tmp[2][half], tmp[3][half], ALU.add)

        def tileA(k, t):
            is_ctx = t >= NLT
            need_q = not (last and is_ctx)
            x = xt[k % 2]
            self.dma(x, self.xsrc(i, t))
            self.cut()
            if t == 0:
                self.load_bcr(Wb, i, 0, ("A1", "B1"))
            if t == NLT:
                self.load_bcr(Wb, i, 1, ("A1", "B1"))
            self.rms_mod(x, junk, A1, B1, h32, ss[k % 2], rstd[k % 2], outb=hb[k % 2])
            self.cut()
            h = hT4[k % 4]
            pb = PS[0].bitcast(BF16)
            self.trs([(pb[:, q * 128:(q + 1) * 128], hb[k % 2][:, q * 128:(q + 1) * 128]) for q in range(8)], self.identB)
            self.copy("act", h, pb.re("p (k n) -> p k n", k=8))
            if not is_ctx:
                self.dma(cs[k % 3], T(csd[:, :, t * 128:(t + 1) * 128], Buf("ropecs")))
            self.cut()
            if not is_ctx:
                self.ts("pool", cs16[k % 3], cs[k % 3], 0.0625, op0=ALU.mult)
            if need_q:
                rope_group(WQ, qr4[k % 4], 1, cs[k % 3], h, is_ctx, ta)
            self.cut()
            rope_group(WK, kr3[k % 3], 3, cs16[k % 3], h, is_ctx, tak)
            self.cut()
            nb = 0
            for (Wm, dst, fn) in ((WV, vv[k % 2], AF.Copy), (WGF, gf[k % 2], AF.Silu), (WGB, gb[k % 2], AF.Silu)):
                if last and is_ctx and Wm is not WV:
                    continue
                for half in range(2):
                    bank = PS[5 + nb % 2]
                    nb += 1
                    self.mm(bank, [(h[:, kc, :], Wm[:, kc, half * 512:(half + 1) * 512]) for kc in range(8)])
                    self.act(dst[:, half * 512:(half + 1) * 512], bank, fn)
            pb7 = PS[7].bitcast(BF16)
            self.trs([(pb7[:, oc * 128:(oc + 1) * 128], kr3[k % 3][:, oc, :]) for oc in range(8)], self.identB)
            self.copy("dve", kk[k % 2], pb7)
            self.cut()
            if need_q:
                self.dma(T(self.QTd[t], RB["QT"][t]), qr4[k % 4])
            self.dma(T(self.KTd[t], RB["KT"][t]), kr3[k % 3])
            self.dma(T(self.KKd[t], RB["KK"][t]), kk[k % 2])
            self.dma(T(self.VVd[t], RB["VV"][t]), vv[k % 2])
            if not (last and is_ctx):
                self.dma(T(self.GFd[t], RB["GF"][t]), gf[k % 2])
                self.dma(T(self.GBd[t], RB["GB"][t]), gb[k % 2])

        self.pipeline2(tileA, list(range(NT)))

        p.barrier()
        self.arena_reset()
        dcy = self.alloc([128, 8], F32, "dcy")
        lg = self.alloc([128, 8], F32, "lg")
        nlg = self.alloc([128, 8], F32, "nlg")
        one = self.alloc([128, 1], F32, "one")
        maskT = self.alloc([128, 8, 128], F32, "maskT")
        qdec = self.alloc([128, 8, 128], F32, "qdec")
        kdec = self.alloc([128, 8], F32, "kdec")
        cd = self.alloc([128, 8], F32, "cd")
        diff = self.alloc([128, 128], F32, "diff")
        rowf = self.alloc([128, 128], F32, "rowf")
        rowb = self.alloc([128, 128], F32, "rowb")
        colf = self.alloc([128, 1], F32, "colf")
        colb = self.alloc([128, 1], F32, "colb")
        self.load_bc(dcy[:, 0:4], I["r_decay_f"][j])
        self.load_bc(dcy[:, 4:8], I["r_decay_b"][j])
        self.memset("dve", one, 1.0)
        self.act(nlg, dcy, AF.Exp, scale=-1.0)
        self.act(nlg, nlg, AF.Ln, bias=one, scale=1.0)
        self.ts("dve", lg, nlg, -1.0, op0=ALU.mult)

        def iota(tile, pattern, base, cm):
            o = tile.ap
            p.op("pool", lambda e, o=o: e.iota(o, pattern=pattern, base=base, channel_multiplier=cm,
                                               allow_small_or_imprecise_dtypes=True), (), _b(tile))
        iota(diff, [[1, 128]], 0, -1)
        iota(rowf, [[1, 128]], 1, 0)
        iota(rowb, [[-1, 128]], 128, 0)
        iota(colf, [[0, 1]], 127, -1)
        iota(colb, [[0, 1]], 0, 1)
        for h in range(4):
            mf, mb = maskT[:, h, :], maskT[:, 4 + h, :]
            self.act(mf, diff, AF.Exp, scale=lg[:, h:h + 1])
            o = mf.ap
            p.op("pool", lambda e, o=o: e.affine_select(out=o, in_=o, pattern=[[1, 128]], base=0, channel_multiplier=-1,
                                                        compare_op=ALU.is_ge, fill=0.0), _b(mf), _b(mf))
            self.act(mb, diff, AF.Exp, scale=nlg[:, 4 + h:5 + h])
            o = mb.ap
            p.op("pool", lambda e, o=o: e.affine_select(out=o, in_=o, pattern=[[-1, 128]], base=0, channel_multiplier=1,
                                                        compare_op=ALU.is_gt, fill=0.0), _b(mb), _b(mb))
            self.act(qdec[:, h, :], rowf, AF.Exp, scale=lg[:, h:h + 1])
            self.act(qdec[:, 4 + h, :], rowb, AF.Exp, scale=lg[:, 4 + h:5 + h])
            self.act(kdec[:, h:h + 1], colf, AF.Exp, scale=lg[:, h:h + 1])
            self.act(kdec[:, 4 + h:5 + h], colb, AF.Exp, scale=lg[:, 4 + h:5 + h])
        self.act(cd, lg, AF.Exp, scale=128.0)
        keep = self.aoff

        def scan_pass(d):
            p.barrier()
            self.arena_reset(keep)
            ring = [{n: self.alloc(([128, 8, 128] if n in ("QT", "KT") else [128, D]), BF16, "%s%d" % (n, q))
                     for n in ("QT", "KT", "KK", "VV")} for q in range(5)]
            Qd = self.alloc_n(3, [128, 8, 128], BF16, "Qd")
            Kd = self.alloc_n(3, [128, D], BF16, "Kd")
            attm = [self.alloc_n(2, [128, 128], BF16, "attm%d" % h) for h in range(4)]
            S32 = [self.alloc([128, 2, 256], F32, "S32_%d" % h) for h in range(4)]
            Sbf = [self.alloc([128, 2, 256], BF16, "Sbf_%d" % h) for h in range(4)]
            o32 = self.alloc_n(2, [128, D], F32, "o32")
            HN = self.alloc_n(2, [128, D], F32, "HN")
            bst = [self.alloc_n(2, [128, 6], F32, "bst%d" % h) for h in range(4)]
            mvA = self.alloc_n(2, [128, 4, 2], F32, "mvA")
            rsA = self.alloc_n(2, [128, 4], F32, "rsA")
            nbA = self.alloc_n(2, [128, 4], F32, "nbA")
            for h in range(4):
                self.memset("pool", S32[h], 0.0)
                self.memset("pool", Sbf[h], 0.0)
            if d == 1:
                HNf = self.alloc_n(2, [128, D], F32, "HNf")
                GFc = self.alloc_n(2, [128, D], BF16, "GFc")
                GBc = self.alloc_n(2, [128, D], BF16, "GBc")
                yb = self.alloc_n(2, [128, D], BF16, "yb")
            order = [NLT, NLT + 1] + list(range(NLT)) if d == 0 else [NLT + 1, NLT] + list(range(NLT - 1, -1, -1))

            def chunk(k, c):
                is_ctx = c >= NLT
                want_out = not (last and is_ctx)
                R_ = ring[k % 5]
                if want_out:
                    self.dma(R_["QT"], T(self.QTd[c], RB["QT"][c]))
                    self.dma(R_["KT"], T(self.KTd[c], RB["KT"][c]))
                self.dma(R_["KK"], T(self.KKd[c], RB["KK"][c]))
                self.dma(R_["VV"], T(self.VVd[c], RB["VV"][c]))
                self.cut()
                QTc, KTc, KKc, VVc = R_["QT"], R_["KT"], R_["KK"], R_["VV"]
                qd, kd = Qd[k % 3], Kd[k % 3]
                if want_out:
                    self.tt("pool" if d == 0 else "dve", qd.re("p (h c) n -> p h c n", c=2), QTc.re("p (h c) n -> p h c n", c=2),
                            qdec[:, d * 4:(d + 1) * 4, :].bc(2, [128, 4, 2, 128]), ALU.mult)
                for h in range(4):
                    self.act(kd[:, h * 256:(h + 1) * 256], KKc[:, h * 256:(h + 1) * 256], AF.Identity,
                             scale=kdec[:, d * 4 + h:d * 4 + h + 1])
                self.cut()
                attb = PS[0] if k % 2 == 0 else PS[7]
                if want_out:
                    for h in range(4):
                        att = attb[:, h * 128:(h + 1) * 128]
                        self.mm(att, [(KTc[:, 2 * h + jj, :], QTc[:, 2 * h + jj, :]) for jj in range(2)])
                    for h in range(4):
                        self.tt("dve", attm[h][k % 2], attb[:, h * 128:(h + 1) * 128], maskT[:, d * 4 + h, :], ALU.mult)
                self.cut()
                psos = [PS[1 + h // 2][:, (h % 2) * 256:(h % 2 + 1) * 256] for h in range(4)]
                vhs = [VVc[:, h * 256:(h + 1) * 256] for h in range(4)]
                o_sb = o32[k % 2]
                if want_out:
                    for h in range(4):
                        self.mm(psos[h], [(attm[h][k % 2], vhs[h]), (qd[:, 2 * h, :], Sbf[h][:, 0, :]),
                                          (qd[:, 2 * h + 1, :], Sbf[h][:, 1, :])])
                for h in range(4):
                    pss = PS[3 + h]
                    for jj in range(2):
                        self.mm(pss[:, jj * 256:(jj + 1) * 256], [(kd[:, h * 256 + jj * 128:h * 256 + (jj + 1) * 128], vhs[h])])
                for h in range(4):
                    s32 = S32[h].re("p a b -> p (a b)")
                    self.stt(s32, s32, cd[:, d * 4 + h:d * 4 + h + 1], PS[3 + h], ALU.mult, ALU.add)
                for h in range(4):
                    self.copy("act", Sbf[h].re("p a b -> p (a b)"), S32[h].re("p a b -> p (a b)"))
                if not want_out:
                    return
                for half in range(2):
                    self.copy("act", o_sb[:, half * 512:(half + 1) * 512], PS[1 + half])
                if d == 1:
                    self.dma(HNf[k % 2], T(self.HNd[c], RB["HN"][c]))
                    self.dma(GFc[k % 2], T(self.GFd[c], RB["GF"][c]))
                    self.dma(GBc[k % 2], T(self.GBd[c], RB["GB"][c]))
                self.cut()
                hn = HN[k % 2]
                mva, rsa, nba = mvA[k % 2], rsA[k % 2], nbA[k % 2]
                for h in range(4):
                    o_, a_ = bst[h][k % 2].ap, o_sb[:, h * 256:(h + 1) * 256].ap
                    p.op("dve", lambda e, o_=o_, a_=a_: e.bn_stats(out=o_, in_=a_), _b(o_sb), _b(bst[h][k % 2]))
                for h in range(4):
                    o_, a_ = mva[:, h, :].ap, bst[h][k % 2].ap
                    p.op("dve", lambda e, o_=o_, a_=a_: e.bn_aggr(out=o_, in_=a_), _b(bst[h][k % 2]), _b(mva))
                self.rsqrt(rsa, mva[:, :, 1], 1.0)
                self.stt(nba, mva[:, :, 0], -1.0, rsa, ALU.mult, ALU.mult)
                for h in range(4):
                    self.act(hn[:, h * 256:(h + 1) * 256], o_sb[:, h * 256:(h + 1) * 256], AF.Identity,
                             bias=nba[:, h:h + 1], scale=rsa[:, h:h + 1])
                self.cut()
                if d == 0:
                    self.dma(T(self.HNd[c], RB["HN"][c]), hn)
                    return
                self.tt("pool", HNf[k % 2], HNf[k % 2], GFc[k % 2], ALU.mult)
                self.tt("dve", hn, hn, GBc[k % 2], ALU.mult)
                self.tt("pool", yb[k % 2], HNf[k % 2], hn, ALU.add)
                self.dma(T(self.YYd[c], RB["YY"][c]), yb[k % 2])

            self.pipeline2(chunk, order)

        scan_pass(0)
        scan_pass(1)

        p.barrier()
        self.arena_reset()
        stage = self.alloc_n(2, [128, 2, 1024], F32, "stg")
        WO = self.alloc([128, 8, D], BF16, "WO")
        self.load_cast(WO, I["r_w_out"][j], stage)
        W = self.alloc_prep(i)
        for n in ("G1", "A2", "B2"):
            W[n] = self.alloc([128, D], F32, n)
        ybc = self.alloc_n(2, [128, D], BF16, "ybc")
        yT = self.alloc_n(2, [128, 8, 128], BF16, "yT")
        xt = self.alloc_n(4, [128, D], F32, "xt")
        xn = self.alloc_n(3, [128, D], F32, "xn")
        tiles = list(range(NLT)) + ([] if last else [NLT, NLT + 1])

        def bc_reload(t, names):
            if t == 0:
                self.load_bcr(W, i, 0, names)
            if t == NLT:
                self.load_bcr(W, i, 1, names)

        xn4 = xn + self.alloc_n(1, [128, D], F32, "xnx")

        def tileC(k, t):
            self.dma(ybc[k % 2], T(self.YYd[t], RB["YY"][t]))
            self.dma(xt[k % 4], self.xsrc(i, t))
            self.cut()
            pb = PS[0].bitcast(BF16)
            self.trs([(pb[:, q * 128:(q + 1) * 128], ybc[k % 2][:, q * 128:(q + 1) * 128]) for q in range(8)], self.identB)
            self.copy("act", yT[k % 2], pb.re("p (k n) -> p k n", k=8))
            self.cut()
            bc_reload(t, ("G1",))
            x_n = xn4[k % 4]
            for half in range(2):
                py = PS[1 + half]
                self.mm(py, [(yT[k % 2][:, kc, :], WO[:, kc, half * 512:(half + 1) * 512]) for kc in range(8)])
                self.tt("dve", x_n[:, half * 512:(half + 1) * 512], py, W["G1"][:, half * 512:(half + 1) * 512], ALU.mult)
            self.tt("pool", x_n, x_n, xt[k % 4], ALU.add)
            self.cut()
            bc_reload(t, ("A2", "B2"))
            self.moe_prep(i, t, x_n, W, k, cuts=True)
            self.store_xh(t, x_n, W, k)

        self.pipeline2(tileC, tiles)

    def phase_routing(self, i):
        p = self.p
        p.barrier()
        self.arena_reset()
        last = i == DEPTH - 1
        PS = self.PS
        NP = 64
        pstage = self.alloc_n(3, [128, 2, 1024], F32, "stg")
        pWS0 = [self.alloc([128, 8, D], BF16, "W0_%d" % m) for m in range(3)]
        self.pref = (pstage, pWS0)
        Mm = None
        affT = self.alloc([NP, NL], F32, "affT")
        msk = self.alloc([NP, NL], F32, "msk")
        cum = self.alloc([NP, NL], F32, "cum")
        junk = self.alloc([NP, NL], BF16, "rjunk")
        posT = self.alloc([128, NT, NP], F32, "posT")
        rhs5 = self.alloc([128, NT, NE, 5], BF16, "rhs5")
        affC = self.alloc([128, 2, 48], F32, "affC")
        sm = {n: self.alloc([NP, 1], F32, n) for n in ("lo", "hi", "mid", "cnt", "ge", "d", "cap", "zero")}
        r1 = self.alloc([128, NT, NE], F32, "r1")
        pc = self.alloc([128, NT, NE], F32, "pc")
        Zc = self.alloc([128, 4, 128], F32, "Zc")
        A2 = self.alloc([128, 512], F32, "A2")
        junkA = self.alloc([128, 512], BF16, "junkA")
        loA, midA, tA = [self.alloc([128, 1], F32, n) for n in ("loA", "midA", "tA")]
        cnt2 = self.alloc([128, 2], F32, "cnt2")
        cntA = cnt2[:, 0:1]
        ohs = [self.alloc([128, 512], BF16, "oh%d" % s) for s in range(4)]
        ohc = [self.alloc([128, 32], BF16, "ohc%d" % s) for s in range(2)]
        res = self.alloc([128, 5, 5], F32, "res")
        idf = self.alloc([128, 5], F32, "idf")
        self.memset("dve", res, 0.0)

        mxs = self.alloc([128, NT], F32, "mxs")
        aT = self.affTok
        o_, a_ = mxs.ap, aT.ap
        p.op("dve", lambda e, o_=o_, a_=a_: e.tensor_reduce(out=o_, in_=a_, axis=AX.X, op=ALU.max), _b(aT), _b(mxs))
        self.tt("dve", aT, aT, mxs.bc(2, [128, NT, NE]), ALU.subtract)
        self.act(aT, aT, AF.Exp)
        p.op("dve", lambda e, o_=o_, a_=a_: e.tensor_reduce(out=o_, in_=a_, axis=AX.X, op=ALU.add), _b(aT), _b(mxs))
        self.recip(mxs, mxs)
        self.tt("dve", aT, aT, mxs.bc(2, [128, NT, NE]), ALU.mult)
        if self.debug and "AFF" in self.debug:
            self.dma(T(self.AFFd, Buf("affd")), self.affTok.re("p t e -> p (t e)"))
        def fg():
            self.memset("pool", affT, -1.0)
            for g in range(8):
                pp = PS[g % 2]
                self.trs([(pp[0:NE, k * 128:(k + 1) * 128], self.affTok[:, g * 4 + k, :]) for k in range(4)], self.identF)
                self.copy("dve", affT[0:NE, g * 512:(g + 1) * 512], pp[0:NE, :])
            if not last:
                self.memset("dve", affC, 0.0)
                self.copy("dve", affC[:, :, 32:48], self.affTok[:, NLT:NT, :])
                pp = PS[2]
                self.trs([(pp[0:48, k * 128:(k + 1) * 128], affC[:, k, :]) for k in range(2)], self.identF)
                self.copy("dve", affT[32:48, 0:256], pp[32:48, 0:256])
            self.copy("dve", Zc.re("p k (g e) -> p k g e", g=8), self.affTok[:, 0:NLT, :].re("p (g k) e -> p k g e", k=4))
            pz = PS[2]
            self.trs([(pz[:, k * 128:(k + 1) * 128], Zc[:, k, :]) for k in range(4)], self.identF)
            self.copy("dve", A2, pz)
            self.memset("dve", sm["lo"], 0.0)
            self.memset("dve", loA, 0.0)
            lo = sm["lo"]
            loC, midC, cntC, tC = lo[32:48, :], sm["mid"][32:48, :], sm["cnt"][32:48, :], sm["d"][32:48, :]
            pcnt = PS[5][:, 0:1]
            self.memset("dve", cnt2, 0.0)
            for it in range(30):
                c = 0.5 ** (it + 1)
                self.ts("dve", midA, loA, c, op0=ALU.add)
                self.ts("dve", junkA, A2, midA, 0.0, op0=ALU.is_ge, op1=ALU.add, accum=cntA)
                self.mm(PS[5][:, 0:2], [(self.Bm, cnt2)])
                if not last:
                    self.ts("dve", midC, loC, c, op0=ALU.add)
                    self.ts("dve", junk[32:48, 0:256], affT[32:48, 0:256], midC, 0.0, op0=ALU.is_ge, op1=ALU.add, accum=cntC)
                    self.ts("dve", tC, cntC, float(CAPC), c, op0=ALU.is_ge, op1=ALU.mult)
                    self.tt("dve", loC, loC, tC, ALU.add)
                self.ts("dve", tA, pcnt, float(CAP), c, op0=ALU.is_ge, op1=ALU.mult)
                self.tt("dve", loA, loA, tA, ALU.add)
            self.copy("dve", lo[0:NE, :], loA[0:NE, :])
            self.ts("dve", msk, affT, lo, op0=ALU.is_ge)
            o, a = cum.ap, msk.ap
            self.memset("dve", sm["zero"], 0.0)
            z = sm["zero"].ap
            p.op("dve", lambda e, o=o, a=a, z=z: e.tensor_tensor_scan(out=o, data0=a, data1=a, initial=z, op0=ALU.add, op1=ALU.max),
                 _b(msk, sm["zero"]), _b(cum))
            self.tt("dve", cum, cum, msk, ALU.mult)
            self.ts("dve", cum, cum, -1.0, op0=ALU.add)
            for g in range(4):
                pp = PS[3 + g % 2]
                self.trs([(pp[:, k * NP:(k + 1) * NP], cum[:, (g * 8 + k) * 128:(g * 8 + k + 1) * 128]) for k in range(8)],
                         self.identF)
                self.copy("dve", posT[:, g * 8:(g + 1) * 8, :], pp.re("p (k n) -> p k n", k=8))
            if not last:
                pp = PS[5]
                self.trs([(pp[:, k * NP:(k + 1) * NP], cum[:, k * 128:(k + 1) * 128]) for k in range(2)], self.identF)
                self.copy("dve", posT[:, NLT:NT, :], pp[:, 0:2 * NP].re("p (k n) -> p k n", k=2))
            o = pc.ap
            p.op("pool", lambda e, o=o: e.iota(o, pattern=[[0, NT * NE]], base=0, channel_multiplier=1,
                                          allow_small_or_imprecise_dtypes=True), (), _b(pc))
            self.copy("pool", rhs5[:, :, :, 0], pc)
            p.op("pool", lambda e, o=o: e.iota(o, pattern=[[1, NT], [0, NE]], base=0, channel_multiplier=0,
                                          allow_small_or_imprecise_dtypes=True), _b(rhs5), _b(pc))
            self.copy("pool", rhs5[:, :, :, 1], pc)
            self.copy("dve", rhs5[:, :, :, 2], self.affTok)
            self.tt("dve", r1, self.affTok, rhs5[:, :, :, 2], ALU.subtract)
            self.copy("dve", rhs5[:, :, :, 3], r1)
            self.tt("dve", r1, r1, rhs5[:, :, :, 3], ALU.subtract)
            self.copy("dve", rhs5[:, :, :, 4], r1)
            k = 0
            for e in range(NE):
                banks = [PS[(e % 2) * 4 + c] for c in range(4)]
                for t in range(NLT):
                    oh = ohs[k % 4]
                    k += 1
                    self.ts("dve", oh, self.iota512, posT[:, t, e:e + 1], op0=ALU.is_equal)
                    for c in range(4):
                        o_, l_, r_ = banks[c][:, 0:5].ap, oh[:, c * 128:(c + 1) * 128].ap, rhs5[:, t, e, :].ap
                        st, sp_ = (t == 0), (t == NLT - 1)
                        p.op("pe", lambda en, o_=o_, l_=l_, r_=r_, st=st, sp_=sp_: en.matmul(o_, l_, r_, start=st, stop=sp_),
                             _b(oh, rhs5), _b(banks[c]))
                for c in range(4):
                    self.copy("dve", res[:, c, :], banks[c][:, 0:5])
                if not last:
                    for tt_ in range(2):
                        oc_ = ohc[tt_]
                        self.ts("dve", oc_, self.iota512[:, 0:32], posT[:, NLT + tt_, 32 + e:33 + e], op0=ALU.is_equal)
                    bk = banks[0]
                    self.mm(bk[0:32, 8:13], [(ohc[tt_], rhs5[:, NLT + tt_, e, :]) for tt_ in range(2)])
                    self.copy("dve", res[0:32, 4, :], bk[0:32, 8:13])
                nch = 4 if last else 5
                self.stt(idf[:, 0:nch], res[:, 0:nch, 1], 128.0, res[:, 0:nch, 0], ALU.mult, ALU.add)
                self.copy("dve", self.idxAll[:, e, 0:nch], idf[:, 0:nch])
                self.tt("dve", idf[:, 0:nch], res[:, 0:nch, 2], res[:, 0:nch, 3], ALU.add)
                self.tt("dve", self.gateAll[:, e, 0:nch], idf[:, 0:nch], res[:, 0:nch, 4], ALU.add)
            if self.debug and "AFF" in self.debug:
                self.dma(T(self.IDXd, Buf("idxd")), self.idxAll.re("p e c -> p (e c)"))
                self.dma(T(self.GATd, Buf("gatd")), self.gateAll.re("p e c -> p (e c)"))

        def bg():
            I = self.I
            if Mm is not None:
                self.mods_layer(i + 1, Mm, [PS[6], PS[7]])
            for m_, nm in enumerate(("moe_w_gate", "moe_w_up", "moe_w_down")):
                for f in self.cast_pieces(pWS0[m_], I[nm][i, 0], pstage, engs=("act",)):
                    f()

        self.interleave([fg, bg])

    def phase_experts(self, i):
        p = self.p
        p.barrier()
        self.arena_reset()
        last = i == DEPTH - 1
        I = self.I
        PS = self.PS
        stage = self.alloc_n(3, [128, 2, 1024], F32, "stg")
        WS = [[self.alloc([128, 8, D], BF16, "W%d_%d" % (s, m)) for m in range(3)] for s in range(2)]
        if self.pref is not None:
            stage, WS[0] = self.pref
            self.pref = None
            pre0 = True
        else:
            pre0 = False
        NS = 512 if last else 544
        nch = 4 if last else 5
        xs = [self.alloc([128, 5, D], BF16, "xs%d" % s) for s in range(2)]
        xsT = self.alloc([128, 8, 544], BF16, "xsT")
        hidT = self.alloc([128, 8, 544], BF16, "hidT")
        sg = self.alloc([128, 544], F32, "sg")
        ye = self.alloc_n(4, [128, D], F32, "ye")
        fence = self.alloc([128, 1], F32, "fence")
        GATE = [Buf("gate%d" % q) for q in range(NE + 1)]
        G2 = self.alloc([128, D], F32, "G2")
        G2c = self.alloc([128, D], F32, "G2c")
        self.load_bcr({"G2": G2}, i, 0, ("G2",))
        if not last:
            self.load_bcr({"G2": G2c}, i, 1, ("G2",))
        XSC = Buf("xscatter")
        halves = [(0, NS // 2), (NS // 2, NS)]
        nye = 0
        allX = self.XB

        def gather(e):
            x = xs[e % 2]
            for c in range(nch):
                rows = 128 if c < 4 else 32
                o_ = x[0:rows, c, :].ap
                ix = self.idxAll[0:rows, e, c:c + 1].ap
                src = self.H2d
                p.dma("pool", lambda en, o_=o_, ix=ix, src=src: en.indirect_dma_start(
                    out=o_, out_offset=None, in_=src, in_offset=bass.IndirectOffsetOnAxis(ap=ix, axis=0)),
                    _b(self.idxAll) + self.H2B, _b(x))

        def wpieces(e):
            s = e % 2
            return (self.cast_pieces(WS[s][0], I["moe_w_gate"][i, e], stage)
                    + self.cast_pieces(WS[s][1], I["moe_w_up"][i, e], stage)
                    + self.cast_pieces(WS[s][2], I["moe_w_down"][i, e], stage))

        gather(0)
        if not pre0:
            for f in wpieces(0):
                f()
        for e in range(NE):
            pend = []
            if e + 1 < NE:
                gather(e + 1)
                pend = wpieces(e + 1)
            x = xs[e % 2]
            WG, WU, WD = WS[e % 2]
            for c in range(nch):
                rows = 128 if c < 4 else 32
                pb = PS[c % 2].bitcast(BF16)
                self.trs([(pb[:, k * 128:k * 128 + rows], x[0:rows, c, k * 128:(k + 1) * 128]) for k in range(8)],
                         self.identB)
                self.copy("act" if c % 2 else "dve", xsT[:, :, c * 128:c * 128 + rows],
                          pb.re("p (k n) -> p k n", k=8)[:, :, 0:rows])
            for fc in range(8):
                if pend:
                    pend.pop(0)()
                for hi_, (a0, a1) in enumerate(halves):
                    pg, pu = PS[2 + hi_], PS[4 + hi_]
                    n = a1 - a0
                    self.mm(pg[:, 0:n], [(WG[:, kc, fc * 128:(fc + 1) * 128], xsT[:, kc, a0:a1]) for kc in range(8)])
                    self.mm(pu[:, 0:n], [(WU[:, kc, fc * 128:(fc + 1) * 128], xsT[:, kc, a0:a1]) for kc in range(8)])
                    self.act(sg[:, a0:a1], pg[:, 0:n], AF.Silu)
                    self.tt("dve", hidT[:, fc, a0:a1], pu[:, 0:n], sg[:, a0:a1], ALU.mult)
            self.p.op("pool", lambda en, f=fence.ap: en.memset(f, 0.0), (), _b(fence) + [GATE[e]])
            for c in range(nch):
                if pend:
                    pend.pop(0)()
                rows = 128 if c < 4 else 32
                y = ye[nye % 4]
                nye += 1
                g2 = G2 if c < 4 else G2c
                for half in range(2):
                    py = PS[6 + half]
                    self.mm(py[0:rows, :], [(hidT[:, fc, c * 128:c * 128 + rows], WD[:, fc, half * 512:(half + 1) * 512])
                                            for fc in range(8)])
                    self.stt(y[0:rows, half * 512:(half + 1) * 512], py[0:rows, :], self.gateAll[0:rows, e, c:c + 1],
                             g2[0:rows, half * 512:(half + 1) * 512], ALU.mult, ALU.mult)
                o_ = self.Xd
                ix = self.idxAll[0:rows, e, c:c + 1].ap
                i_ = y[0:rows, :].ap
                p.dma("pool", lambda en, ix=ix, i_=i_, o_=o_: en.indirect_dma_start(
                    out=o_, out_offset=bass.IndirectOffsetOnAxis(ap=ix, axis=0), in_=i_, in_offset=None,
                    compute_op=ALU.add), _b(self.idxAll, y, fence) + [GATE[e + 1]], ())
            for f in pend:
                f()

    def phase_final(self):
        p = self.p
        p.barrier()
        self.arena_reset()
        FG = self.alloc([128, D], F32, "FG")
        self.load_bc(FG, self.I["final_norm_g"])
        xts = self.alloc_n(3, [128, D], F32, "fx")
        outs = self.alloc_n(3, [128, D], F32, "fo")
        junk = self.alloc([128, D], BF16, "fjunk")
        ss = self.alloc_n(2, [128, 1], F32, "fss")
        rstd = self.alloc_n(2, [128, 1], F32, "frstd")

        def tile(k, t):
            xt, o = xts[k % 3], outs[k % 3]
            self.dma(xt, T(self.Xd[t * 128:(t + 1) * 128, :], self.XB[t]))
            self.cut()
            self.act(junk, xt, AF.Square, accum=ss[k % 2])
            self.rsqrt(rstd[k % 2], ss[k % 2], 1.0 / D)
            self.stt(o, xt, rstd[k % 2], FG, ALU.mult, ALU.mult)
            self.cut()
            self.dma(T(self.out[t * 128:(t + 1) * 128, :], self.outB[t]), o)

        self.pipeline2(tile, list(range(NLT)))


def rope_tables():
    n = np.arange(NL)
    pos_r = (n // 64).astype(np.float32)
    pos_c = (n % 64).astype(np.float32)
    inv = np.power(np.float32(10000.0), -np.arange(64, dtype=np.float32) / np.float32(64)).astype(np.float32)
    ang = np.concatenate([pos_r[:, None] * inv[None], pos_c[:, None] * inv[None]], axis=-1)
    cs = np.stack([np.cos(ang).T, np.sin(ang).T]).astype(np.float32)
    return np.ascontiguousarray(cs)


_CACHE = {}


def make_in_maps(inputs, cores):
    f = lambda a: np.ascontiguousarray(np.asarray(a, dtype=np.float32))
    shared = {k: f(inputs[k]) for k in ("ada_w", "ada_b", "norm_mix_g", "norm_ffn_g", "a_w_in", "a_ln_g", "a_ln_b",
                                        "a_w_s", "a_b_s", "a_w_out", "r_w_in", "r_decay_f", "r_decay_b", "r_w_out",
                                        "moe_w_router", "moe_w_gate", "moe_w_up", "moe_w_down", "final_norm_g")}
    shared["ropecs"] = rope_tables()
    x, c, ctx, c_ctx = f(inputs["x"]), f(inputs["c"]), f(inputs["ctx"]), f(inputs["c_ctx"])
    maps = []
    for b in cores:
        m = dict(shared)
        m["x"] = x[b]
        m["ctx"] = ctx[b]
        m["cvec"] = np.ascontiguousarray(np.stack([c[b], c_ctx]))
        maps.append(m)
    return maps


def kernel(**inputs):
    if "nc" not in _CACHE:
        _CACHE["nc"] = K().build()
    nc = _CACHE["nc"]
    maps = make_in_maps(inputs, list(range(8)))
    res = run_bass_kernel_spmd(nc, maps, core_ids=list(range(8)))
    out = np.stack([np.asarray(r["out"], dtype=np.float32) for r in res.results], axis=0)
    return out
```
